# Optimizing a Trainium2 kernel written in Bass

```python
import math
import jax
import jax.numpy as jnp
from jax import lax
import numpy as np

D_MODEL = 1024
BATCH = 2
SEQ = 8192
DEPTH = 4

GDN_HEADS = 4
GDN_DK = 128
GDN_DV = 128
CONV_WIDTH = 4
GDN_CHUNK = 64
ATT_HEADS = 4
ATT_DH = 128
IDX_HEADS = 4
IDX_DH = 64
TOPK_MAX = 256
Q_BLOCK = 128
GLA_HEADS = 4
GLA_DK = D_MODEL // 2 // GLA_HEADS
GLA_DV = D_MODEL // GLA_HEADS
GLA_RANK = 16
GLA_TAU = 16.0
GLA_CHUNK = 64
N_GROUPS = 4
EXPERTS_PER_GROUP = 8
N_EXPERTS = N_GROUPS * EXPERTS_PER_GROUP
TOP_K_EXPERTS = 2
D_EXPERT = 512
MOE_BLOCK = 128
ROPE_THETA = 10000.0
EPS = 1e-6

GDN_QK = GDN_HEADS * GDN_DK
GDN_V = GDN_HEADS * GDN_DV
ATT_W = ATT_HEADS * ATT_DH
EVEN_SPLITS = [GDN_QK, GDN_QK, GDN_V, GDN_V, GDN_HEADS, GDN_HEADS,
               ATT_W, ATT_W, ATT_W, IDX_HEADS * IDX_DH, IDX_DH, IDX_HEADS]
EVEN_IN = int(sum(EVEN_SPLITS))
EVEN_MIX = GDN_V + ATT_W
ODD_SPLITS = [GLA_HEADS * GLA_DK, GLA_HEADS * GLA_DK, GLA_HEADS * GLA_DV,
              GLA_HEADS * GLA_DV, GLA_RANK]
ODD_IN = int(sum(ODD_SPLITS))
ODD_MIX = GLA_HEADS * GLA_DV

kernel_name = "hybrid_gdn_dsa_gla_hmoe_trunk"

F32 = jnp.float32


def split_cols(t, sizes):
    idx = np.cumsum(sizes)[:-1].tolist()
    return jnp.split(t, idx, axis=-1)


def rms_norm(t, g):
    tf = t.astype(F32)
    y = tf * lax.rsqrt(jnp.mean(tf * tf, axis=-1, keepdims=True) + EPS)
    return (y * g.astype(F32)).astype(t.dtype)


def l2_normalize(t):
    tf = t.astype(F32)
    return (tf * lax.rsqrt(jnp.sum(tf * tf, axis=-1, keepdims=True) + EPS)).astype(t.dtype)


def modulate(hn, shift, scale):
    return hn * (1 + scale[:, None, :]) + shift[:, None, :]


def rope_tables(positions, dim):
    inv = 1.0 / (ROPE_THETA ** (jnp.arange(0, dim, 2, dtype=F32) / dim))
    ang = positions.astype(F32)[..., None] * inv
    return jnp.cos(ang), jnp.sin(ang)


def apply_rope(t, cos, sin):
    t1, t2 = jnp.split(t.astype(F32), 2, axis=-1)
    c = cos[:, :, None, :]
    s = sin[:, :, None, :]
    return jnp.concatenate([t1 * c - t2 * s, t2 * c + t1 * s], axis=-1).astype(t.dtype)


def causal_depthwise_conv(t, w):
    width = w.shape[0]
    return lax.conv_general_dilated(
        t, w[:, None, :].astype(t.dtype), window_strides=(1,),
        padding=((width - 1, 0),), dimension_numbers=('NWC', 'WIO', 'NWC'),
        feature_group_count=t.shape[-1])


def to_chunks(t, chunk):
    b, s, h = t.shape[:3]
    t = t.reshape((b, s // chunk, chunk, h) + t.shape[3:])
    return t.transpose((1, 0, 3, 2) + tuple(range(4, t.ndim)))


def from_chunks(t):
    n, b, h, c = t.shape[:4]
    t = t.transpose((1, 0, 3, 2) + tuple(range(4, t.ndim)))
    return t.reshape((b, n * c, h) + t.shape[4:])


def gated_delta_rule(q, k, v, g, beta):
    dtype = v.dtype
    b_, _, h, dk = q.shape
    dv = v.shape[-1]
    c = GDN_CHUNK
    q = to_chunks(q.astype(F32) * dk ** -0.5, c)
    k = to_chunks(k.astype(F32), c)
    v = to_chunks(v.astype(F32), c)
    g = to_chunks(g.astype(F32), c)
    beta = to_chunks(beta.astype(F32), c)
    gc = jnp.cumsum(g, axis=-1)
    incl = jnp.tril(jnp.ones((c, c), bool))
    strict = jnp.tril(jnp.ones((c, c), bool), -1)
    decay = jnp.exp(jnp.where(incl, gc[..., :, None] - gc[..., None, :], -jnp.inf))
    kb = k * beta[..., None]
    lmat = jnp.where(strict, jnp.einsum('nbhid,nbhjd->nbhij', kb, k) * decay, 0.0)
    eye = jnp.eye(c, dtype=F32)
    tmat = lax.linalg.triangular_solve(eye + lmat, jnp.broadcast_to(eye, lmat.shape),
                                       left_side=True, lower=True, unit_diagonal=True)
    w = tmat @ (kb * jnp.exp(gc)[..., None])
    u = tmat @ (v * beta[..., None])
    aqk = jnp.einsum('nbhid,nbhjd->nbhij', q, k) * decay
    qg = q * jnp.exp(gc)[..., None]
    glast = gc[..., -1:]
    kd = k * jnp.exp(glast - gc)[..., None]

    def step(state, xs):
        qg_i, aqk_i, w_i, u_i, kd_i, gl_i = xs
        v_new = u_i - w_i @ state
        o = qg_i @ state + aqk_i @ v_new
        state = state * jnp.exp(gl_i)[..., None] + jnp.swapaxes(kd_i, -1, -2) @ v_new
        return state, o

    s0 = jnp.zeros((b_, h, dk, dv), F32)
    _, o = lax.scan(step, s0, (qg, aqk, w, u, kd, glast))
    return from_chunks(o).astype(dtype)


def gla_chunked(q, k, v, log_a):
    dtype = v.dtype
    b_, _, h, dk = q.shape
    dv = v.shape[-1]
    c = GLA_CHUNK
    q = to_chunks(q.astype(F32) * dk ** -0.5, c)
    k = to_chunks(k.astype(F32), c)
    v = to_chunks(v.astype(F32), c)
    bcum = jnp.cumsum(to_chunks(log_a.astype(F32), c), axis=-2)
    incl = jnp.tril(jnp.ones((c, c), bool))

    def step(state, xs):
        q_i, k_i, v_i, b_i = xs
        bl = b_i[..., -1:, :]
        o_inter = (q_i * jnp.exp(b_i)) @ state
        diff = jnp.where(incl[..., None], b_i[..., :, None, :] - b_i[..., None, :, :], -jnp.inf)
        att = jnp.einsum('bhid,bhjd,bhijd->bhij', q_i, k_i, jnp.exp(diff))
        o = o_inter + att @ v_i
        state = (state * jnp.swapaxes(jnp.exp(bl), -1, -2)
                 + jnp.swapaxes(k_i * jnp.exp(bl - b_i), -1, -2) @ v_i)
        return state, o

    s0 = jnp.zeros((b_, h, dk, dv), F32)
    _, o = lax.scan(step, s0, (q, k, v, bcum))
    return from_chunks(o).astype(dtype)


def dsa_sparse_attention(q, k, v, q_idx, k_idx, w_idx):
    b_, s_, h, dh = q.shape
    topk = min(TOPK_MAX, s_ // 4)
    nblk = s_ // Q_BLOCK
    key_pos = jnp.arange(s_)
    k_idx32 = k_idx.astype(F32)

    def blockify(t):
        return jnp.swapaxes(t.reshape((b_, nblk, Q_BLOCK) + t.shape[2:]), 0, 1)

    def one_block(args):
        q_b, qi_b, wi_b, start = args
        q_pos = start + jnp.arange(Q_BLOCK)
        dots = jnp.einsum('bqhd,bsd->bqhs', qi_b.astype(F32), k_idx32)
        score = jnp.einsum('bqh,bqhs->bqs', wi_b.astype(F32), jax.nn.relu(dots))
        causal = key_pos[None, :] <= q_pos[:, None]
        score = jnp.where(causal[None], score, -jnp.inf)
        _, sel = lax.top_k(score, topk)
        k_sel = jax.vmap(lambda kk, ii: kk[ii])(k, sel)
        v_sel = jax.vmap(lambda vv, ii: vv[ii])(v, sel)
        logits = jnp.einsum('bqhd,bqkhd->bqhk', q_b, k_sel).astype(F32) * dh ** -0.5
        valid = sel <= q_pos[None, :, None]
        logits = jnp.where(valid[:, :, None, :], logits, -jnp.inf)
        p = jax.nn.softmax(logits, axis=-1).astype(v.dtype)
        return jnp.einsum('bqhk,bqkhd->bqhd', p, v_sel)

    starts = jnp.arange(nblk) * Q_BLOCK
    out = lax.map(one_block, (blockify(q), blockify(q_idx), blockify(w_idx), starts))
    return jnp.swapaxes(out, 0, 1).reshape(b_, s_, h, dh)


def even_mixer(h, cos_a, sin_a, cos_i, sin_i, w_in, conv_w, a_log, dt_bias, out_norm, w_out):
    b_, s_, _ = h.shape
    heads = lambda t, n: t.reshape(b_, s_, n, -1)
    proj = h @ w_in
    qa, ka, va, za, ba, aa, qb, kb, vb, qi, ki, wi = split_cols(proj, EVEN_SPLITS)
    qkv = jax.nn.silu(causal_depthwise_conv(jnp.concatenate([qa, ka, va], axis=-1), conv_w))
    qa, ka, va = split_cols(qkv, [GDN_QK, GDN_QK, GDN_V])
    qa = l2_normalize(heads(qa, GDN_HEADS))
    ka = l2_normalize(heads(ka, GDN_HEADS))
    va = heads(va, GDN_HEADS)
    beta = jax.nn.sigmoid(ba.astype(F32))
    g = -jnp.exp(a_log.astype(F32)) * jax.nn.softplus(aa.astype(F32) + dt_bias.astype(F32))
    oa = gated_delta_rule(qa, ka, va, g, beta)
    oa = rms_norm(oa, out_norm) * jax.nn.silu(heads(za, GDN_HEADS))
    qb = apply_rope(heads(qb, ATT_HEADS), cos_a, sin_a)
    kb = apply_rope(heads(kb, ATT_HEADS), cos_a, sin_a)
    vb = heads(vb, ATT_HEADS)
    qi = apply_rope(heads(qi, IDX_HEADS), cos_i, sin_i)
    ki = apply_rope(ki[:, :, None, :], cos_i, sin_i)[:, :, 0, :]
    wi = wi * (IDX_HEADS * IDX_DH) ** -0.5
    ob = dsa_sparse_attention(qb, kb, vb, qi, ki, wi)
    o = jnp.concatenate([oa.reshape(b_, s_, -1), ob.reshape(b_, s_, -1)], axis=-1)
    return o @ w_out


def gla_mixer(h, w_in, w_gate2, b_gate2, out_norm, w_out):
    b_, s_, _ = h.shape
    heads = lambda t: t.reshape(b_, s_, GLA_HEADS, -1)
    proj = h @ w_in
    q, k, v, r, glr = split_cols(proj, ODD_SPLITS)
    log_a = jax.nn.log_sigmoid((glr @ w_gate2 + b_gate2).astype(F32)) / GLA_TAU
    o = gla_chunked(heads(q), heads(k), heads(v), heads(log_a))
    o = rms_norm(o, out_norm) * jax.nn.silu(heads(r))
    return o.reshape(b_, s_, -1) @ w_out


def hier_moe(h, wg, bg, we, be, w1, w3, w2):
    b_, s_, d = h.shape
    t = b_ * s_
    xt = h.reshape(t, d)
    tok_ids = jnp.arange(t)
    g_logits = (xt @ wg + bg).astype(F32)
    g_prob = jax.nn.softmax(g_logits, axis=-1)
    g_sel = jnp.argmax(g_logits, axis=-1)
    g_gate = g_prob[tok_ids, g_sel]
    e_logits = (jnp.einsum('td,gde->tge', xt, we) + be).astype(F32)
    e_logits = e_logits[tok_ids, g_sel]
    top_l, top_i = lax.top_k(e_logits, TOP_K_EXPERTS)
    gates = g_gate[:, None] * jax.nn.softmax(top_l, axis=-1)
    eid = (g_sel[:, None] * EXPERTS_PER_GROUP + top_i).reshape(-1).astype(jnp.int32)
    wts = gates.reshape(-1)
    tok = jnp.repeat(tok_ids.astype(jnp.int32), TOP_K_EXPERTS)
    m = t * TOP_K_EXPERTS
    counts = jnp.bincount(eid, length=N_EXPERTS)
    padded = ((counts + MOE_BLOCK - 1) // MOE_BLOCK) * MOE_BLOCK
    pad_end = jnp.cumsum(padded)
    pad_start = pad_end - padded
    raw_start = jnp.cumsum(counts) - counts
    order = jnp.argsort(eid)
    se = eid[order]
    dest = pad_start[se] + (jnp.arange(m) - raw_start[se])
    p_rows = (-(-m // MOE_BLOCK) + N_EXPERTS) * MOE_BLOCK
    buf_tok = jnp.zeros((p_rows,), jnp.int32).at[dest].set(tok[order])
    buf_w = jnp.zeros((p_rows,), F32).at[dest].set(wts[order])
    nblk = p_rows // MOE_BLOCK
    blk_exp = jnp.clip(jnp.searchsorted(pad_end, jnp.arange(nblk) * MOE_BLOCK, side='right'),
                       0, N_EXPERTS - 1)
    xb = xt[buf_tok].reshape(nblk, MOE_BLOCK, d)

    def expert_block(args):
        xi, e = args
        hid = jax.nn.silu(xi @ w1[e]) * (xi @ w3[e])
        return hid @ w2[e]

    yb = lax.map(expert_block, (xb, blk_exp)).reshape(p_rows, d)
    y = jnp.zeros((t, d), h.dtype).at[buf_tok].add(yb * buf_w[:, None].astype(yb.dtype))
    return y.reshape(b_, s_, d)


def setup_inputs(seed: int = 0) -> dict:
    key = jax.random.key(seed)
    d = D_MODEL
    n_even = (DEPTH + 1) // 2
    n_odd = DEPTH // 2

    def nrm(i, shape, scale):
        return jax.random.normal(jax.random.fold_in(key, i), shape, F32) * scale

    def gain(i, shape):
        return 1.0 + nrm(i, shape, 0.02)

    return {
        "x": nrm(0, (BATCH, SEQ, d), 1.0),
        "c": nrm(1, (BATCH, d), 1.0),
        "positions": jnp.broadcast_to(jnp.arange(SEQ, dtype=jnp.int32), (BATCH, SEQ)),
        "ada_w": nrm(2, (DEPTH, d, 6 * d), 0.5 * d ** -0.5),
        "ada_b": nrm(3, (DEPTH, 6 * d), 0.02),
        "norm_mix": gain(4, (DEPTH, d)),
        "norm_ffn": gain(5, (DEPTH, d)),
        "even_w_in": nrm(6, (n_even, d, EVEN_IN), d ** -0.5),
        "gdn_conv_w": nrm(7, (n_even, CONV_WIDTH, 2 * GDN_QK + GDN_V), CONV_WIDTH ** -0.5),
        "gdn_a_log": jnp.log(jax.random.uniform(jax.random.fold_in(key, 8), (n_even, GDN_HEADS), F32, 1.0, 16.0)),
        "gdn_dt_bias": nrm(9, (n_even, GDN_HEADS), 0.1),
        "gdn_out_norm": gain(10, (n_even, GDN_DV)),
        "even_w_out": nrm(11, (n_even, EVEN_MIX, d), EVEN_MIX ** -0.5),
        "gla_w_in": nrm(12, (n_odd, d, ODD_IN), d ** -0.5),
        "gla_w_gate2": nrm(13, (n_odd, GLA_RANK, GLA_HEADS * GLA_DK), GLA_RANK ** -0.5),
        "gla_b_gate2": nrm(14, (n_odd, GLA_HEADS * GLA_DK), 0.1),
        "gla_out_norm": gain(15, (n_odd, GLA_DV)),
        "gla_w_out": nrm(16, (n_odd, ODD_MIX, d), ODD_MIX ** -0.5),
        "router_group_w": nrm(17, (DEPTH, d, N_GROUPS), d ** -0.5),
        "router_group_b": nrm(18, (DEPTH, N_GROUPS), 0.01),
        "router_exp_w": nrm(19, (DEPTH, N_GROUPS, d, EXPERTS_PER_GROUP), d ** -0.5),
        "router_exp_b": nrm(20, (DEPTH, N_GROUPS, EXPERTS_PER_GROUP), 0.01),
        "exp_w1": nrm(21, (DEPTH, N_EXPERTS, d, D_EXPERT), d ** -0.5),
        "exp_w3": nrm(22, (DEPTH, N_EXPERTS, d, D_EXPERT), d ** -0.5),
        "exp_w2": nrm(23, (DEPTH, N_EXPERTS, D_EXPERT, d), D_EXPERT ** -0.5),
        "final_norm": gain(24, (d,)),
    }


def reference(x, c, positions, ada_w, ada_b, norm_mix, norm_ffn,
              even_w_in, gdn_conv_w, gdn_a_log, gdn_dt_bias, gdn_out_norm, even_w_out,
              gla_w_in, gla_w_gate2, gla_b_gate2, gla_out_norm, gla_w_out,
              router_group_w, router_group_b, router_exp_w, router_exp_b,
              exp_w1, exp_w3, exp_w2, final_norm):
    cos_a, sin_a = rope_tables(positions, ATT_DH)
    cos_i, sin_i = rope_tables(positions, IDX_DH)
    cond = jax.nn.silu(c)
    for layer in range(DEPTH):
        mod = cond @ ada_w[layer] + ada_b[layer]
        sh_m, sc_m, gt_m, sh_f, sc_f, gt_f = jnp.split(mod, 6, axis=-1)
        h = modulate(rms_norm(x, norm_mix[layer]), sh_m, sc_m)
        i = layer // 2
        if layer % 2 == 0:
            mix = even_mixer(h, cos_a, sin_a, cos_i, sin_i, even_w_in[i], gdn_conv_w[i],
                             gdn_a_log[i], gdn_dt_bias[i], gdn_out_norm[i], even_w_out[i])
        else:
            mix = gla_mixer(h, gla_w_in[i], gla_w_gate2[i], gla_b_gate2[i],
                            gla_out_norm[i], gla_w_out[i])
        x = x + gt_m[:, None, :] * mix
        h = modulate(rms_norm(x, norm_ffn[layer]), sh_f, sc_f)
        x = x + gt_f[:, None, :] * hier_moe(h, router_group_w[layer], router_group_b[layer],
                                           router_exp_w[layer], router_exp_b[layer],
                                           exp_w1[layer], exp_w3[layer], exp_w2[layer])
    return rms_norm(x, final_norm)
```

```python
import numpy as np
import ml_dtypes
from concourse.bass_utils import run_bass_kernel_spmd

from contextlib import ExitStack
import numpy as np
import concourse.bass as bass
import concourse.mybir as mybir

F32 = mybir.dt.float32
BF16 = mybir.dt.bfloat16
I32 = mybir.dt.int32
AF = mybir.ActivationFunctionType
ALU = mybir.AluOpType
AX = mybir.AxisListType

NDS = 24


class Buf:
    def __init__(self, t, name):
        self.t = t
        self.name = name
        self.w = {}
        self.r = {}

    def __getitem__(self, idx):
        return self.t[idx]


class View(Buf):
    pass


class K:
    def __init__(self):
        self.nc = bass.Bass("TRN2", target_bir_lowering=False)
        self.es = ExitStack()
        nc = self.nc
        self.eng = dict(pe=nc.tensor, act=nc.scalar, dve=nc.vector, pool=nc.gpsimd, sp=nc.sync)
        self.sem = {e: self.es.enter_context(nc.semaphore("s_" + e)) for e in self.eng}
        self.cnt = {e: 0 for e in self.eng}
        self.waited = {e: {} for e in self.eng}
        self.dsem = [self.es.enter_context(nc.semaphore("d%d" % i)) for i in range(NDS)]
        self.dcnt = [0] * NDS
        self.dnext = 0
        self.dnext_sw = 0
        self.nwaits = 0
        self.nins = 0
        self.out_toks = {}

    def sb(self, name, shape, dt=F32):
        esz = 2 if dt == BF16 else 4
        n = 1
        for d in shape[1:]:
            n *= d
        if (n * esz) % 64 != 0:
            npad = ((n * esz + 63) // 64) * 64 // esz
            t = self.es.enter_context(self.nc.sbuf_tensor("sb_" + name, [shape[0], npad], dt))
            v = t[:, 0:n]
            if len(shape) == 3:
                v = v.rearrange("p (a b) -> p a b", a=shape[1])
            return Buf(v, name)
        t = self.es.enter_context(self.nc.sbuf_tensor("sb_" + name, list(shape), dt))
        return Buf(t, name)

    def ps(self, name, shape, dt=F32):
        t = self.es.enter_context(self.nc.psum_tensor("ps_" + name, list(shape), dt))
        return Buf(t, name)

    def dram_in(self, name, shape, dt=F32):
        return Buf(self.nc.dram_tensor(name, list(shape), dt, kind="ExternalInput"), name)

    def dram_out(self, name, shape, dt=F32):
        return Buf(self.nc.dram_tensor(name, list(shape), dt, kind="ExternalOutput"), name)

    def dram_tmp(self, name, shape, dt=F32):
        return Buf(self.nc.dram_tensor(name, list(shape), dt), name)

    def _semof(self, key):
        if key[0] == 'c':
            return self.sem[key[1]], 1
        return self.dsem[key[1]], 16

    def _wait(self, e, deps):
        w = self.waited[e]
        for key, val in deps.items():
            if w.get(key, 0) >= val:
                continue
            s, mult = self._semof(key)
            self.eng[e].wait_ge(s, val * mult)
            w[key] = val
            self.nwaits += 1

    def _deps(self, e, reads, writes, waw=True, self_sync=True):
        deps = {}
        def add(d):
            for k, v in d.items():
                if not self_sync and k == ('c', e):
                    continue
                if deps.get(k, 0) < v:
                    deps[k] = v
        for b in reads:
            add(b.w)
        for b in writes:
            add(b.r)
            if waw:
                add(b.w)
        return deps

    def _mark(self, key, val, reads, writes):
        for b in reads:
            if b.r.get(key, 0) < val:
                b.r[key] = val
        for b in writes:
            if b.w.get(key, 0) < val:
                b.w[key] = val

    def op(self, e, emit, reads=(), writes=(), waw=True, self_sync=True):
        self._wait(e, self._deps(e, reads, writes, waw, self_sync))
        ins = emit(self.eng[e])
        self.cnt[e] += 1
        ins.then_inc(self.sem[e], 1)
        self.nins += 1
        self._mark(('c', e), self.cnt[e], reads, writes)
        return ins

    def dma(self, q, out_ap, in_ap, reads=(), writes=(), **kw):
        if q == 'pool':
            i = 16 + self.dnext_sw
            self.dnext_sw = (self.dnext_sw + 1) % (NDS - 16)
        else:
            i = self.dnext
            self.dnext = (self.dnext + 1) % 16
        deps = self._deps(q, reads, writes)
        if self.dcnt[i] > 0:
            k = ('d', i)
            if deps.get(k, 0) < self.dcnt[i]:
                deps[k] = self.dcnt[i]
        self._wait(q, deps)
        ins = self.eng[q].dma_start(out=out_ap, in_=in_ap, **kw)
        self.dcnt[i] += 1
        ins.then_inc(self.dsem[i], 16)
        self.nins += 1
        self._mark(('d', i), self.dcnt[i], reads, writes)
        return ins

    def barrier(self, bufs=()):
        deps = {('c', e): c for e, c in self.cnt.items() if c > 0}
        for i in range(NDS):
            if self.dcnt[i] > 0:
                deps[('d', i)] = self.dcnt[i]
        for e in self.eng:
            d = {k: v for k, v in deps.items() if k != ('c', e)}
            self._wait(e, d)

    def finish(self, out_bufs):
        deps = {}
        for b in out_bufs:
            for k, v in b.w.items():
                if deps.get(k, 0) < v:
                    deps[k] = v
        self._wait('sp', deps)
        d2 = {('c', e): c for e, c in self.cnt.items() if c > 0 and e != 'sp'}
        self._wait('sp', d2)
        self.es.close()
        return self.nc

    def mm(self, out, lhsT, rhs, start, stop, reads, writes):
        return self.op('pe', lambda E: E.matmul(out, lhsT, rhs, start=start, stop=stop),
                       reads=reads, writes=writes, waw=False, self_sync=False)

    def tr(self, out, in_, ident, reads, writes):
        return self.op('pe', lambda E: E.transpose(out, in_, ident[:]),
                       reads=list(reads) + [ident], writes=writes, waw=False, self_sync=False)


A_T = 2048
A_TG = 512
A_NG = A_T // A_TG
A_D = 1024
A_KC = A_D // 128
A_EPS = 1e-6
A_PI = float(np.pi)


def norm_modulate(k, xT_d, xg, hT, ones, gmod, shift, sq, tmp, ps_ss, rstd):
    for g in range(A_NG):
        tsl = slice(g * A_TG, (g + 1) * A_TG)
        xT = xg[g % 2]
        k.dma('sp' if g % 2 == 0 else 'act', xT[:], xT_d[:, tsl].rearrange("(c p) n -> p c n", p=128), reads=[xT_d], writes=[xT])
        for c in range(A_KC):
            k.op('act', lambda E: E.activation(sq[:], xT[:, c, :], AF.Square), reads=[xT], writes=[sq])
            k.mm(ps_ss[:], ones[:], sq[:], c == 0, c == A_KC - 1, reads=[ones, sq], writes=[ps_ss])
        k.op('act', lambda E: E.activation(rstd[:], ps_ss[:], AF.Ln, bias=A_EPS, scale=1.0 / A_D), reads=[ps_ss], writes=[rstd])
        k.op('act', lambda E: E.activation(rstd[:], rstd[:], AF.Exp, scale=-0.5), reads=[rstd], writes=[rstd])
        for c in range(A_KC):
            k.op('dve', lambda E: E.scalar_tensor_tensor(tmp[:], xT[:, c, :], gmod[:, c:c + 1], rstd[:], ALU.mult, ALU.mult),
                 reads=[xT, gmod, rstd], writes=[tmp])
            k.op('act', lambda E: E.activation(hT[:, c, tsl], tmp[:], AF.Identity, bias=shift[:, c:c + 1], scale=1.0),
                 reads=[tmp, shift], writes=[hT], waw=False)


def adaln_vectors(k, condT_d, ada_w_d, ada_b_d, ncol_chunks, cond, wst, ps_mod, modv):
    k.dma('sp', cond[:], condT_d[:], reads=[condT_d], writes=[cond])
    k.op('act', lambda E: E.activation(cond[:], cond[:], AF.Silu), reads=[cond], writes=[cond])
    k.dma('sp', modv[:], ada_b_d[:], reads=[ada_b_d], writes=[modv])
    for j in range(ncol_chunks):
        k.dma('sp' if j % 2 == 0 else 'act', wst[j % 2][:],
              ada_w_d[:, j * 128:(j + 1) * 128].rearrange("(c p) n -> p c n", p=128),
              reads=[ada_w_d], writes=[wst[j % 2]])
        for c in range(A_KC):
            k.mm(ps_mod[:, j:j + 1], wst[j % 2][:, c, :], cond[:, c:c + 1], c == 0, c == A_KC - 1,
                 reads=[wst[j % 2], cond], writes=[ps_mod])
    k.op('dve', lambda E: E.tensor_tensor(modv[:], modv[:], ps_mod[:, 0:ncol_chunks], ALU.add), reads=[modv, ps_mod], writes=[modv])


def build_A(kind):
    k = K()
    if kind == 'even':
        NCW = 3840 + 1280 + 64 + 64 + 12
        NOUT = 3916
    else:
        NCW = 3088
        NOUT = 3088 + 512
    xT_d = k.dram_in("xT", [A_D, A_T])
    condT_d = k.dram_in("condT", [128, A_KC])
    adaw_d = k.dram_in("ada_w", [A_D, 2 * A_D])
    adab_d = k.dram_in("ada_b", [128, 2 * A_KC])
    g_d = k.dram_in("norm_g", [128, A_KC])
    w_d = k.dram_in("w_ext", [A_D, NCW])
    out_d = k.dram_out("projT", [NOUT, A_T])
    outbf_d = k.dram_out("projbf", [1536, A_T], BF16) if kind == 'even' else None
    if kind == 'even':
        pos_d = k.dram_in("pos", [1, A_T], I32)
        inv_d = k.dram_in("inv", [128, 2])
        sgn_d = k.dram_in("sgn", [128, 2])
    else:
        wg2_d = k.dram_in("wg2", [16, 512])
        bg2_d = k.dram_in("bg2", [128, 4])

    xg = [k.sb("xg%d" % i, [128, A_KC, A_TG]) for i in range(2)]
    hT = k.sb("hT_s", [128, A_KC, A_T], BF16)
    ones = k.sb("ones", [128, 128])
    cond = k.sb("cond", [128, A_KC])
    modv = k.sb("modv", [128, 2 * A_KC])
    gmod = k.sb("gmod", [128, A_KC])
    gvec = k.sb("gvec", [128, A_KC])
    wst = [k.sb("wst%d" % i, [128, A_KC, 128]) for i in range(2)]
    sq = k.sb("sq", [128, A_TG])
    tmp = k.sb("tmp", [128, A_TG])
    rstd = k.sb("rstd", [128, A_TG])
    ps_ss = k.ps("ps_ss", [128, A_TG])
    ps_mod = k.ps("ps_mod", [128, 512])

    k.op('pool', lambda E: E.memset(ones[:], 1.0), writes=[ones])
    k.dma('pool', gvec[:], g_d[:], reads=[g_d], writes=[gvec])
    adaln_vectors(k, condT_d, adaw_d, adab_d, 2 * A_KC, cond, wst, ps_mod, modv)
    k.op('dve', lambda E: E.scalar_tensor_tensor(gmod[:], modv[:, A_KC:2 * A_KC], 1.0, gvec[:], ALU.add, ALU.mult),
         reads=[modv, gvec], writes=[gmod])
    norm_modulate(k, xT_d, xg, hT, ones, gmod, modv, sq, tmp, ps_ss, rstd)

    WG = 512
    wf = [k.sb("wf%d" % i, [128, A_KC, WG]) for i in range(2)]
    wb = [k.sb("wb%d" % i, [128, A_KC, WG], BF16) for i in range(2)]
    pacc = [k.ps("pacc%d" % i, [128, A_TG]) for i in range(4)]
    ost = [k.sb("ost%d" % i, [128, A_T]) for i in range(2)]
    ngroups = (NCW + WG - 1) // WG
    state = dict(pi=0, oi=0, gi=0)

    def load_group(gi):
        c0 = gi * WG
        n = min(WG, NCW - c0)
        b = gi % 2
        k.dma('sp', wf[b][:, 0:4, 0:n], w_d[0:512, c0:c0 + n].rearrange("(c p) n -> p c n", p=128), reads=[w_d], writes=[wf[b]])
        k.dma('act', wf[b][:, 4:8, 0:n], w_d[512:1024, c0:c0 + n].rearrange("(c p) n -> p c n", p=128), reads=[w_d], writes=[wf[b]])
        k.op('pool', lambda E: E.tensor_copy(wb[b][:, :, 0:n], wf[b][:, :, 0:n]), reads=[wf[b]], writes=[wb[b]])

    def proj_chunk(col0, m, ps, g):
        gi = col0 // WG
        b = gi % 2
        lc = col0 - gi * WG
        for c in range(A_KC):
            k.mm(ps[0:m, :], wb[b][:, c, lc:lc + m], hT[:, c, g * A_TG:(g + 1) * A_TG], c == 0, c == A_KC - 1,
                 reads=[wb[b], hT], writes=[ps])

    obf = k.sb("obf", [128, A_T], BF16)

    def store_rows(o, row0, m):
        k.dma('pool', out_d[row0:row0 + m, :], o[0:m, :], reads=[o], writes=[out_d])
        if kind == 'even' and 2048 <= row0 < 3584:
            k.op('pool', lambda E: E.tensor_copy(obf[:], o[:]), reads=[o], writes=[obf])
            k.dma('pool', outbf_d[row0 - 2048:row0 - 2048 + m, :], obf[0:m, :], reads=[obf], writes=[outbf_d])

    def nxt_ps():
        p = pacc[state['pi'] % 4]
        state['pi'] += 1
        return p

    def nxt_o():
        o = ost[state['oi'] % 2]
        state['oi'] += 1
        return o

    slot = {}

    def need(gi):
        if slot.get(gi % 2) != gi:
            load_group(gi)
            slot[gi % 2] = gi

    def plain(col0, m, row0):
        need(col0 // WG)
        o = nxt_o()
        for g in range(A_NG):
            ps = nxt_ps()
            proj_chunk(col0, m, ps, g)
            eng = 'dve' if g % 2 == 0 else 'act'
            if eng == 'dve':
                k.op('dve', lambda E: E.tensor_copy(o[0:m, g * A_TG:(g + 1) * A_TG], ps[0:m, :]), reads=[ps], writes=[o], waw=False)
            else:
                k.op('act', lambda E: E.copy(o[0:m, g * A_TG:(g + 1) * A_TG], ps[0:m, :]), reads=[ps], writes=[o], waw=False)
        store_rows(o, row0, m)
        return o

    if kind == 'even':
        posi = k.sb("posi", [128, A_T], I32)
        posf = k.sb("posf", [128, A_T])
        inv = k.sb("inv", [128, 2])
        sgn = k.sb("sgn", [128, 2])
        tabs = {}
        k.dma('sp', posi[:], pos_d[0:1, :].partition_broadcast(128), reads=[pos_d], writes=[posi])
        k.dma('sp', inv[:], inv_d[:], reads=[inv_d], writes=[inv])
        k.dma('sp', sgn[:], sgn_d[:], reads=[sgn_d], writes=[sgn])
        k.op('dve', lambda E: E.tensor_copy(posf[:], posi[:]), reads=[posi], writes=[posf])
        ang = ost[0]
        yred = ost[1]
        for nm, col in (('a', 0), ('i', 1)):
            C = k.sb("C" + nm, [128, A_T])
            S = k.sb("S" + nm, [128, A_T])
            def sin_of(dst, phase):
                k.op('dve', lambda E: E.tensor_scalar_mul(ang[:], posf[:], inv[:, col:col + 1]), reads=[posf, inv], writes=[ang])
                if phase != 0.0:
                    k.op('dve', lambda E: E.tensor_scalar_add(ang[:], ang[:], phase), reads=[ang], writes=[ang])
                k.op('dve', lambda E: E.tensor_scalar_mul(yred[:], ang[:], 1.0 / (2 * A_PI)), reads=[ang], writes=[yred])
                k.op('dve', lambda E: E.tensor_copy(posi[:], yred[:]), reads=[yred], writes=[posi])
                k.op('dve', lambda E: E.tensor_copy(yred[:], posi[:]), reads=[posi], writes=[yred])
                k.op('dve', lambda E: E.scalar_tensor_tensor(ang[:], yred[:], -2 * A_PI, ang[:], ALU.mult, ALU.add), reads=[yred, ang], writes=[ang])
                k.op('dve', lambda E: E.tensor_scalar(yred[:], ang[:], A_PI, -2 * A_PI, ALU.is_gt, ALU.mult), reads=[ang], writes=[yred])
                k.op('dve', lambda E: E.tensor_tensor(ang[:], ang[:], yred[:], ALU.add), reads=[ang, yred], writes=[ang])
                k.op('dve', lambda E: E.tensor_scalar(ang[:], ang[:], -3.1415925, 3.1415925, ALU.max, ALU.min), reads=[ang], writes=[ang])
                k.op('act', lambda E: E.activation(dst[:], ang[:], AF.Sin), reads=[ang], writes=[dst])
            sin_of(S, 0.0)
            sin_of(C, 0.5 * A_PI)
            k.op('dve', lambda E: E.tensor_scalar_mul(S[:], S[:], sgn[:, col:col + 1]), reads=[S, sgn], writes=[S])
            tabs[nm] = (C, S)

        def roped(col0, col0_sw, m, row0, tab):
            C, S = tabs[tab]
            need(col0 // WG)
            need(col0_sw // WG)
            o = nxt_o()
            for g in range(A_NG):
                tsl = slice(g * A_TG, (g + 1) * A_TG)
                ps = nxt_ps()
                ps2 = nxt_ps()
                proj_chunk(col0, m, ps, g)
                proj_chunk(col0_sw, m, ps2, g)
                k.op('dve', lambda E: E.tensor_tensor(o[0:m, tsl], ps[0:m, :], C[0:m, tsl], ALU.mult), reads=[ps, C], writes=[o], waw=False)
                k.op('dve', lambda E: E.tensor_tensor(tmp[0:m, :], ps2[0:m, :], S[0:m, tsl], ALU.mult), reads=[ps2, S], writes=[tmp])
                k.op('pool', lambda E: E.tensor_tensor(o[0:m, tsl], o[0:m, tsl], tmp[0:m, :], ALU.add), reads=[tmp, o], writes=[o], waw=False)
            store_rows(o, row0, m)

        for j in range(16):
            plain(j * 128, 128, j * 128)
        for j in range(4):
            roped(4 * 512 + j * 128, 5 * 512 + j * 128, 128, 2048 + j * 128, 'a')
        for j in range(4):
            roped(6 * 512 + j * 128, 7 * 512 + j * 128, 128, 2560 + j * 128, 'a')
        for j in range(4):
            plain(8 * 512 + j * 128, 128, 3072 + j * 128)
        for j in range(2):
            roped(9 * 512 + j * 128, 9 * 512 + 256 + j * 128, 128, 3584 + j * 128, 'i')
        roped(10 * 512, 10 * 512 + 64, 64, 3840, 'i')
        plain(10 * 512 + 128, 12, 3904)
    else:
        for j in range(24):
            plain(j * 128, 128, j * 128)
        o_glr0 = plain(3072, 16, 3072)
        o_glr = k.sb("glr_s", [16, A_T])
        k.op('pool', lambda E: E.tensor_copy(o_glr[:], o_glr0[0:16, :]), reads=[o_glr0], writes=[o_glr])
        wg2 = k.sb("wg2", [16, 512])
        bg2 = k.sb("bg2", [128, 4])
        k.dma('sp', wg2[:], wg2_d[:], reads=[wg2_d], writes=[wg2])
        k.dma('sp', bg2[:], bg2_d[:], reads=[bg2_d], writes=[bg2])
        t2 = k.sb("t2", [128, A_TG])
        for j in range(4):
            o = nxt_o()
            for g in range(A_NG):
                tsl = slice(g * A_TG, (g + 1) * A_TG)
                ps = nxt_ps()
                k.mm(ps[:], wg2[:, j * 128:(j + 1) * 128], o_glr[0:16, tsl], True, True, reads=[wg2, o_glr], writes=[ps])
                k.op('act', lambda E: E.activation(tmp[:], ps[:], AF.Identity, bias=bg2[:, j:j + 1], scale=1.0), reads=[ps, bg2], writes=[tmp])
                k.op('act', lambda E: E.activation(t2[:], tmp[:], AF.Abs), reads=[tmp], writes=[t2])
                k.op('act', lambda E: E.activation(t2[:], t2[:], AF.Exp, scale=-1.0), reads=[t2], writes=[t2])
                k.op('act', lambda E: E.activation(t2[:], t2[:], AF.Ln, bias=1.0, scale=1.0), reads=[t2], writes=[t2])
                k.op('dve', lambda E: E.scalar_tensor_tensor(tmp[:], tmp[:], 0.0, t2[:], ALU.min, ALU.subtract), reads=[tmp, t2], writes=[tmp])
                k.op('dve', lambda E: E.tensor_scalar_mul(o[:, tsl], tmp[:], 1.0 / 16.0), reads=[tmp], writes=[o], waw=False)
            store_rows(o, 3088 + j * 128, 128)
    return k.finish([out_d] + ([outbf_d] if kind == "even" else []))


def host_inputs_A(kind, layer, inputs, xT_cores):
    i = layer // 2
    ada_w = np.asarray(inputs["ada_w"][layer])
    ada_b = np.asarray(inputs["ada_b"][layer])
    c = np.asarray(inputs["c"])
    g = np.asarray(inputs["norm_mix"][layer])
    maps = []
    if kind == 'even':
        W = np.asarray(inputs["even_w_in"][i])
        offs = np.cumsum([0, 512, 512, 512, 512, 4, 4, 512, 512, 512, 256, 64, 4])
        qa, ka, va, za, ba, aa, qb, kb, vb, qi, ki, wi = [W[:, offs[j]:offs[j + 1]] for j in range(12)]

        def swap(m, dh):
            n = m.shape[1] // dh
            m4 = m.reshape(m.shape[0], n, 2, dh // 2)
            return m4[:, :, ::-1, :].reshape(m.shape[0], -1)
        w_ext = np.concatenate([qa, ka, va, za, qb, swap(qb, 128), kb, swap(kb, 128), vb, qi, swap(qi, 64), ki, swap(ki, 64), ba, aa, wi], axis=1)
        p = np.arange(128)
        inv_a = (1.0 / (10000.0 ** (np.arange(0, 128, 2, dtype=np.float32) / 128))).astype(np.float32)
        inv_i = (1.0 / (10000.0 ** (np.arange(0, 64, 2, dtype=np.float32) / 64))).astype(np.float32)
        inv = np.stack([inv_a[p % 64], inv_i[p % 32]], axis=1).astype(np.float32)
        sgn = np.stack([np.where(p < 64, -1.0, 1.0), np.where((p % 64) < 32, -1.0, 1.0)], axis=1).astype(np.float32)
    else:
        w_ext = np.asarray(inputs["gla_w_in"][i])
    w_ext = np.ascontiguousarray(w_ext, dtype=np.float32)
    adaw = np.ascontiguousarray(ada_w[:, 0:2 * A_D])
    for core in range(8):
        b = core // 4
        s0 = (core % 4) * A_T
        m = {
            "xT": xT_cores[core],
            "condT": np.ascontiguousarray(c[b].reshape(A_KC, 128).T),
            "ada_w": adaw,
            "ada_b": np.ascontiguousarray(ada_b[0:2 * A_D].reshape(2 * A_KC, 128).T),
            "norm_g": np.ascontiguousarray(g.reshape(A_KC, 128).T),
            "w_ext": w_ext,
        }
        if kind == 'even':
            m["pos"] = np.ascontiguousarray(np.asarray(inputs["positions"])[b:b + 1, s0:s0 + A_T]).astype(np.int32)
            m["inv"] = inv
            m["sgn"] = sgn
        else:
            m["wg2"] = np.ascontiguousarray(inputs["gla_w_gate2"][i])
            m["bg2"] = np.ascontiguousarray(np.asarray(inputs["gla_b_gate2"][i]).reshape(4, 128).T)
        maps.append(m)
    return maps


GLA_S_LEN = 8192
GLA_CH = 128
GLA_NCH = GLA_S_LEN // GLA_CH
GLA_GRP = 8
GLA_NGRP = GLA_NCH // GLA_GRP
GLA_DK = 128
GLA_DV = 256
GLA_EPS = 1e-6


def build_gla(k=None, nchunks=GLA_NCH):
    own = k is None
    if own:
        k = K()
    qT_d = k.dram_in("qT", [GLA_DK, GLA_S_LEN])
    kT_d = k.dram_in("kT", [GLA_DK, GLA_S_LEN])
    kt_d = k.dram_in("k_tok", [GLA_S_LEN, GLA_DK])
    la_d = k.dram_in("la_tok", [GLA_S_LEN, GLA_DK])
    v_d = k.dram_in("v_tok", [GLA_S_LEN, GLA_DV])
    r_d = k.dram_in("r_tok", [GLA_S_LEN, GLA_DV])
    g_d = k.dram_in("gnorm", [1, GLA_DV])
    U_d = k.dram_in("U", [128, 128])
    o_d = k.dram_out("o_tok", [GLA_S_LEN, GLA_DV])

    U = k.sb("U", [128, 128])
    Ub = k.sb("Ub", [128, 128], BF16)
    gb = k.sb("gb", [128, GLA_DV])
    k.dma('sp', U[:], U_d[:], reads=[U_d], writes=[U])
    k.dma('sp', gb[:], g_d[0:1, :].partition_broadcast(128), reads=[g_d], writes=[gb])
    k.op('dve', lambda E: E.tensor_copy(Ub[:], U[:]), reads=[U], writes=[Ub])

    S = k.sb("S", [128, GLA_DV])
    Sb = k.sb("Sb", [128, GLA_DV], BF16)
    k.op('dve', lambda E: E.memset(S[:], 0.0), writes=[S])
    k.op('dve', lambda E: E.memset(Sb[:], 0.0), writes=[Sb])

    qTg = [k.sb("qTg%d" % i, [128, GLA_GRP * GLA_CH]) for i in range(2)]
    kTg = [k.sb("kTg%d" % i, [128, GLA_GRP * GLA_CH]) for i in range(2)]
    ktg = [k.sb("ktg%d" % i, [128, GLA_GRP, GLA_DK]) for i in range(2)]
    lag = [k.sb("lag%d" % i, [128, GLA_GRP, GLA_DK]) for i in range(2)]
    vg = [k.sb("vg%d" % i, [128, GLA_GRP, GLA_DV]) for i in range(2)]
    vgb = [k.sb("vgb%d" % i, [128, GLA_GRP, GLA_DV], BF16) for i in range(2)]
    rg = [k.sb("rg%d" % i, [128, GLA_GRP, GLA_DV]) for i in range(2)]
    og = [k.sb("og%d" % i, [128, GLA_GRP, GLA_DV]) for i in range(2)]

    ps_bT = k.ps("bT", [128, 128])
    ps_bt = k.ps("bt", [128, 128])
    ps_A = k.ps("A", [128, 128])
    ps_o = k.ps("o", [128, GLA_DV])
    ps_kv = k.ps("kv", [128, GLA_DV])
    E_ = k.sb("E", [128, 128])
    En = k.sb("En", [128, 128])
    Et = k.sb("Et", [128, 128])
    qe = k.sb("qe", [128, 128], BF16)
    ke = k.sb("ke", [128, 128], BF16)
    ket = k.sb("ket", [128, 128], BF16)
    Am = k.sb("Am", [128, 128], BF16)
    junk = k.sb("junk", [128, GLA_DV])
    ss = k.sb("ss", [128, 1])
    rstd = k.sb("rstd", [128, 1])

    ngrp = (nchunks + GLA_GRP - 1) // GLA_GRP
    for gi in range(ngrp):
        b = gi % 2
        t0 = gi * GLA_GRP * GLA_CH
        tsl = slice(t0, t0 + GLA_GRP * GLA_CH)
        k.dma('sp', qTg[b][:], qT_d[:, tsl], reads=[qT_d], writes=[qTg[b]])
        k.dma('act', kTg[b][:], kT_d[:, tsl], reads=[kT_d], writes=[kTg[b]])
        k.dma('sp', ktg[b][:], kt_d[tsl, :].rearrange("(c p) d -> p c d", p=128), reads=[kt_d], writes=[ktg[b]])
        k.dma('act', lag[b][:], la_d[tsl, :].rearrange("(c p) d -> p c d", p=128), reads=[la_d], writes=[lag[b]])
        k.dma('sp', vg[b][:], v_d[tsl, :].rearrange("(c p) d -> p c d", p=128), reads=[v_d], writes=[vg[b]])
        k.dma('act', rg[b][:], r_d[tsl, :].rearrange("(c p) d -> p c d", p=128), reads=[r_d], writes=[rg[b]])
        k.op('pool', lambda E: E.tensor_copy(vgb[b][:], vg[b][:]), reads=[vg[b]], writes=[vgb[b]])
        k.op('act', lambda E: E.activation(rg[b][:], rg[b][:], AF.Silu), reads=[rg[b]], writes=[rg[b]])
        for c in range(GLA_GRP):
            k.op('pool', lambda E: E.tensor_tensor(rg[b][:, c, :], rg[b][:, c, :], gb[:], ALU.mult), reads=[rg[b], gb], writes=[rg[b]])
        for c in range(GLA_GRP):
            if gi * GLA_GRP + c >= nchunks:
                break
            csl = slice(c * GLA_CH, (c + 1) * GLA_CH)
            k.mm(ps_bT[:], lag[b][:, c, :], U[:], True, True, reads=[lag[b], U], writes=[ps_bT])
            k.mm(ps_bt[:], U[:], lag[b][:, c, :], True, True, reads=[lag[b], U], writes=[ps_bt])
            k.op('act', lambda E: E.activation(E_[:], ps_bT[:], AF.Exp), reads=[ps_bT], writes=[E_])
            k.op('act', lambda E: E.activation(En[:], ps_bT[:], AF.Exp, scale=-1.0), reads=[ps_bT], writes=[En])
            k.op('act', lambda E: E.activation(Et[:], ps_bt[:], AF.Exp, scale=-1.0), reads=[ps_bt], writes=[Et])
            k.op('dve', lambda E: E.scalar_tensor_tensor(qe[:], qTg[b][:, csl], float(GLA_DK) ** -0.5, E_[:], ALU.mult, ALU.mult),
                 reads=[qTg[b], E_], writes=[qe])
            k.op('dve', lambda E: E.tensor_tensor(ke[:], kTg[b][:, csl], En[:], ALU.mult), reads=[kTg[b], En], writes=[ke])
            k.op('pool', lambda E: E.tensor_tensor(ket[:], ktg[b][:, c, :], Et[:], ALU.mult), reads=[ktg[b], Et], writes=[ket])
            k.mm(ps_A[:], ke[:], qe[:], True, True, reads=[ke, qe], writes=[ps_A])
            k.op('dve', lambda E: E.tensor_tensor(Am[:], ps_A[:], U[:], ALU.mult), reads=[ps_A, U], writes=[Am])
            k.mm(ps_o[:], qe[:], Sb[:], True, False, reads=[qe, Sb], writes=[ps_o])
            k.mm(ps_o[:], Am[:], vgb[b][:, c, :], False, True, reads=[Am, vgb[b]], writes=[ps_o])
            k.mm(ps_kv[:], ket[:], vgb[b][:, c, :], True, True, reads=[ket, vgb[b]], writes=[ps_kv])
            k.op('dve', lambda E: E.tensor_tensor(S[:], ps_kv[:], S[:], ALU.add), reads=[ps_kv, S], writes=[S])
            k.op('dve', lambda E: E.tensor_scalar_mul(S[:], S[:], E_[:, GLA_CH - 1:GLA_CH]), reads=[S, E_], writes=[S])
            k.op('act', lambda E: E.copy(Sb[:], S[:]), reads=[S], writes=[Sb])
            k.op('act', lambda E: E.activation(junk[:], ps_o[:], AF.Square, accum_out=ss[:]), reads=[ps_o], writes=[junk, ss])
            k.op('act', lambda E: E.activation(rstd[:], ss[:], AF.Ln, bias=GLA_EPS, scale=1.0 / GLA_DV), reads=[ss], writes=[rstd])
            k.op('act', lambda E: E.activation(rstd[:], rstd[:], AF.Exp, scale=-0.5), reads=[rstd], writes=[rstd])
            k.op('dve', lambda E: E.scalar_tensor_tensor(og[b][:, c, :], ps_o[:], rstd[:], rg[b][:, c, :], ALU.mult, ALU.mult),
                 reads=[ps_o, rstd, rg[b]], writes=[og[b]], waw=False)
        k.dma('pool', o_d[tsl, :].rearrange("(c p) d -> p c d", p=128), og[b][:], reads=[og[b]], writes=[o_d])
    if own:
        return k.finish([o_d])
    return o_d


def host_inputs_gla(layer, inputs, projT_cores):
    i = layer // 2
    P = np.asarray(projT_cores)
    full = [np.concatenate([P[b * 4 + j] for j in range(4)], axis=1) for b in range(2)]
    U = np.triu(np.ones((128, 128), np.float32))
    g = np.ascontiguousarray(np.asarray(inputs["gla_out_norm"][i]).reshape(1, GLA_DV))
    maps = []
    for core in range(8):
        b, h = core // 4, core % 4
        F = full[b]
        qT = F[h * 128:(h + 1) * 128]
        kT = F[512 + h * 128:512 + (h + 1) * 128]
        vT = F[1024 + h * 256:1024 + (h + 1) * 256]
        rT = F[2048 + h * 256:2048 + (h + 1) * 256]
        laT = F[3088 + h * 128:3088 + (h + 1) * 128]
        maps.append(dict(qT=np.ascontiguousarray(qT), kT=np.ascontiguousarray(kT), k_tok=np.ascontiguousarray(kT.T),
                         la_tok=np.ascontiguousarray(laT.T), v_tok=np.ascontiguousarray(vT.T), r_tok=np.ascontiguousarray(rT.T),
                         gnorm=g, U=U))
    return maps


GDN_S_LEN = 8192
GDN_CH = 128
GDN_NCH = GDN_S_LEN // GDN_CH
GDN_GRP = 8
GDN_DK = 128
GDN_DV = 128
GDN_EPS = 1e-6


class Stop(Exception):
    pass


def build_gdn(k, nchunks=GDN_NCH, pfx="", stage=99):
    raw_d = [k.dram_in(pfx + n, [128, 3 + GDN_S_LEN]) for n in ("qraw", "kraw", "vraw")]
    cw_d = k.dram_in(pfx + "cw", [128, 12])
    z_d = k.dram_in(pfx + "z_tok", [GDN_S_LEN, GDN_DV])
    ba_d = k.dram_in(pfx + "ba_tok", [128, GDN_NCH])
    aa_d = k.dram_in(pfx + "aa_tok", [128, GDN_NCH])
    al_d = k.dram_in(pfx + "alog", [128, 1])
    dtb_d = k.dram_in(pfx + "dtb", [128, 1])
    g_d = k.dram_in(pfx + "gnorm", [1, GDN_DV])
    U_d = k.dram_in(pfx + "U", [128, 128])
    Us_d = k.dram_in(pfx + "Us", [128, 128])
    id_d = k.dram_in(pfx + "ident", [128, 128])
    o_d = k.dram_out(pfx + "oa_tok", [GDN_S_LEN, GDN_DV])

    def ld(name, src, shape):
        t = k.sb(name, shape)
        k.dma('sp', t[:], src, reads=[], writes=[t])
        return t
    U = ld("U", U_d[:], [128, 128])
    Us = ld("Us", Us_d[:], [128, 128])
    ident = ld("ident", id_d[:], [128, 128])
    cw = ld("cw", cw_d[:], [128, 12])
    gb = ld("gb", g_d[0:1, :].partition_broadcast(128), [128, GDN_DV])
    ba = ld("ba", ba_d[:], [128, GDN_NCH])
    aa = ld("aa", aa_d[:], [128, GDN_NCH])
    al = ld("al", al_d[:], [128, 1])
    dtb = ld("dtb", dtb_d[:], [128, 1])
    ones = k.sb("ones", [128, 128])
    k.op('pool', lambda E: E.memset(ones[:], 1.0), writes=[ones])

    beta = k.sb("beta", [128, GDN_NCH])
    gt = k.sb("gt", [128, GDN_NCH])
    t1 = k.sb("t1", [128, GDN_NCH])
    k.op('act', lambda E: E.activation(beta[:], ba[:], AF.Sigmoid), reads=[ba], writes=[beta])
    k.op('act', lambda E: E.activation(gt[:], aa[:], AF.Identity, bias=dtb[:], scale=1.0), reads=[aa, dtb], writes=[gt])
    k.op('act', lambda E: E.activation(t1[:], gt[:], AF.Abs), reads=[gt], writes=[t1])
    k.op('act', lambda E: E.activation(t1[:], t1[:], AF.Exp, scale=-1.0), reads=[t1], writes=[t1])
    k.op('act', lambda E: E.activation(t1[:], t1[:], AF.Ln, bias=1.0, scale=1.0), reads=[t1], writes=[t1])
    k.op('dve', lambda E: E.scalar_tensor_tensor(gt[:], gt[:], 0.0, t1[:], ALU.max, ALU.add), reads=[gt, t1], writes=[gt])
    k.op('act', lambda E: E.activation(al[:], al[:], AF.Exp), reads=[al], writes=[al])
    k.op('dve', lambda E: E.tensor_scalar_mul(al[:], al[:], -1.0), reads=[al], writes=[al])
    k.op('dve', lambda E: E.tensor_scalar_mul(gt[:], gt[:], al[:]), reads=[gt, al], writes=[gt])
    ps_misc = k.ps("misc", [128, 512])
    gccol = k.sb("gccol", [128, GDN_NCH])
    k.mm(ps_misc[:, 0:GDN_NCH], U[:], gt[:], True, True, reads=[U, gt], writes=[ps_misc])
    k.op('dve', lambda E: E.tensor_copy(gccol[:], ps_misc[:, 0:GDN_NCH]), reads=[ps_misc], writes=[gccol])

    if stage == 1:
        raise Stop
    S = k.sb("S", [128, GDN_DV])
    Sb = k.sb("Sb", [128, GDN_DV], BF16)
    k.op('dve', lambda E: E.memset(S[:], 0.0), writes=[S])
    k.op('dve', lambda E: E.memset(Sb[:], 0.0), writes=[Sb])

    GW = GDN_GRP * GDN_CH
    rawg = [[k.sb("raw%d_%d" % (j, i), [128, 3 + GW]) for i in range(2)] for j in range(3)]
    cv = [k.sb("cv%d" % j, [128, GW]) for j in range(3)]
    sqb = k.sb("sqb", [128, 512])
    rin = k.sb("rin", [128, 512])
    zg = [k.sb("zg%d" % i, [128, GDN_GRP, GDN_DV]) for i in range(2)]
    og = [k.sb("og%d" % i, [128, GDN_GRP, GDN_DV]) for i in range(2)]
    ps_ss = k.ps("ss", [128, 512])
    ps_a = k.ps("pa", [128, 128])
    ps_b = k.ps("pb", [128, 128])
    ps_c = k.ps("pc", [128, 128])
    ps_o = k.ps("po", [128, GDN_DV])
    ps_s = k.ps("pss", [128, GDN_DV])

    ktok = k.sb("ktok", [128, GDN_DK])
    vtok = k.sb("vtok", [128, GDN_DV])
    gbc = k.sb("gbc", [128, 128])
    grow = k.sb("grow", [128, 128])
    Erow = k.sb("Erow", [128, 128])
    arg = k.sb("arg", [128, 128])
    DT = k.sb("DT", [128, 128])
    DTs = k.sb("DTs", [128, 128])
    NT = k.sb("NT", [128, 128])
    Nn = k.sb("Nn", [128, 128])
    P = [k.sb("P%d" % i, [128, 128]) for i in range(2)]
    PT = [k.sb("PT%d" % i, [128, 128]) for i in range(2)]
    Y = k.sb("Y", [128, 128])
    kgT = k.sb("kgT", [128, 128], BF16)
    qgT = k.sb("qgT", [128, 128], BF16)
    kd = k.sb("kd", [128, 128], BF16)
    Aq = k.sb("Aq", [128, 128], BF16)
    rhsm = k.sb("rhsm", [128, GDN_DV])
    vnew = k.sb("vnew", [128, GDN_DV], BF16)
    ed = k.sb("ed", [128, 1])
    egl = k.sb("egl", [128, 1])
    ngc = k.sb("ngc", [128, 1])
    junk = k.sb("junk", [128, GDN_DV])
    ss = k.sb("ss1", [128, 1])
    rstd = k.sb("rstd1", [128, 1])

    def two(t, name, shape, dt=F32):
        return [t, k.sb(name + "_2", shape, dt)]
    ktok2, vtok2 = two(ktok, "ktok", [128, GDN_DK]), two(vtok, "vtok", [128, GDN_DV])
    gbc2, grow2, Erow2, arg2 = two(gbc, "gbc", [128, 128]), two(grow, "grow", [128, 128]), two(Erow, "Erow", [128, 128]), two(arg, "arg", [128, 128])
    DT2, DTs2, NT2, Nn2, Y2 = two(DT, "DT", [128, 128]), two(DTs, "DTs", [128, 128]), two(NT, "NT", [128, 128]), two(Nn, "Nn", [128, 128]), two(Y, "Y", [128, 128])
    P2 = [P, [k.sb("P%d_2" % i, [128, 128]) for i in range(2)]]
    PT2 = [PT, [k.sb("PT%d_2" % i, [128, 128]) for i in range(2)]]
    kgT2, qgT2 = two(kgT, "kgT", [128, 128], BF16), two(qgT, "qgT", [128, 128], BF16)
    kd2, Aq2 = two(kd, "kd", [128, 128], BF16), two(Aq, "Aq", [128, 128], BF16)
    ed2, egl2, ngc2 = two(ed, "ed", [128, 1]), two(egl, "egl", [128, 1]), two(ngc, "ngc", [128, 1])
    qn, kn, vn = cv

    def group_pre(gi):
        b = gi % 2
        t0 = gi * GW
        for j in range(3):
            k.dma('sp' if j != 1 else 'act', rawg[j][b][:], raw_d[j][:, t0:t0 + 3 + GW], reads=[raw_d[j]], writes=[rawg[j][b]])
        k.dma('act', zg[b][:], z_d[t0:t0 + GW, :].rearrange("(c p) d -> p c d", p=128), reads=[z_d], writes=[zg[b]])
        k.op('act', lambda E: E.activation(zg[b][:], zg[b][:], AF.Silu), reads=[zg[b]], writes=[zg[b]])
        for c in range(GDN_GRP):
            k.op('pool', lambda E: E.tensor_tensor(zg[b][:, c, :], zg[b][:, c, :], gb[:], ALU.mult), reads=[zg[b], gb], writes=[zg[b]])
        for j in range(3):
            r = rawg[j][b]
            k.op('dve', lambda E: E.tensor_scalar_mul(cv[j][:], r[:, 0:GW], cw[:, 4 * j:4 * j + 1]), reads=[r, cw], writes=[cv[j]])
            for w in range(1, 4):
                k.op('dve', lambda E: E.scalar_tensor_tensor(cv[j][:], r[:, w:w + GW], cw[:, 4 * j + w:4 * j + w + 1], cv[j][:], ALU.mult, ALU.add),
                     reads=[r, cw, cv[j]], writes=[cv[j]])
            k.op('act', lambda E: E.activation(cv[j][:], cv[j][:], AF.Silu), reads=[cv[j]], writes=[cv[j]])
        for j in range(2):
            for h2 in range(GW // 512):
                sl = slice(h2 * 512, (h2 + 1) * 512)
                k.op('act', lambda E: E.activation(sqb[:], cv[j][:, sl], AF.Square), reads=[cv[j]], writes=[sqb])
                k.mm(ps_ss[:], ones[:], sqb[:], True, True, reads=[ones, sqb], writes=[ps_ss])
                k.op('act', lambda E: E.activation(rin[:], ps_ss[:], AF.Ln, bias=GDN_EPS, scale=1.0), reads=[ps_ss], writes=[rin])
                k.op('act', lambda E: E.activation(rin[:], rin[:], AF.Exp, scale=-0.5), reads=[rin], writes=[rin])
                k.op('dve', lambda E: E.tensor_tensor(cv[j][:, sl], cv[j][:, sl], rin[:], ALU.mult), reads=[cv[j], rin], writes=[cv[j]])

    def prep(ci):
        x = ci % 2
        c = ci % GDN_GRP
        csl = slice(c * GDN_CH, (c + 1) * GDN_CH)
        ktok, vtok, gbc, grow, Erow, arg = ktok2[x], vtok2[x], gbc2[x], grow2[x], Erow2[x], arg2[x]
        DT, DTs, NT, Nn, Y, P, PT = DT2[x], DTs2[x], NT2[x], Nn2[x], Y2[x], P2[x], PT2[x]
        kgT, qgT, kd, Aq, ed, egl, ngc = kgT2[x], qgT2[x], kd2[x], Aq2[x], ed2[x], egl2[x], ngc2[x]
        k.mm(ps_a[:], kn[:, csl], ident[:], True, True, reads=[kn, ident], writes=[ps_a])
        k.op('dve', lambda E: E.tensor_copy(ktok[:], ps_a[:]), reads=[ps_a], writes=[ktok])
        k.mm(ps_b[:], vn[:, csl], ident[:], True, True, reads=[vn, ident], writes=[ps_b])
        k.op('dve', lambda E: E.tensor_copy(vtok[:], ps_b[:]), reads=[ps_b], writes=[vtok])
        yield
        k.op('dve', lambda E: E.tensor_scalar_mul(gbc[:], ones[:], gt[:, ci:ci + 1]), reads=[gt, ones], writes=[gbc])
        k.mm(ps_c[:], gbc[:], U[:], True, True, reads=[gbc, U], writes=[ps_c])
        k.op('dve', lambda E: E.tensor_copy(grow[:], ps_c[:]), reads=[ps_c], writes=[grow])
        k.op('act', lambda E: E.activation(Erow[:], grow[:], AF.Exp), reads=[grow], writes=[Erow])
        yield
        k.op('dve', lambda E: E.tensor_scalar_sub(arg[:], grow[:], gccol[:, ci:ci + 1]), reads=[grow, gccol], writes=[arg])
        k.op('dve', lambda E: E.tensor_scalar_min(arg[:], arg[:], 0.0), reads=[arg], writes=[arg])
        k.op('act', lambda E: E.activation(arg[:], arg[:], AF.Exp), reads=[arg], writes=[arg])
        k.op('dve', lambda E: E.tensor_tensor(DT[:], arg[:], U[:], ALU.mult), reads=[arg, U], writes=[DT])
        k.op('pool', lambda E: E.tensor_tensor(DTs[:], arg[:], Us[:], ALU.mult), reads=[arg, Us], writes=[DTs])
        yield
        k.op('act', lambda E: E.activation(egl[:], grow[:, GDN_CH - 1:GDN_CH], AF.Exp), reads=[grow], writes=[egl])
        k.op('dve', lambda E: E.tensor_tensor(ngc[:], grow[:, GDN_CH - 1:GDN_CH], gccol[:, ci:ci + 1], ALU.subtract), reads=[grow, gccol], writes=[ngc])
        k.op('act', lambda E: E.activation(ed[:], ngc[:], AF.Exp), reads=[ngc], writes=[ed])
        k.mm(ps_a[:], kn[:, csl], kn[:, csl], True, True, reads=[kn], writes=[ps_a])
        k.op('dve', lambda E: E.scalar_tensor_tensor(NT[:], ps_a[:], beta[:, ci:ci + 1], DTs[:], ALU.mult, ALU.mult),
             reads=[ps_a, beta, DTs], writes=[NT])
        yield
        k.mm(ps_b[:], NT[:], ident[:], True, True, reads=[NT, ident], writes=[ps_b])
        k.op('dve', lambda E: E.tensor_copy(Nn[:], ps_b[:]), reads=[ps_b], writes=[Nn])
        k.op('dve', lambda E: E.tensor_tensor(Y[:], ident[:], NT[:], ALU.subtract), reads=[ident, NT], writes=[Y])
        yield
        k.mm(ps_a[:], NT[:], Nn[:], True, True, reads=[NT, Nn], writes=[ps_a])
        k.op('dve', lambda E: E.tensor_copy(P[0][:], ps_a[:]), reads=[ps_a], writes=[P[0]])
        k.mm(ps_b[:], Nn[:], NT[:], True, True, reads=[NT, Nn], writes=[ps_b])
        k.op('dve', lambda E: E.tensor_copy(PT[0][:], ps_b[:]), reads=[ps_b], writes=[PT[0]])
        yield
        for lv in range(6):
            a_, n_ = lv % 2, (lv + 1) % 2
            k.mm(ps_c[:], P[a_][:], Y[:], True, True, reads=[P[a_], Y], writes=[ps_c])
            k.op('dve', lambda E: E.tensor_tensor(Y[:], Y[:], ps_c[:], ALU.add), reads=[Y, ps_c], writes=[Y])
            if lv < 5:
                k.mm(ps_a[:], PT[a_][:], P[a_][:], True, True, reads=[PT[a_], P[a_]], writes=[ps_a])
                k.mm(ps_b[:], P[a_][:], PT[a_][:], True, True, reads=[PT[a_], P[a_]], writes=[ps_b])
                k.op('dve', lambda E: E.tensor_copy(P[n_][:], ps_a[:]), reads=[ps_a], writes=[P[n_]])
                k.op('dve', lambda E: E.tensor_copy(PT[n_][:], ps_b[:]), reads=[ps_b], writes=[PT[n_]])
            yield
        k.op('dve', lambda E: E.tensor_tensor(kgT[:], kn[:, csl], Erow[:], ALU.mult), reads=[kn, Erow], writes=[kgT])
        k.op('dve', lambda E: E.scalar_tensor_tensor(qgT[:], qn[:, csl], float(GDN_DK) ** -0.5, Erow[:], ALU.mult, ALU.mult),
             reads=[qn, Erow], writes=[qgT])
        k.op('dve', lambda E: E.tensor_scalar_mul(kd[:], ktok[:], ed[:]), reads=[ktok, ed], writes=[kd])
        yield
        k.mm(ps_a[:], kn[:, csl], qn[:, csl], True, True, reads=[kn, qn], writes=[ps_a])
        k.op('dve', lambda E: E.scalar_tensor_tensor(Aq[:], ps_a[:], float(GDN_DK) ** -0.5, DT[:], ALU.mult, ALU.mult),
             reads=[ps_a, DT], writes=[Aq])
        yield

    def seq(ci):
        x = ci % 2
        c = ci % GDN_GRP
        b = (ci // GDN_GRP) % 2
        vtok, Y, kgT, qgT, kd, Aq, egl = vtok2[x], Y2[x], kgT2[x], qgT2[x], kd2[x], Aq2[x], egl2[x]
        k.mm(ps_s[:], kgT[:], Sb[:], True, True, reads=[kgT, Sb], writes=[ps_s])
        k.op('dve', lambda E: E.tensor_tensor(rhsm[:], vtok[:], ps_s[:], ALU.subtract), reads=[vtok, ps_s], writes=[rhsm])
        yield
        k.mm(ps_s[:], Y[:], rhsm[:], True, True, reads=[Y, rhsm], writes=[ps_s])
        k.op('dve', lambda E: E.tensor_scalar_mul(vnew[:], ps_s[:], beta[:, ci:ci + 1]), reads=[ps_s, beta], writes=[vnew])
        yield
        k.mm(ps_o[:], qgT[:], Sb[:], True, False, reads=[qgT, Sb], writes=[ps_o])
        k.mm(ps_o[:], Aq[:], vnew[:], False, True, reads=[Aq, vnew], writes=[ps_o])
        k.mm(ps_s[:], kd[:], vnew[:], True, True, reads=[kd, vnew], writes=[ps_s])
        k.op('dve', lambda E: E.scalar_tensor_tensor(S[:], S[:], egl[:], ps_s[:], ALU.mult, ALU.add), reads=[S, egl, ps_s], writes=[S])
        k.op('act', lambda E: E.copy(Sb[:], S[:]), reads=[S], writes=[Sb])
        yield
        k.op('act', lambda E: E.activation(junk[:], ps_o[:], AF.Square, accum_out=ss[:]), reads=[ps_o], writes=[junk, ss])
        k.op('act', lambda E: E.activation(rstd[:], ss[:], AF.Ln, bias=GDN_EPS, scale=1.0 / GDN_DV), reads=[ss], writes=[rstd])
        k.op('act', lambda E: E.activation(rstd[:], rstd[:], AF.Exp, scale=-0.5), reads=[rstd], writes=[rstd])
        yield
        k.op('dve', lambda E: E.scalar_tensor_tensor(og[b][:, c, :], ps_o[:], rstd[:], zg[b][:, c, :], ALU.mult, ALU.mult),
             reads=[ps_o, rstd, zg[b]], writes=[og[b]], waw=False)
        yield

    def drain(g):
        for _ in g:
            pass

    def corun(g_seq, g_prep):
        alive_s = alive_p = True
        while alive_s or alive_p:
            if alive_s:
                try:
                    next(g_seq)
                except StopIteration:
                    alive_s = False
            for _ in range(3):
                if alive_p:
                    try:
                        next(g_prep)
                    except StopIteration:
                        alive_p = False

    group_pre(0)
    drain(prep(0))
    for ci in range(nchunks):
        nxt = ci + 1
        if nxt < nchunks:
            if nxt % GDN_GRP == 0:
                group_pre(nxt // GDN_GRP)
            corun(seq(ci), prep(nxt))
        else:
            drain(seq(ci))
        if ci % GDN_GRP == GDN_GRP - 1 or ci == nchunks - 1:
            gi = ci // GDN_GRP
            t0 = gi * GW
            k.dma('pool', o_d[t0:t0 + GW, :].rearrange("(c p) d -> p c d", p=128), og[gi % 2][:], reads=[og[gi % 2]], writes=[o_d])
    return o_d


def host_inputs_gdn(layer, inputs, full, core):
    i = layer // 2
    b, h = core // 4, core % 4
    F = full[b]

    def raw(row0):
        a = np.zeros((128, 3 + GDN_S_LEN), np.float32)
        a[:, 3:] = F[row0 + h * 128:row0 + (h + 1) * 128]
        return a
    cwf = np.asarray(inputs["gdn_conv_w"][i])
    cw = np.concatenate([cwf[:, j * 512 + h * 128:j * 512 + (h + 1) * 128].T for j in range(3)], axis=1)
    m = dict(qraw=raw(0), kraw=raw(512), vraw=raw(1024),
             cw=np.ascontiguousarray(cw, dtype=np.float32),
             z_tok=np.ascontiguousarray(F[1536 + h * 128:1536 + (h + 1) * 128].T),
             ba_tok=np.ascontiguousarray(F[3904 + h].reshape(GDN_NCH, 128).T),
             aa_tok=np.ascontiguousarray(F[3908 + h].reshape(GDN_NCH, 128).T),
             alog=np.full((128, 1), np.asarray(inputs["gdn_a_log"])[i, h], np.float32),
             dtb=np.full((128, 1), np.asarray(inputs["gdn_dt_bias"])[i, h], np.float32),
             gnorm=np.ascontiguousarray(np.asarray(inputs["gdn_out_norm"][i]).reshape(1, GDN_DV)),
             U=np.triu(np.ones((128, 128), np.float32)), Us=np.triu(np.ones((128, 128), np.float32), 1),
             ident=np.eye(128, dtype=np.float32))
    return m


DSA_S_LEN = 8192
DSA_QT = 128
DSA_KB = 512
DSA_TOPK = 256
DSA_DH = 128
DSA_NH = 4
DSA_NEG = -30000.0
DSA_NBIS = 30
DSA_NSLOT = 16


def build_dsa_v1(k, nslots=DSA_NSLOT, slot0=0, pfx=""):
    qb_d = k.dram_in(pfx + "qbT", [DSA_NSLOT, 128, DSA_NH, DSA_QT], BF16)
    qi_d = k.dram_in(pfx + "qiT", [DSA_NSLOT, 64, DSA_NH, DSA_QT])
    wi_d = k.dram_in(pfx + "wi", [DSA_NSLOT, DSA_QT, DSA_NH])
    kb_d = k.dram_in(pfx + "kbT", [DSA_NH, 128, DSA_S_LEN], BF16)
    vb_d = k.dram_in(pfx + "vb", [DSA_NH, 128, DSA_S_LEN // 128, DSA_DH], BF16)
    ki_d = k.dram_in(pfx + "kiT", [64, DSA_S_LEN])
    cb_d = k.dram_in(pfx + "cb", [128, DSA_KB])
    idb_d = k.dram_in(pfx + "identb", [128, 128], BF16)
    p2_d = k.dram_in(pfx + "pow2", [128, DSA_NBIS])
    o_d = k.dram_out(pfx + "ob", [DSA_NSLOT, DSA_QT, DSA_NH * DSA_DH])

    kiT = k.sb("kiT", [64, DSA_S_LEN])
    cb = k.sb("cb", [128, DSA_KB])
    idb = k.sb("idb", [128, 128], BF16)
    k.dma('sp', kiT[:, 0:4096], ki_d[:, 0:4096], writes=[kiT])
    k.dma('act', kiT[:, 4096:], ki_d[:, 4096:], writes=[kiT])
    k.dma('sp', cb[:], cb_d[:], writes=[cb])
    k.dma('sp', idb[:], idb_d[:], writes=[idb])
    pow2 = k.sb("pow2", [128, DSA_NBIS])
    dtab = k.sb("dtab", [128, DSA_NBIS])
    k.dma('sp', pow2[:], p2_d[:], writes=[pow2])

    score = k.sb("score", [128, DSA_S_LEN])
    selb = k.sb("selb", [128, DSA_S_LEN], BF16)
    L = k.sb("L", [128, DSA_S_LEN])
    Pm = k.sb("Pm", [128, DSA_S_LEN], BF16)
    KhT = k.sb("KhT", [128, DSA_S_LEN], BF16)
    Vh = k.sb("Vh", [128, DSA_S_LEN // 128, DSA_DH], BF16)
    qb = k.sb("qb", [128, DSA_NH, DSA_QT], BF16)
    qi = k.sb("qi", [64, DSA_NH, DSA_QT])
    wi = k.sb("wi", [128, DSA_NH])
    relu_t = [k.sb("relu%d" % i, [128, DSA_KB]) for i in range(2)]
    PT = [k.sb("PT%d" % i, [128, 128], BF16) for i in range(2)]
    osb = k.sb("osb", [128, DSA_NH * DSA_DH])
    ps_big = [k.ps("big%d" % i, [128, DSA_KB]) for i in range(3)]
    ps_T = [k.ps("psT%d" % i, [128, 128], BF16) for i in range(2)]
    ps_o = k.ps("pso", [128, DSA_DH])
    sc = {n: k.sb("sc_" + n, [128, 1]) for n in ("lo", "hi", "d", "nmid", "acc", "ge", "gh", "t", "mx", "nmx", "rs", "rinv")}
    nbig = [0]

    def big():
        p = ps_big[nbig[0] % 3]
        nbig[0] += 1
        return p

    for n in range(slot0, slot0 + nslots):
        N = DSA_KB * (n + 1)
        NB = n + 1
        k.dma('sp', qb[:], qb_d[n], reads=[], writes=[qb])
        k.dma('act', qi[:], qi_d[n], reads=[], writes=[qi])
        k.dma('sp', wi[:], wi_d[n], reads=[], writes=[wi])
        k.op('dve', lambda E: E.tensor_scalar_mul(wi[:], wi[:], float(256.0 ** -0.5)), reads=[wi], writes=[wi])
        for kb_ in range(NB):
            ksl = slice(kb_ * DSA_KB, (kb_ + 1) * DSA_KB)
            for h in range(DSA_NH):
                ps = big()
                k.mm(ps[:], qi[:, h, :], kiT[:, ksl], True, True, reads=[qi, kiT], writes=[ps])
                rt = relu_t[h % 2]
                k.op('act', lambda E: E.activation(rt[:], ps[:], AF.Relu), reads=[ps], writes=[rt])
                if h == 0:
                    k.op('dve', lambda E: E.tensor_scalar_mul(score[:, ksl], rt[:], wi[:, 0:1]), reads=[rt, wi], writes=[score], waw=False)
                else:
                    k.op('dve', lambda E: E.scalar_tensor_tensor(score[:, ksl], rt[:], wi[:, h:h + 1], score[:, ksl], ALU.mult, ALU.add),
                         reads=[rt, wi, score], writes=[score])
        lo, hi, d, nmid, acc, ge, gh, t_, mx, nmx, rs, rinv = [sc[x] for x in ("lo", "hi", "d", "nmid", "acc", "ge", "gh", "t", "mx", "nmx", "rs", "rinv")]
        k.op('dve', lambda E: E.tensor_reduce(hi[:], score[:, 0:N], AX.X, ALU.max), reads=[score], writes=[hi])
        k.op('dve', lambda E: E.tensor_reduce(lo[:], score[:, 0:N], AX.X, ALU.min), reads=[score], writes=[lo])
        k.op('dve', lambda E: E.tensor_scalar_add(hi[:], hi[:], 1.0), reads=[hi], writes=[hi])
        k.op('dve', lambda E: E.tensor_scalar_add(lo[:], lo[:], -1.0), reads=[lo], writes=[lo])
        k.op('dve', lambda E: E.tensor_tensor(score[:, N - DSA_KB:N], score[:, N - DSA_KB:N], cb[:], ALU.add), reads=[score, cb], writes=[score])
        k.op('dve', lambda E: E.tensor_tensor(d[:], hi[:], lo[:], ALU.subtract), reads=[hi, lo], writes=[d])
        k.op('dve', lambda E: E.tensor_scalar_mul(dtab[:], pow2[:], d[:]), reads=[pow2, d], writes=[dtab])
        for it in range(DSA_NBIS):
            k.op('dve', lambda E: E.scalar_tensor_tensor(nmid[:], lo[:], -1.0, dtab[:, it:it + 1], ALU.mult, ALU.subtract), reads=[lo, dtab], writes=[nmid])
            k.op('act', lambda E: E.activation(L[:, 0:N], score[:, 0:N], AF.Sign, bias=nmid[:], scale=1.0, accum_out=acc[:]),
                 reads=[score, nmid], writes=[L, acc])
            k.op('dve', lambda E: E.tensor_scalar(ge[:], acc[:], float(2 * DSA_TOPK - N), 1.0, ALU.is_ge, ALU.mult), reads=[acc], writes=[ge])
            k.op('dve', lambda E: E.scalar_tensor_tensor(lo[:], ge[:], dtab[:, it:it + 1], lo[:], ALU.mult, ALU.add), reads=[ge, dtab, lo], writes=[lo])
        k.op('dve', lambda E: E.tensor_scalar_sub(L[:, 0:N], score[:, 0:N], lo[:]), reads=[score, lo], writes=[L])
        k.op('dve', lambda E: E.tensor_scalar(L[:, 0:N], L[:, 0:N], 0.0, 1.0, ALU.is_ge, ALU.mult), reads=[L], writes=[L])
        k.op('dve', lambda E: E.tensor_scalar(selb[:, 0:N], L[:, 0:N], -1.0, -DSA_NEG, ALU.add, ALU.mult), reads=[L], writes=[selb])
        for h in range(DSA_NH):
            k.dma('sp', KhT[:, 0:N], kb_d[h, :, 0:N], reads=[], writes=[KhT])
            k.dma('act', Vh[:, 0:4 * NB, :], vb_d[h, :, 0:4 * NB, :], reads=[], writes=[Vh])
            for kb_ in range(NB):
                ksl = slice(kb_ * DSA_KB, (kb_ + 1) * DSA_KB)
                ps = big()
                k.mm(ps[:], qb[:, h, :], KhT[:, ksl], True, True, reads=[qb, KhT], writes=[ps])
                k.op('dve', lambda E: E.scalar_tensor_tensor(L[:, ksl], ps[:], float(DSA_DH) ** -0.5, selb[:, ksl], ALU.mult, ALU.add),
                     reads=[ps, selb], writes=[L], waw=False)
            k.op('dve', lambda E: E.tensor_reduce(mx[:], L[:, 0:N], AX.X, ALU.max), reads=[L], writes=[mx])
            k.op('dve', lambda E: E.tensor_scalar_mul(nmx[:], mx[:], -1.0), reads=[mx], writes=[nmx])
            k.op('act', lambda E: E.activation(Pm[:, 0:N], L[:, 0:N], AF.Exp, bias=nmx[:], scale=1.0, accum_out=rs[:]),
                 reads=[L, nmx], writes=[Pm, rs])
            nkt = 4 * NB
            for kt in range(nkt):
                pt = ps_T[kt % 2]
                sbt = PT[kt % 2]
                k.tr(pt[:], Pm[:, kt * 128:(kt + 1) * 128], idb, reads=[Pm], writes=[pt])
                if kt % 2 == 0:
                    k.op('act', lambda E: E.copy(sbt[:], pt[:]), reads=[pt], writes=[sbt])
                else:
                    k.op('dve', lambda E: E.tensor_copy(sbt[:], pt[:]), reads=[pt], writes=[sbt])
                k.mm(ps_o[:], sbt[:], Vh[:, kt, :], kt == 0, kt == nkt - 1, reads=[sbt, Vh], writes=[ps_o])
            k.op('dve', lambda E: E.reciprocal(rinv[:], rs[:]), reads=[rs], writes=[rinv])
            k.op('dve', lambda E: E.tensor_scalar_mul(osb[:, h * DSA_DH:(h + 1) * DSA_DH], ps_o[:], rinv[:]), reads=[ps_o, rinv], writes=[osb])
        k.dma('pool', o_d[n], osb[:], reads=[osb], writes=[o_d])
    return o_d


def build_dsa(k, nslots=DSA_NSLOT, slot0=0, pfx=""):
    qb_d = k.dram_in(pfx + "qbT", [DSA_NSLOT, 128, DSA_NH, DSA_QT], BF16)
    qi_d = k.dram_in(pfx + "qiT", [DSA_NSLOT, 64, DSA_NH, DSA_QT])
    wi_d = k.dram_in(pfx + "wi", [DSA_NSLOT, DSA_QT, DSA_NH])
    kb_d = k.dram_in(pfx + "kbT", [DSA_NH, 128, DSA_S_LEN], BF16)
    vb_d = k.dram_in(pfx + "vb", [DSA_NH, 128, DSA_S_LEN // 128, DSA_DH], BF16)
    ki_d = k.dram_in(pfx + "kiT", [64, DSA_S_LEN])
    cb_d = k.dram_in(pfx + "cb", [128, DSA_KB])
    idb_d = k.dram_in(pfx + "identb", [128, 128], BF16)
    p2_d = k.dram_in(pfx + "pow2", [128, DSA_NBIS])
    o_d = k.dram_out(pfx + "ob", [DSA_NSLOT, DSA_QT, DSA_NH * DSA_DH])

    kiT = k.sb("kiT", [64, DSA_S_LEN])
    cb = k.sb("cb", [128, DSA_KB])
    idb = k.sb("idb", [128, 128], BF16)
    pow2 = k.sb("pow2", [128, DSA_NBIS])
    dtab = k.sb("dtab", [128, DSA_NBIS])
    k.dma('sp', kiT[:, 0:4096], ki_d[:, 0:4096], writes=[kiT])
    k.dma('act', kiT[:, 4096:], ki_d[:, 4096:], writes=[kiT])
    k.dma('sp', cb[:], cb_d[:], writes=[cb])
    k.dma('sp', idb[:], idb_d[:], writes=[idb])
    k.dma('sp', pow2[:], p2_d[:], writes=[pow2])

    score = k.sb("score", [128, DSA_S_LEN])
    junk = k.sb("junk", [128, DSA_S_LEN], BF16)
    selbs = [k.sb("selb%d" % i, [128, DSA_S_LEN], BF16) for i in range(2)]
    L = k.sb("L", [128, DSA_S_LEN])
    Pm = k.sb("Pm", [128, DSA_S_LEN], BF16)
    KhT = k.sb("KhT", [128, DSA_S_LEN], BF16)
    Vh = k.sb("Vh", [128, DSA_S_LEN // 128, DSA_DH], BF16)
    qbs = [k.sb("qb%d" % i, [128, DSA_NH, DSA_QT], BF16) for i in range(2)]
    qi = k.sb("qi", [64, DSA_NH, DSA_QT])
    wi = k.sb("wi", [128, DSA_NH])
    relu_t = [k.sb("relu%d" % i, [128, DSA_KB]) for i in range(2)]
    PT = [k.sb("PT%d" % i, [128, 128], BF16) for i in range(2)]
    osb = k.sb("osb", [128, DSA_NH * DSA_DH])
    ps_ix = [k.ps("ix%d" % i, [128, DSA_KB]) for i in range(2)]
    ps_lg = [k.ps("lg%d" % i, [128, DSA_KB]) for i in range(2)]
    ps_T = [k.ps("psT%d" % i, [128, 128], BF16) for i in range(2)]
    ps_o = k.ps("pso", [128, DSA_DH])
    sc = {n: k.sb("sc_" + n, [128, 1]) for n in ("lo", "hi", "d", "nmid", "acc", "ge", "mx", "nmx", "rs", "rinv")}
    cnt = dict(ix=0, lg=0)

    def phase1(n):
        N = DSA_KB * (n + 1)
        NB = n + 1
        qb = qbs[n % 2]
        selb = selbs[n % 2]
        lo, hi, d, nmid, acc, ge = [sc[x] for x in ("lo", "hi", "d", "nmid", "acc", "ge")]
        k.dma('sp', qb[:], qb_d[n], reads=[], writes=[qb])
        k.dma('sp', qi[:], qi_d[n], reads=[], writes=[qi])
        k.dma('sp', wi[:], wi_d[n], reads=[], writes=[wi])
        k.op('dve', lambda E: E.tensor_scalar_mul(wi[:], wi[:], float(256.0 ** -0.5)), reads=[wi], writes=[wi])
        for kb_ in range(NB):
            ksl = slice(kb_ * DSA_KB, (kb_ + 1) * DSA_KB)
            for h in range(DSA_NH):
                ps = ps_ix[cnt['ix'] % 2]
                cnt['ix'] += 1
                k.mm(ps[:], qi[:, h, :], kiT[:, ksl], True, True, reads=[qi, kiT], writes=[ps])
                rt = relu_t[h % 2]
                k.op('act', lambda E: E.activation(rt[:], ps[:], AF.Relu), reads=[ps], writes=[rt])
                if h == 0:
                    k.op('dve', lambda E: E.tensor_scalar_mul(score[:, ksl], rt[:], wi[:, 0:1]), reads=[rt, wi], writes=[score], waw=False)
                else:
                    k.op('dve', lambda E: E.scalar_tensor_tensor(score[:, ksl], rt[:], wi[:, h:h + 1], score[:, ksl], ALU.mult, ALU.add),
                         reads=[rt, wi, score], writes=[score])
            yield True
        k.op('dve', lambda E: E.tensor_reduce(hi[:], score[:, 0:N], AX.X, ALU.max), reads=[score], writes=[hi])
        k.op('dve', lambda E: E.tensor_reduce(lo[:], score[:, 0:N], AX.X, ALU.min), reads=[score], writes=[lo])
        k.op('dve', lambda E: E.tensor_scalar_add(hi[:], hi[:], 1.0), reads=[hi], writes=[hi])
        k.op('dve', lambda E: E.tensor_scalar_add(lo[:], lo[:], -1.0), reads=[lo], writes=[lo])
        k.op('dve', lambda E: E.tensor_tensor(score[:, N - DSA_KB:N], score[:, N - DSA_KB:N], cb[:], ALU.add), reads=[score, cb], writes=[score])
        k.op('dve', lambda E: E.tensor_tensor(d[:], hi[:], lo[:], ALU.subtract), reads=[hi, lo], writes=[d])
        k.op('dve', lambda E: E.tensor_scalar_mul(dtab[:], pow2[:], d[:]), reads=[pow2, d], writes=[dtab])
        yield True
        for it in range(DSA_NBIS):
            k.op('dve', lambda E: E.scalar_tensor_tensor(nmid[:], lo[:], -1.0, dtab[:, it:it + 1], ALU.mult, ALU.subtract), reads=[lo, dtab], writes=[nmid])
            k.op('act', lambda E: E.activation(junk[:, 0:N], score[:, 0:N], AF.Sign, bias=nmid[:], scale=1.0, accum_out=acc[:]),
                 reads=[score, nmid], writes=[junk, acc])
            yield True
            k.op('dve', lambda E: E.tensor_scalar(ge[:], acc[:], float(2 * DSA_TOPK - N), 1.0, ALU.is_ge, ALU.mult), reads=[acc], writes=[ge])
            k.op('dve', lambda E: E.scalar_tensor_tensor(lo[:], ge[:], dtab[:, it:it + 1], lo[:], ALU.mult, ALU.add), reads=[ge, dtab, lo], writes=[lo])
        k.op('dve', lambda E: E.tensor_scalar_sub(score[:, 0:N], score[:, 0:N], lo[:]), reads=[score, lo], writes=[score])
        yield True
        k.op('dve', lambda E: E.tensor_scalar(score[:, 0:N], score[:, 0:N], 0.0, 1.0, ALU.is_ge, ALU.mult), reads=[score], writes=[score])
        yield True
        k.op('dve', lambda E: E.tensor_scalar(selb[:, 0:N], score[:, 0:N], -1.0, -DSA_NEG, ALU.add, ALU.mult), reads=[score], writes=[selb])
        yield True

    def phase2(n):
        N = DSA_KB * (n + 1)
        NB = n + 1
        qb = qbs[n % 2]
        selb = selbs[n % 2]
        mx, nmx, rs, rinv = [sc[x] for x in ("mx", "nmx", "rs", "rinv")]
        for h in range(DSA_NH):
            k.dma('sp', KhT[:, 0:N], kb_d[h, :, 0:N], reads=[], writes=[KhT])
            k.dma('sp', Vh[:, 0:4 * NB, :], vb_d[h, :, 0:4 * NB, :], reads=[], writes=[Vh])
            for kb_ in range(NB):
                ksl = slice(kb_ * DSA_KB, (kb_ + 1) * DSA_KB)
                ps = ps_lg[cnt['lg'] % 2]
                cnt['lg'] += 1
                k.mm(ps[:], qb[:, h, :], KhT[:, ksl], True, True, reads=[qb, KhT], writes=[ps])
                k.op('dve', lambda E: E.scalar_tensor_tensor(L[:, ksl], ps[:], float(DSA_DH) ** -0.5, selb[:, ksl], ALU.mult, ALU.add),
                     reads=[ps, selb], writes=[L], waw=False)
                if kb_ % 2 == 1:
                    yield
            k.op('dve', lambda E: E.tensor_reduce(mx[:], L[:, 0:N], AX.X, ALU.max), reads=[L], writes=[mx])
            k.op('dve', lambda E: E.tensor_scalar_mul(nmx[:], mx[:], -1.0), reads=[mx], writes=[nmx])
            k.op('act', lambda E: E.activation(Pm[:, 0:N], L[:, 0:N], AF.Exp, bias=nmx[:], scale=1.0, accum_out=rs[:]),
                 reads=[L, nmx], writes=[Pm, rs])
            yield
            nkt = 4 * NB
            for kt in range(nkt):
                pt = ps_T[kt % 2]
                sbt = PT[kt % 2]
                k.tr(pt[:], Pm[:, kt * 128:(kt + 1) * 128], idb, reads=[Pm], writes=[pt])
                k.op('dve', lambda E: E.tensor_copy(sbt[:], pt[:]), reads=[pt], writes=[sbt])
                k.mm(ps_o[:], sbt[:], Vh[:, kt, :], kt == 0, kt == nkt - 1, reads=[sbt, Vh], writes=[ps_o])
                if kt % 4 == 3:
                    yield
            k.op('dve', lambda E: E.reciprocal(rinv[:], rs[:]), reads=[rs], writes=[rinv])
            k.op('dve', lambda E: E.tensor_scalar_mul(osb[:, h * DSA_DH:(h + 1) * DSA_DH], ps_o[:], rinv[:]), reads=[ps_o, rinv], writes=[osb])
            yield
        k.dma('pool', o_d[n], osb[:], reads=[osb], writes=[o_d])
        yield

    def drain(g):
        for _ in g:
            pass

    def corun(g2, g1, n2, w1):
        done2 = 0
        wi_ = 0
        alive2 = True
        for _ in g1:
            wi_ += 1
            target = -(-n2 * wi_ // max(w1, 1))
            while alive2 and done2 < target:
                try:
                    next(g2)
                    done2 += 1
                except StopIteration:
                    alive2 = False
        if alive2:
            drain(g2)

    last = slot0 + nslots - 1
    drain(phase1(slot0))
    for n in range(slot0, slot0 + nslots):
        if n < last:
            NB2 = n + 1
            n2 = 4 * (NB2 // 2 + NB2 + 2) + 1
            w1 = (n + 2) + 1 + DSA_NBIS + 3
            corun(phase2(n), phase1(n + 1), n2, w1)
        else:
            drain(phase2(n))
    return o_d


def host_inputs_dsa(full, fullbf, core):
    b, r = core // 4, core % 4
    F, Fb = full[b], fullbf[b]
    tiles = [4 * n + r for n in range(DSA_NSLOT)]
    qbf = Fb[0:512].reshape(DSA_NH, 128, DSA_S_LEN)
    qif = F[3584:3840].reshape(DSA_NH, 64, DSA_S_LEN)
    qbT = np.stack([qbf[:, :, i * 128:(i + 1) * 128].transpose(1, 0, 2) for i in tiles])
    qiT = np.stack([qif[:, :, i * 128:(i + 1) * 128].transpose(1, 0, 2) for i in tiles])
    wi = np.stack([F[3912:3916, i * 128:(i + 1) * 128].T for i in tiles])
    sp = np.arange(DSA_KB)[None, :]
    tq = np.arange(128)[:, None]
    cb = np.where(sp <= 128 * r + tq, 0.0, -1e9).astype(np.float32)
    return dict(qbT=np.ascontiguousarray(qbT), qiT=np.ascontiguousarray(qiT, dtype=np.float32), wi=np.ascontiguousarray(wi, dtype=np.float32),
                kbT=np.ascontiguousarray(Fb[512:1024].reshape(DSA_NH, 128, DSA_S_LEN)),
                vb=np.ascontiguousarray(Fb[1024:1536].reshape(DSA_NH, 128, DSA_S_LEN // 128, 128).transpose(0, 3, 2, 1)),
                kiT=np.ascontiguousarray(F[3840:3904], dtype=np.float32), cb=cb,
                identb=np.eye(128, dtype=np.float32).astype(ml_dtypes.bfloat16),
                pow2=np.tile((0.5 ** np.arange(1, DSA_NBIS + 1)).astype(np.float32)[None, :], (128, 1)))


C_T = 2048
C_NT_ = C_T // 128
C_D = 1024
C_KC = 8
C_DE = 512
C_NE = 32
C_EPS = 1e-6


def ge_ap(k, out, in_ap, sc_ap, reads, writes):
    k.op('dve', lambda E: E.tensor_scalar_sub(out, in_ap, sc_ap), reads=reads, writes=writes)
    k.op('dve', lambda E: E.tensor_scalar(out, out, 0.0, 1.0, ALU.is_ge, ALU.mult), reads=writes, writes=writes)


def build_C(final=False):
    k = K()
    x_d = k.dram_in("x_tok", [C_T, C_D])
    oT_d = k.dram_in("oT", [C_D, C_T])
    wout_d = k.dram_in("w_out", [C_D, C_D])
    condT_d = k.dram_in("condT", [128, C_KC])
    adaw_d = k.dram_in("ada_w", [C_D, 4 * C_D])
    adab_d = k.dram_in("ada_b", [1, 4 * C_D])
    g_d = k.dram_in("norm_g", [1, C_D])
    wr_d = k.dram_in("w_router", [C_D, 36])
    br_d = k.dram_in("b_router", [1, 36])
    w1_d = k.dram_in("w1", [C_NE, C_D, C_DE])
    w3_d = k.dram_in("w3", [C_NE, C_D, C_DE])
    w2_d = k.dram_in("w2", [C_NE, C_DE, C_D])
    id_d = k.dram_in("ident", [128, 128])
    fin_d = k.dram_in("final_g", [1, C_D])
    out_d = k.dram_out("x_out", [C_T, C_D])

    ident = k.sb("ident", [128, 128])
    identb = k.sb("identb", [128, 128], BF16)
    ones1 = k.sb("ones1", [1, 128])
    k.dma('sp', ident[:], id_d[:], writes=[ident])
    k.op('dve', lambda E: E.tensor_copy(identb[:], ident[:]), reads=[ident], writes=[identb])
    k.op('pool', lambda E: E.memset(ones1[:], 1.0), writes=[ones1])

    cond = k.sb("cond", [128, C_KC])
    k.dma('sp', cond[:], condT_d[:], writes=[cond])
    k.op('act', lambda E: E.activation(cond[:], cond[:], AF.Silu), reads=[cond], writes=[cond])
    wst = [k.sb("wst%d" % i, [128, C_KC, 512]) for i in range(2)]
    psA = [k.ps("psA%d" % i, [128, 512]) for i in range(4)]
    npa = [0]

    def pa():
        p = psA[npa[0] % 4]
        npa[0] += 1
        return p
    modbc = k.sb("modbc", [128, 4 * C_D])
    k.dma('sp', modbc[0:1, :], adab_d[:], writes=[modbc])
    for j in range(8):
        b = j % 2
        k.dma('sp' if b == 0 else 'act', wst[b][:], adaw_d[:, j * 512:(j + 1) * 512].rearrange("(c p) n -> p c n", p=128), writes=[wst[b]])
        ps = pa()
        for c in range(C_KC):
            k.mm(ps[0:1, :], cond[:, c:c + 1], wst[b][:, c, :], c == 0, c == C_KC - 1, reads=[cond, wst[b]], writes=[ps])
        k.op('dve', lambda E: E.tensor_tensor(modbc[0:1, j * 512:(j + 1) * 512], ps[0:1, :], modbc[0:1, j * 512:(j + 1) * 512], ALU.add),
             reads=[ps, modbc], writes=[modbc])
    for j in range(8):
        ps = pa()
        k.mm(ps[:], ones1[:], modbc[0:1, j * 512:(j + 1) * 512], True, True, reads=[ones1, modbc], writes=[ps])
        k.op('act', lambda E: E.copy(modbc[:, j * 512:(j + 1) * 512], ps[:]), reads=[ps], writes=[modbc])
    gbc = k.sb("gbc", [128, C_D])
    k.dma('sp', gbc[:], g_d[0:1, :].partition_broadcast(128), writes=[gbc])
    k.op('dve', lambda E: E.scalar_tensor_tensor(modbc[:, 2 * C_D:3 * C_D], modbc[:, 2 * C_D:3 * C_D], 1.0, gbc[:], ALU.add, ALU.mult),
         reads=[modbc, gbc], writes=[modbc])
    if final:
        k.dma('sp', gbc[:], fin_d[0:1, :].partition_broadcast(128), reads=[modbc], writes=[gbc])

    x1 = k.sb("x1", [128, C_NT_, C_D])
    k.dma('sp', x1[:, 0:8, :], x_d[0:1024, :].rearrange("(c p) d -> p c d", p=128), writes=[x1])
    k.dma('act', x1[:, 8:16, :], x_d[1024:2048, :].rearrange("(c p) d -> p c d", p=128), writes=[x1])
    wo_b = k.sb("wo_b", [128, C_KC, C_D], BF16)
    for hh in range(2):
        k.dma('sp', wst[hh][:], wout_d[:, hh * 512:(hh + 1) * 512].rearrange("(c p) n -> p c n", p=128), writes=[wst[hh]])
        k.op('pool', lambda E: E.tensor_copy(wo_b[:, :, hh * 512:(hh + 1) * 512], wst[hh][:]), reads=[wst[hh]], writes=[wo_b])
    w1bs = [k.sb("w1b%d" % i, [128, C_KC, C_DE], BF16) for i in range(2)]
    oTb = w1bs[0]
    mtmp = k.sb("mtmp", [128, 512])
    for g in range(4):
        b = g % 2
        k.dma('sp', wst[b][:], oT_d[:, g * 512:(g + 1) * 512].rearrange("(c p) n -> p c n", p=128), reads=[wo_b], writes=[wst[b]])
        k.op('pool', lambda E: E.tensor_copy(oTb[:], wst[b][:]), reads=[wst[b]], writes=[oTb])
        for tt in range(4):
            ti = g * 4 + tt
            for dh in range(2):
                ps = pa()
                for c in range(C_KC):
                    k.mm(ps[:], oTb[:, c, tt * 128:(tt + 1) * 128], wo_b[:, c, dh * 512:(dh + 1) * 512], c == 0, c == C_KC - 1,
                         reads=[oTb, wo_b], writes=[ps])
                k.op('dve', lambda E: E.tensor_tensor(mtmp[:], ps[:], modbc[:, dh * 512:(dh + 1) * 512], ALU.mult), reads=[ps, modbc], writes=[mtmp])
                k.op('pool', lambda E: E.tensor_tensor(x1[:, ti, dh * 512:(dh + 1) * 512], x1[:, ti, dh * 512:(dh + 1) * 512], mtmp[:], ALU.add),
                     reads=[mtmp, x1], writes=[x1])

    h2T = k.sb("h2T", [128, C_KC, C_T], BF16)
    hscr = k.sb("hscr", [128, 2 * C_D])
    h2f = Buf(hscr[:, 0:C_D], "h2f")
    h2Tf = Buf(hscr[:, C_D:2 * C_D].rearrange("p (c n) -> p c n", c=C_KC), "h2Tf")
    junk = h2f
    wr = k.sb("wr", [128, C_KC, 36])
    brb = k.sb("brb", [128, 36])
    k.dma('sp', wr[:], wr_d[:].rearrange("(c p) n -> p c n", p=128), writes=[wr])
    k.dma('sp', brb[:], br_d[0:1, :].partition_broadcast(128), writes=[brb])
    W32 = k.sb("W32", [128, C_NT_, C_NE])
    lg = k.sb("lg", [128, 36])
    s1 = {n: k.sb("r_" + n, [128, 1]) for n in ("ss", "rstd", "gmax", "gs", "gate", "m1", "m2", "p1", "p2", "t")}
    ohg = k.sb("ohg", [128, 4])
    eg = k.sb("eg", [128, 4])
    el = k.sb("el", [128, 8])
    el2 = k.sb("el2", [128, 8])
    oh1 = k.sb("oh1", [128, 8])
    oh2 = k.sb("oh2", [128, 8])
    we8 = k.sb("we8", [128, 8])
    ps_r = k.ps("ps_r", [128, 36])
    ps_ts = [k.ps("ps_t%d" % i, [128, 128]) for i in range(2)]
    ps_t = ps_ts[0]
    for ti in range(C_NT_):
        ss, rstd = s1["ss"], s1["rstd"]
        k.op('act', lambda E: E.activation(junk[:], x1[:, ti, :], AF.Square, accum_out=ss[:]), reads=[x1], writes=[junk, ss])
        k.op('act', lambda E: E.activation(rstd[:], ss[:], AF.Ln, bias=C_EPS, scale=1.0 / C_D), reads=[ss], writes=[rstd])
        k.op('act', lambda E: E.activation(rstd[:], rstd[:], AF.Exp, scale=-0.5), reads=[rstd], writes=[rstd])
        k.op('dve', lambda E: E.scalar_tensor_tensor(h2f[:], x1[:, ti, :], rstd[:], modbc[:, 2 * C_D:3 * C_D], ALU.mult, ALU.mult),
             reads=[x1, rstd, modbc], writes=[h2f])
        k.op('dve', lambda E: E.tensor_tensor(h2f[:], h2f[:], modbc[:, C_D:2 * C_D], ALU.add), reads=[h2f, modbc], writes=[h2f])
        for c in range(C_KC):
            pst = ps_ts[c % 2]
            k.mm(pst[:], h2f[:, c * 128:(c + 1) * 128], ident[:], True, True, reads=[h2f, ident], writes=[pst])
            k.op('act', lambda E: E.copy(h2Tf[:, c, :], pst[:]), reads=[pst], writes=[h2Tf], waw=False)
            k.op('pool', lambda E: E.tensor_copy(h2T[:, c, ti * 128:(ti + 1) * 128], h2Tf[:, c, :]), reads=[h2Tf], writes=[h2T], waw=False)
        for c in range(C_KC):
            k.mm(ps_r[:], h2Tf[:, c, :], wr[:, c, :], c == 0, c == C_KC - 1, reads=[h2Tf, wr], writes=[ps_r])
        k.op('dve', lambda E: E.tensor_tensor(lg[:], ps_r[:], brb[:], ALU.add), reads=[ps_r, brb], writes=[lg])
        gmax, gs, gate, m1, m2, p1, p2, t_ = [s1[n] for n in ("gmax", "gs", "gate", "m1", "m2", "p1", "p2", "t")]
        k.op('dve', lambda E: E.tensor_reduce(gmax[:], lg[:, 0:4], AX.X, ALU.max), reads=[lg], writes=[gmax])
        ge_ap(k, ohg[:], lg[:, 0:4], gmax[:], [lg, gmax], [ohg])
        k.op('dve', lambda E: E.tensor_scalar_mul(t_[:], gmax[:], -1.0), reads=[gmax], writes=[t_])
        k.op('act', lambda E: E.activation(eg[:], lg[:, 0:4], AF.Exp, bias=t_[:], scale=1.0, accum_out=gs[:]), reads=[lg, t_], writes=[eg, gs])
        k.op('dve', lambda E: E.reciprocal(gate[:], gs[:]), reads=[gs], writes=[gate])
        k.op('dve', lambda E: E.tensor_scalar_mul(el[:], lg[:, 4:12], ohg[:, 0:1]), reads=[lg, ohg], writes=[el])
        for gq in range(1, 4):
            k.op('dve', lambda E: E.scalar_tensor_tensor(el[:], lg[:, 4 + 8 * gq:12 + 8 * gq], ohg[:, gq:gq + 1], el[:], ALU.mult, ALU.add),
                 reads=[lg, ohg, el], writes=[el])
        k.op('dve', lambda E: E.tensor_reduce(m1[:], el[:], AX.X, ALU.max), reads=[el], writes=[m1])
        ge_ap(k, oh1[:], el[:], m1[:], [el, m1], [oh1])
        k.op('dve', lambda E: E.scalar_tensor_tensor(el2[:], oh1[:], -1e9, el[:], ALU.mult, ALU.add), reads=[oh1, el], writes=[el2])
        k.op('dve', lambda E: E.tensor_reduce(m2[:], el2[:], AX.X, ALU.max), reads=[el2], writes=[m2])
        ge_ap(k, oh2[:], el2[:], m2[:], [el2, m2], [oh2])
        k.op('dve', lambda E: E.tensor_tensor(t_[:], m2[:], m1[:], ALU.subtract), reads=[m1, m2], writes=[t_])
        k.op('act', lambda E: E.activation(t_[:], t_[:], AF.Exp), reads=[t_], writes=[t_])
        k.op('dve', lambda E: E.tensor_scalar_add(t_[:], t_[:], 1.0), reads=[t_], writes=[t_])
        k.op('dve', lambda E: E.reciprocal(p1[:], t_[:]), reads=[t_], writes=[p1])
        k.op('dve', lambda E: E.tensor_scalar(p2[:], p1[:], -1.0, 1.0, ALU.mult, ALU.add), reads=[p1], writes=[p2])
        k.op('dve', lambda E: E.tensor_tensor(p1[:], p1[:], gate[:], ALU.mult), reads=[p1, gate], writes=[p1])
        k.op('dve', lambda E: E.tensor_tensor(p2[:], p2[:], gate[:], ALU.mult), reads=[p2, gate], writes=[p2])
        k.op('dve', lambda E: E.tensor_scalar_mul(we8[:], oh1[:], p1[:]), reads=[oh1, p1], writes=[we8])
        k.op('dve', lambda E: E.scalar_tensor_tensor(we8[:], oh2[:], p2[:], we8[:], ALU.mult, ALU.add), reads=[oh2, p2, we8], writes=[we8])
        for gq in range(4):
            k.op('dve', lambda E: E.tensor_scalar_mul(W32[:, ti, gq * 8:(gq + 1) * 8], we8[:], ohg[:, gq:gq + 1]), reads=[we8, ohg], writes=[W32], waw=False)

    w3b1 = Buf(modbc[:, 0:2 * C_D].bitcast(BF16).rearrange("p (c n) -> p c n", c=C_KC), "w3b1")
    w3b1.w, w3b1.r = dict(modbc.w), dict(modbc.r)
    w3bs = [k.sb("w3b0", [128, C_KC, C_DE], BF16), w3b1]
    w2b = Buf(hscr[:].bitcast(BF16).rearrange("p (c n) -> p c n", c=4), "w2b")
    def _merge(*ds):
        o = {}
        for d_ in ds:
            for kk, vv in d_.items():
                if o.get(kk, 0) < vv:
                    o[kk] = vv
        return o
    w2b.w, w2b.r = _merge(h2f.w, h2Tf.w), _merge(h2f.r, h2Tf.r)
    hid = Buf(wo_b[:].rearrange("p c n -> p (c n)").rearrange("p (f t) -> p f t", f=4), "hid")
    hid.w, hid.r = wo_b.w, wo_b.r
    sil = Buf(modbc[:, 2 * C_D:2 * C_D + 512], "sil")
    sil.w, sil.r = dict(modbc.w), dict(modbc.r)
    gtf = slice(3 * C_D, 4 * C_D)
    for e in range(C_NE):
        w1b, w3b = w1bs[e % 2], w3bs[e % 2]
        k.dma('sp', wst[0][:], w1_d[e].rearrange("(c p) n -> p c n", p=128), reads=[], writes=[wst[0]])
        k.op('pool', lambda E: E.tensor_copy(w1b[:], wst[0][:]), reads=[wst[0]], writes=[w1b])
        k.dma('sp', wst[1][:], w3_d[e].rearrange("(c p) n -> p c n", p=128), reads=[], writes=[wst[1]])
        k.op('pool', lambda E: E.tensor_copy(w3b[:], wst[1][:]), reads=[wst[1]], writes=[w3b])
        for fc in range(4):
            for g in range(4):
                p1_, p3_ = pa(), pa()
                for c in range(C_KC):
                    k.mm(p1_[:], w1b[:, c, fc * 128:(fc + 1) * 128], h2T[:, c, g * 512:(g + 1) * 512], c == 0, c == C_KC - 1, reads=[w1b, h2T], writes=[p1_])
                for c in range(C_KC):
                    k.mm(p3_[:], w3b[:, c, fc * 128:(fc + 1) * 128], h2T[:, c, g * 512:(g + 1) * 512], c == 0, c == C_KC - 1, reads=[w3b, h2T], writes=[p3_])
                k.op('act', lambda E: E.activation(sil[:], p1_[:], AF.Silu), reads=[p1_], writes=[sil])
                k.op('dve', lambda E: E.tensor_tensor(hid[:, fc, g * 512:(g + 1) * 512], sil[:], p3_[:], ALU.mult), reads=[sil, p3_], writes=[hid], waw=False)
        k.dma('sp', wst[0][:].rearrange("p c n -> p (c n)").rearrange("p (c n) -> p c n", c=4), w2_d[e].rearrange("(c p) n -> p c n", p=128),
              reads=[], writes=[wst[0]])
        w2v = wst[0][:].rearrange("p c n -> p (c n)").rearrange("p (c n) -> p c n", c=4)
        for fc in range(4):
            k.op('pool', lambda E: E.tensor_tensor(w2b[:, fc, :], w2v[:, fc, :], modbc[:, gtf], ALU.mult), reads=[wst[0], modbc], writes=[w2b], waw=(fc == 0))
        for ti in range(C_NT_):
            for dh in range(2):
                ps = pa()
                for fc in range(4):
                    k.mm(ps[:], hid[:, fc, ti * 128:(ti + 1) * 128], w2b[:, fc, dh * 512:(dh + 1) * 512], fc == 0, fc == 3, reads=[hid, w2b], writes=[ps])
                k.op('dve', lambda E: E.scalar_tensor_tensor(x1[:, ti, dh * 512:(dh + 1) * 512], ps[:], W32[:, ti, e:e + 1], x1[:, ti, dh * 512:(dh + 1) * 512],
                                                              ALU.mult, ALU.add), reads=[ps, W32, x1], writes=[x1], waw=False)
    k.barrier()
    for ti in range(C_NT_):
        if final:
            ss, rstd = s1["ss"], s1["rstd"]
            k.op('act', lambda E: E.activation(junk[:], x1[:, ti, :], AF.Square, accum_out=ss[:]), reads=[x1], writes=[junk, ss])
            k.op('act', lambda E: E.activation(rstd[:], ss[:], AF.Ln, bias=C_EPS, scale=1.0 / C_D), reads=[ss], writes=[rstd])
            k.op('act', lambda E: E.activation(rstd[:], rstd[:], AF.Exp, scale=-0.5), reads=[rstd], writes=[rstd])
            k.op('dve', lambda E: E.scalar_tensor_tensor(x1[:, ti, :], x1[:, ti, :], rstd[:], gbc[:], ALU.mult, ALU.mult), reads=[x1, rstd, gbc], writes=[x1])
    k.dma('sp', out_d[0:1024, :].rearrange("(c p) d -> p c d", p=128), x1[:, 0:8, :], reads=[x1], writes=[out_d])
    k.dma('act', out_d[1024:2048, :].rearrange("(c p) d -> p c d", p=128), x1[:, 8:16, :], reads=[x1], writes=[out_d])
    return k.finish([out_d])


def host_inputs_C(layer, inputs, x_tok_cores, oT_cores, w_out):
    ada_w = np.ascontiguousarray(np.asarray(inputs["ada_w"][layer])[:, 2 * C_D:6 * C_D])
    ada_b = np.ascontiguousarray(np.asarray(inputs["ada_b"][layer])[2 * C_D:6 * C_D].reshape(1, 4 * C_D))
    c = np.asarray(inputs["c"])
    wg = np.asarray(inputs["router_group_w"][layer])
    we = np.asarray(inputs["router_exp_w"][layer])
    wr = np.ascontiguousarray(np.concatenate([wg] + [we[g] for g in range(4)], axis=1))
    br = np.ascontiguousarray(np.concatenate([np.asarray(inputs["router_group_b"][layer]).reshape(-1),
                                               np.asarray(inputs["router_exp_b"][layer]).reshape(-1)]).reshape(1, 36))
    base = dict(w_out=np.ascontiguousarray(w_out), ada_w=ada_w, ada_b=ada_b,
                norm_g=np.ascontiguousarray(np.asarray(inputs["norm_ffn"][layer]).reshape(1, C_D)),
                w_router=wr, b_router=br, w1=np.asarray(inputs["exp_w1"][layer]), w3=np.asarray(inputs["exp_w3"][layer]),
                w2=np.asarray(inputs["exp_w2"][layer]), ident=np.eye(128, dtype=np.float32),
                final_g=np.ascontiguousarray(np.asarray(inputs["final_norm"]).reshape(1, C_D)))
    maps = []
    for core in range(8):
        b = core // 4
        m = dict(base)
        m["x_tok"] = x_tok_cores[core]
        m["oT"] = oT_cores[core]
        m["condT"] = np.ascontiguousarray(c[b].reshape(C_KC, 128).T)
        maps.append(m)
    return maps


_CACHE = {}


def _prog(name, builder):
    if name not in _CACHE:
        _CACHE[name] = builder()
    return _CACHE[name]


def _build_gdn_prog():
    k = K()
    o = build_gdn(k)
    return k.finish([o])


def _build_dsa_prog():
    k = K()
    o = build_dsa(k)
    return k.finish([o])


def _run(nc, maps):
    return run_bass_kernel_spmd(nc, maps, core_ids=list(range(8))).results


def kernel(**inputs):
    inputs = {k_: np.asarray(v) for k_, v in inputs.items()}
    x = inputs["x"]
    T = 2048
    x_tok = [np.ascontiguousarray(x[c // 4, (c % 4) * T:(c % 4 + 1) * T, :]) for c in range(8)]
    for layer in range(4):
        i = layer // 2
        kind = "even" if layer % 2 == 0 else "odd"
        xT = [np.ascontiguousarray(a.T) for a in x_tok]
        resA = _run(_prog("A_" + kind, lambda: build_A(kind)), host_inputs_A(kind, layer, inputs, xT))
        P = [r["projT"] for r in resA]
        full = [np.concatenate([P[b * 4 + j] for j in range(4)], axis=1) for b in range(2)]
        if kind == "even":
            Pb = [np.asarray(r["projbf"]) for r in resA]
            fullbf = [np.concatenate([Pb[b * 4 + j] for j in range(4)], axis=1) for b in range(2)]
            resG = _run(_prog("gdn", _build_gdn_prog), [host_inputs_gdn(layer, inputs, full, c) for c in range(8)])
            resD = _run(_prog("dsa", _build_dsa_prog), [host_inputs_dsa(full, fullbf, c) for c in range(8)])
            o_full = []
            for b in range(2):
                oa = np.concatenate([resG[b * 4 + h]["oa_tok"] for h in range(4)], axis=1)
                ob = np.zeros((8192, 512), np.float32)
                for r in range(4):
                    o = resD[b * 4 + r]["ob"]
                    for n in range(16):
                        t0 = (4 * n + r) * 128
                        ob[t0:t0 + 128] = o[n]
                o_full.append(np.concatenate([oa, ob], axis=1))
            w_out = inputs["even_w_out"][i]
        else:
            resG = _run(_prog("gla", build_gla), host_inputs_gla(layer, inputs, P))
            o_full = [np.concatenate([resG[b * 4 + h]["o_tok"] for h in range(4)], axis=1) for b in range(2)]
            w_out = inputs["gla_w_out"][i]
        oT = [np.ascontiguousarray(o_full[c // 4][(c % 4) * T:(c % 4 + 1) * T, :].T) for c in range(8)]
        final = layer == 3
        resC = _run(_prog("C_%d" % final, lambda: build_C(final)), host_inputs_C(layer, inputs, x_tok, oT, w_out))
        x_tok = [r["x_out"] for r in resC]
    out = np.zeros((2, 8192, 1024), np.float32)
    for c in range(8):
        out[c // 4, (c % 4) * T:(c % 4 + 1) * T, :] = x_tok[c]
    return out
```

```python
import numpy as np
import ml_dtypes
from concourse.bass_utils import run_bass_kernel_spmd

from contextlib import ExitStack
import numpy as np
import concourse.bass as bass
import concourse.mybir as mybir

F32 = mybir.dt.float32
BF16 = mybir.dt.bfloat16
I32 = mybir.dt.int32
AF = mybir.ActivationFunctionType
ALU = mybir.AluOpType
AX = mybir.AxisListType

NDS = 24


class Buf:
    def __init__(self, t, name):
        self.t = t
        self.name = name
        self.w = {}
        self.r = {}

    def __getitem__(self, idx):
        return self.t[idx]


class View(Buf):
    pass


class K:
    def __init__(self):
        self.nc = bass.Bass("TRN2", target_bir_lowering=False)
        self.es = ExitStack()
        nc = self.nc
        self.eng = dict(pe=nc.tensor, act=nc.scalar, dve=nc.vector, pool=nc.gpsimd, sp=nc.sync)
        self.sem = {e: self.es.enter_context(nc.semaphore("s_" + e)) for e in self.eng}
        self.cnt = {e: 0 for e in self.eng}
        self.waited = {e: {} for e in self.eng}
        self.dsem = [self.es.enter_context(nc.semaphore("d%d" % i)) for i in range(NDS)]
        self.dcnt = [0] * NDS
        self.dnext = 0
        self.dnext_sw = 0
        self.nwaits = 0
        self.nins = 0
        self.out_toks = {}

    def sb(self, name, shape, dt=F32):
        esz = 2 if dt == BF16 else 4
        n = 1
        for d in shape[1:]:
            n *= d
        if (n * esz) % 64 != 0:
            npad = ((n * esz + 63) // 64) * 64 // esz
            t = self.es.enter_context(self.nc.sbuf_tensor("sb_" + name, [shape[0], npad], dt))
            v = t[:, 0:n]
            if len(shape) == 3:
                v = v.rearrange("p (a b) -> p a b", a=shape[1])
            return Buf(v, name)
        t = self.es.enter_context(self.nc.sbuf_tensor("sb_" + name, list(shape), dt))
        return Buf(t, name)

    def ps(self, name, shape, dt=F32):
        t = self.es.enter_context(self.nc.psum_tensor("ps_" + name, list(shape), dt))
        return Buf(t, name)

    def dram_in(self, name, shape, dt=F32):
        return Buf(self.nc.dram_tensor(name, list(shape), dt, kind="ExternalInput"), name)

    def dram_out(self, name, shape, dt=F32):
        return Buf(self.nc.dram_tensor(name, list(shape), dt, kind="ExternalOutput"), name)

    def dram_tmp(self, name, shape, dt=F32):
        return Buf(self.nc.dram_tensor(name, list(shape), dt), name)

    def _semof(self, key):
        if key[0] == 'c':
            return self.sem[key[1]], 1
        return self.dsem[key[1]], 16

    def _wait(self, e, deps):
        w = self.waited[e]
        for key, val in deps.items():
            if w.get(key, 0) >= val:
                continue
            s, mult = self._semof(key)
            self.eng[e].wait_ge(s, val * mult)
            w[key] = val
            self.nwaits += 1

    def _deps(self, e, reads, writes, waw=True, self_sync=True):
        deps = {}
        def add(d):
            for k, v in d.items():
                if not self_sync and k == ('c', e):
                    continue
                if deps.get(k, 0) < v:
                    deps[k] = v
        for b in reads:
            add(b.w)
        for b in writes:
            add(b.r)
            if waw:
                add(b.w)
        return deps

    def _mark(self, key, val, reads, writes):
        for b in reads:
            if b.r.get(key, 0) < val:
                b.r[key] = val
        for b in writes:
            if b.w.get(key, 0) < val:
                b.w[key] = val

    def op(self, e, emit, reads=(), writes=(), waw=True, self_sync=True):
        self._wait(e, self._deps(e, reads, writes, waw, self_sync))
        ins = emit(self.eng[e])
        self.cnt[e] += 1
        ins.then_inc(self.sem[e], 1)
        self.nins += 1
        self._mark(('c', e), self.cnt[e], reads, writes)
        return ins

    def dma(self, q, out_ap, in_ap, reads=(), writes=(), **kw):
        if q == 'pool':
            i = 16 + self.dnext_sw
            self.dnext_sw = (self.dnext_sw + 1) % (NDS - 16)
        else:
            i = self.dnext
            self.dnext = (self.dnext + 1) % 16
        deps = self._deps(q, reads, writes)
        if self.dcnt[i] > 0:
            k = ('d', i)
            if deps.get(k, 0) < self.dcnt[i]:
                deps[k] = self.dcnt[i]
        self._wait(q, deps)
        ins = self.eng[q].dma_start(out=out_ap, in_=in_ap, **kw)
        self.dcnt[i] += 1
        ins.then_inc(self.dsem[i], 16)
        self.nins += 1
        self._mark(('d', i), self.dcnt[i], reads, writes)
        return ins

    def barrier(self, bufs=()):
        deps = {('c', e): c for e, c in self.cnt.items() if c > 0}
        for i in range(NDS):
            if self.dcnt[i] > 0:
                deps[('d', i)] = self.dcnt[i]
        for e in self.eng:
            d = {k: v for k, v in deps.items() if k != ('c', e)}
            self._wait(e, d)

    def finish(self, out_bufs):
        deps = {}
        for b in out_bufs:
            for k, v in b.w.items():
                if deps.get(k, 0) < v:
                    deps[k] = v
        self._wait('sp', deps)
        d2 = {('c', e): c for e, c in self.cnt.items() if c > 0 and e != 'sp'}
        self._wait('sp', d2)
        self.es.close()
        return self.nc

    def mm(self, out, lhsT, rhs, start, stop, reads, writes):
        return self.op('pe', lambda E: E.matmul(out, lhsT, rhs, start=start, stop=stop),
                       reads=reads, writes=writes, waw=False, self_sync=False)

    def tr(self, out, in_, ident, reads, writes):
        return self.op('pe', lambda E: E.transpose(out, in_, ident[:]),
                       reads=list(reads) + [ident], writes=writes, waw=False, self_sync=False)


A_T = 2048
A_TG = 512
A_NG = A_T // A_TG
A_D = 1024
A_KC = A_D // 128
A_EPS = 1e-6
A_PI = float(np.pi)


def norm_modulate(k, xT_d, xg, hT, ones, gmod, shift, sq, tmp, ps_ss, rstd):
    for g in range(A_NG):
        tsl = slice(g * A_TG, (g + 1) * A_TG)
        xT = xg[g % 2]
        k.dma('sp' if g % 2 == 0 else 'act', xT[:], xT_d[:, tsl].rearrange("(c p) n -> p c n", p=128), reads=[xT_d], writes=[xT])
        for c in range(A_KC):
            k.op('act', lambda E: E.activation(sq[:], xT[:, c, :], AF.Square), reads=[xT], writes=[sq])
            k.mm(ps_ss[:], ones[:], sq[:], c == 0, c == A_KC - 1, reads=[ones, sq], writes=[ps_ss])
        k.op('act', lambda E: E.activation(rstd[:], ps_ss[:], AF.Ln, bias=A_EPS, scale=1.0 / A_D), reads=[ps_ss], writes=[rstd])
        k.op('act', lambda E: E.activation(rstd[:], rstd[:], AF.Exp, scale=-0.5), reads=[rstd], writes=[rstd])
        for c in range(A_KC):
            k.op('dve', lambda E: E.scalar_tensor_tensor(tmp[:], xT[:, c, :], gmod[:, c:c + 1], rstd[:], ALU.mult, ALU.mult),
                 reads=[xT, gmod, rstd], writes=[tmp])
            k.op('act', lambda E: E.activation(hT[:, c, tsl], tmp[:], AF.Identity, bias=shift[:, c:c + 1], scale=1.0),
                 reads=[tmp, shift], writes=[hT], waw=False)


def adaln_vectors(k, condT_d, ada_w_d, ada_b_d, ncol_chunks, cond, wst, ps_mod, modv):
    k.dma('sp', cond[:], condT_d[:], reads=[condT_d], writes=[cond])
    k.op('act', lambda E: E.activation(cond[:], cond[:], AF.Silu), reads=[cond], writes=[cond])
    k.dma('sp', modv[:], ada_b_d[:], reads=[ada_b_d], writes=[modv])
    for j in range(ncol_chunks):
        k.dma('sp' if j % 2 == 0 else 'act', wst[j % 2][:],
              ada_w_d[:, j * 128:(j + 1) * 128].rearrange("(c p) n -> p c n", p=128),
              reads=[ada_w_d], writes=[wst[j % 2]])
        for c in range(A_KC):
            k.mm(ps_mod[:, j:j + 1], wst[j % 2][:, c, :], cond[:, c:c + 1], c == 0, c == A_KC - 1,
                 reads=[wst[j % 2], cond], writes=[ps_mod])
    k.op('dve', lambda E: E.tensor_tensor(modv[:], modv[:], ps_mod[:, 0:ncol_chunks], ALU.add), reads=[modv, ps_mod], writes=[modv])


def build_A(kind):
    k = K()
    if kind == 'even':
        NCW = 3840 + 1280 + 64 + 64 + 12
        NOUT = 3916
    else:
        NCW = 3088
        NOUT = 3088 + 512
    xT_d = k.dram_in("xT", [A_D, A_T])
    condT_d = k.dram_in("condT", [128, A_KC])
    adaw_d = k.dram_in("ada_w", [A_D, 2 * A_D])
    adab_d = k.dram_in("ada_b", [128, 2 * A_KC])
    g_d = k.dram_in("norm_g", [128, A_KC])
    w_d = k.dram_in("w_ext", [A_D, NCW])
    out_d = k.dram_out("projT", [NOUT, A_T])
    outbf_d = k.dram_out("projbf", [1536, A_T], BF16) if kind == 'even' else None
    if kind == 'even':
        pos_d = k.dram_in("pos", [1, A_T], I32)
        inv_d = k.dram_in("inv", [128, 2])
        sgn_d = k.dram_in("sgn", [128, 2])
    else:
        wg2_d = k.dram_in("wg2", [16, 512])
        bg2_d = k.dram_in("bg2", [128, 4])

    xg = [k.sb("xg%d" % i, [128, A_KC, A_TG]) for i in range(2)]
    hT = k.sb("hT_s", [128, A_KC, A_T], BF16)
    ones = k.sb("ones", [128, 128])
    cond = k.sb("cond", [128, A_KC])
    modv = k.sb("modv", [128, 2 * A_KC])
    gmod = k.sb("gmod", [128, A_KC])
    gvec = k.sb("gvec", [128, A_KC])
    wst = [k.sb("wst%d" % i, [128, A_KC, 128]) for i in range(2)]
    sq = k.sb("sq", [128, A_TG])
    tmp = k.sb("tmp", [128, A_TG])
    rstd = k.sb("rstd", [128, A_TG])
    ps_ss = k.ps("ps_ss", [128, A_TG])
    ps_mod = k.ps("ps_mod", [128, 512])

    k.op('pool', lambda E: E.memset(ones[:], 1.0), writes=[ones])
    k.dma('pool', gvec[:], g_d[:], reads=[g_d], writes=[gvec])
    adaln_vectors(k, condT_d, adaw_d, adab_d, 2 * A_KC, cond, wst, ps_mod, modv)
    k.op('dve', lambda E: E.scalar_tensor_tensor(gmod[:], modv[:, A_KC:2 * A_KC], 1.0, gvec[:], ALU.add, ALU.mult),
         reads=[modv, gvec], writes=[gmod])
    norm_modulate(k, xT_d, xg, hT, ones, gmod, modv, sq, tmp, ps_ss, rstd)

    WG = 512
    wf = [k.sb("wf%d" % i, [128, A_KC, WG]) for i in range(2)]
    wb = [k.sb("wb%d" % i, [128, A_KC, WG], BF16) for i in range(2)]
    pacc = [k.ps("pacc%d" % i, [128, A_TG]) for i in range(4)]
    ost = [k.sb("ost%d" % i, [128, A_T]) for i in range(2)]
    ngroups = (NCW + WG - 1) // WG
    state = dict(pi=0, oi=0, gi=0)

    def load_group(gi):
        c0 = gi * WG
        n = min(WG, NCW - c0)
        b = gi % 2
        k.dma('sp', wf[b][:, 0:4, 0:n], w_d[0:512, c0:c0 + n].rearrange("(c p) n -> p c n", p=128), reads=[w_d], writes=[wf[b]])
        k.dma('act', wf[b][:, 4:8, 0:n], w_d[512:1024, c0:c0 + n].rearrange("(c p) n -> p c n", p=128), reads=[w_d], writes=[wf[b]])
        k.op('pool', lambda E: E.tensor_copy(wb[b][:, :, 0:n], wf[b][:, :, 0:n]), reads=[wf[b]], writes=[wb[b]])

    def proj_chunk(col0, m, ps, g):
        gi = col0 // WG
        b = gi % 2
        lc = col0 - gi * WG
        for c in range(A_KC):
            k.mm(ps[0:m, :], wb[b][:, c, lc:lc + m], hT[:, c, g * A_TG:(g + 1) * A_TG], c == 0, c == A_KC - 1,
                 reads=[wb[b], hT], writes=[ps])

    obf = k.sb("obf", [128, A_T], BF16)

    def store_rows(o, row0, m):
        k.dma('pool', out_d[row0:row0 + m, :], o[0:m, :], reads=[o], writes=[out_d])
        if kind == 'even' and 2048 <= row0 < 3584:
            k.op('pool', lambda E: E.tensor_copy(obf[:], o[:]), reads=[o], writes=[obf])
            k.dma('pool', outbf_d[row0 - 2048:row0 - 2048 + m, :], obf[0:m, :], reads=[obf], writes=[outbf_d])

    def nxt_ps():
        p = pacc[state['pi'] % 4]
        state['pi'] += 1
        return p

    def nxt_o():
        o = ost[state['oi'] % 2]
        state['oi'] += 1
        return o

    slot = {}

    def need(gi):
        if slot.get(gi % 2) != gi:
            load_group(gi)
            slot[gi % 2] = gi

    def plain(col0, m, row0):
        need(col0 // WG)
        o = nxt_o()
        for g in range(A_NG):
            ps = nxt_ps()
            proj_chunk(col0, m, ps, g)
            eng = 'dve' if g % 2 == 0 else 'act'
            if eng == 'dve':
                k.op('dve', lambda E: E.tensor_copy(o[0:m, g * A_TG:(g + 1) * A_TG], ps[0:m, :]), reads=[ps], writes=[o], waw=False)
            else:
                k.op('act', lambda E: E.copy(o[0:m, g * A_TG:(g + 1) * A_TG], ps[0:m, :]), reads=[ps], writes=[o], waw=False)
        store_rows(o, row0, m)
        return o

    if kind == 'even':
        posi = k.sb("posi", [128, A_T], I32)
        posf = k.sb("posf", [128, A_T])
        inv = k.sb("inv", [128, 2])
        sgn = k.sb("sgn", [128, 2])
        tabs = {}
        k.dma('sp', posi[:], pos_d[0:1, :].partition_broadcast(128), reads=[pos_d], writes=[posi])
        k.dma('sp', inv[:], inv_d[:], reads=[inv_d], writes=[inv])
        k.dma('sp', sgn[:], sgn_d[:], reads=[sgn_d], writes=[sgn])
        k.op('dve', lambda E: E.tensor_copy(posf[:], posi[:]), reads=[posi], writes=[posf])
        ang = ost[0]
        yred = ost[1]
        for nm, col in (('a', 0), ('i', 1)):
            C = k.sb("C" + nm, [128, A_T])
            S = k.sb("S" + nm, [128, A_T])
            def sin_of(dst, phase):
                k.op('dve', lambda E: E.tensor_scalar_mul(ang[:], posf[:], inv[:, col:col + 1]), reads=[posf, inv], writes=[ang])
                if phase != 0.0:
                    k.op('dve', lambda E: E.tensor_scalar_add(ang[:], ang[:], phase), reads=[ang], writes=[ang])
                k.op('dve', lambda E: E.tensor_scalar_mul(yred[:], ang[:], 1.0 / (2 * A_PI)), reads=[ang], writes=[yred])
                k.op('dve', lambda E: E.tensor_copy(posi[:], yred[:]), reads=[yred], writes=[posi])
                k.op('dve', lambda E: E.tensor_copy(yred[:], posi[:]), reads=[posi], writes=[yred])
                k.op('dve', lambda E: E.scalar_tensor_tensor(ang[:], yred[:], -2 * A_PI, ang[:], ALU.mult, ALU.add), reads=[yred, ang], writes=[ang])
                k.op('dve', lambda E: E.tensor_scalar(yred[:], ang[:], A_PI, -2 * A_PI, ALU.is_gt, ALU.mult), reads=[ang], writes=[yred])
                k.op('dve', lambda E: E.tensor_tensor(ang[:], ang[:], yred[:], ALU.add), reads=[ang, yred], writes=[ang])
                k.op('dve', lambda E: E.tensor_scalar(ang[:], ang[:], -3.1415925, 3.1415925, ALU.max, ALU.min), reads=[ang], writes=[ang])
                k.op('act', lambda E: E.activation(dst[:], ang[:], AF.Sin), reads=[ang], writes=[dst])
            sin_of(S, 0.0)
            sin_of(C, 0.5 * A_PI)
            k.op('dve', lambda E: E.tensor_scalar_mul(S[:], S[:], sgn[:, col:col + 1]), reads=[S, sgn], writes=[S])
            tabs[nm] = (C, S)

        def roped(col0, col0_sw, m, row0, tab):
            C, S = tabs[tab]
            need(col0 // WG)
            need(col0_sw // WG)
            o = nxt_o()
            for g in range(A_NG):
                tsl = slice(g * A_TG, (g + 1) * A_TG)
                ps = nxt_ps()
                ps2 = nxt_ps()
                proj_chunk(col0, m, ps, g)
                proj_chunk(col0_sw, m, ps2, g)
                k.op('dve', lambda E: E.tensor_tensor(o[0:m, tsl], ps[0:m, :], C[0:m, tsl], ALU.mult), reads=[ps, C], writes=[o], waw=False)
                k.op('dve', lambda E: E.tensor_tensor(tmp[0:m, :], ps2[0:m, :], S[0:m, tsl], ALU.mult), reads=[ps2, S], writes=[tmp])
                k.op('pool', lambda E: E.tensor_tensor(o[0:m, tsl], o[0:m, tsl], tmp[0:m, :], ALU.add), reads=[tmp, o], writes=[o], waw=False)
            store_rows(o, row0, m)

        for j in range(16):
            plain(j * 128, 128, j * 128)
        for j in range(4):
            roped(4 * 512 + j * 128, 5 * 512 + j * 128, 128, 2048 + j * 128, 'a')
        for j in range(4):
            roped(6 * 512 + j * 128, 7 * 512 + j * 128, 128, 2560 + j * 128, 'a')
        for j in range(4):
            plain(8 * 512 + j * 128, 128, 3072 + j * 128)
        for j in range(2):
            roped(9 * 512 + j * 128, 9 * 512 + 256 + j * 128, 128, 3584 + j * 128, 'i')
        roped(10 * 512, 10 * 512 + 64, 64, 3840, 'i')
        plain(10 * 512 + 128, 12, 3904)
    else:
        for j in range(24):
            plain(j * 128, 128, j * 128)
        o_glr0 = plain(3072, 16, 3072)
        o_glr = k.sb("glr_s", [16, A_T])
        k.op('pool', lambda E: E.tensor_copy(o_glr[:], o_glr0[0:16, :]), reads=[o_glr0], writes=[o_glr])
        wg2 = k.sb("wg2", [16, 512])
        bg2 = k.sb("bg2", [128, 4])
        k.dma('sp', wg2[:], wg2_d[:], reads=[wg2_d], writes=[wg2])
        k.dma('sp', bg2[:], bg2_d[:], reads=[bg2_d], writes=[bg2])
        t2 = k.sb("t2", [128, A_TG])
        for j in range(4):
            o = nxt_o()
            for g in range(A_NG):
                tsl = slice(g * A_TG, (g + 1) * A_TG)
                ps = nxt_ps()
                k.mm(ps[:], wg2[:, j * 128:(j + 1) * 128], o_glr[0:16, tsl], True, True, reads=[wg2, o_glr], writes=[ps])
                k.op('act', lambda E: E.activation(tmp[:], ps[:], AF.Identity, bias=bg2[:, j:j + 1], scale=1.0), reads=[ps, bg2], writes=[tmp])
                k.op('act', lambda E: E.activation(t2[:], tmp[:], AF.Abs), reads=[tmp], writes=[t2])
                k.op('act', lambda E: E.activation(t2[:], t2[:], AF.Exp, scale=-1.0), reads=[t2], writes=[t2])
                k.op('act', lambda E: E.activation(t2[:], t2[:], AF.Ln, bias=1.0, scale=1.0), reads=[t2], writes=[t2])
                k.op('dve', lambda E: E.scalar_tensor_tensor(tmp[:], tmp[:], 0.0, t2[:], ALU.min, ALU.subtract), reads=[tmp, t2], writes=[tmp])
                k.op('dve', lambda E: E.tensor_scalar_mul(o[:, tsl], tmp[:], 1.0 / 16.0), reads=[tmp], writes=[o], waw=False)
            store_rows(o, 3088 + j * 128, 128)
    return k.finish([out_d] + ([outbf_d] if kind == "even" else []))


def host_inputs_A(kind, layer, inputs, xT_cores):
    i = layer // 2
    ada_w = np.asarray(inputs["ada_w"][layer])
    ada_b = np.asarray(inputs["ada_b"][layer])
    c = np.asarray(inputs["c"])
    g = np.asarray(inputs["norm_mix"][layer])
    maps = []
    if kind == 'even':
        W = np.asarray(inputs["even_w_in"][i])
        offs = np.cumsum([0, 512, 512, 512, 512, 4, 4, 512, 512, 512, 256, 64, 4])
        qa, ka, va, za, ba, aa, qb, kb, vb, qi, ki, wi = [W[:, offs[j]:offs[j + 1]] for j in range(12)]

        def swap(m, dh):
            n = m.shape[1] // dh
            m4 = m.reshape(m.shape[0], n, 2, dh // 2)
            return m4[:, :, ::-1, :].reshape(m.shape[0], -1)
        w_ext = np.concatenate([qa, ka, va, za, qb, swap(qb, 128), kb, swap(kb, 128), vb, qi, swap(qi, 64), ki, swap(ki, 64), ba, aa, wi], axis=1)
        p = np.arange(128)
        inv_a = (1.0 / (10000.0 ** (np.arange(0, 128, 2, dtype=np.float32) / 128))).astype(np.float32)
        inv_i = (1.0 / (10000.0 ** (np.arange(0, 64, 2, dtype=np.float32) / 64))).astype(np.float32)
        inv = np.stack([inv_a[p % 64], inv_i[p % 32]], axis=1).astype(np.float32)
        sgn = np.stack([np.where(p < 64, -1.0, 1.0), np.where((p % 64) < 32, -1.0, 1.0)], axis=1).astype(np.float32)
    else:
        w_ext = np.asarray(inputs["gla_w_in"][i])
    w_ext = np.ascontiguousarray(w_ext, dtype=np.float32)
    adaw = np.ascontiguousarray(ada_w[:, 0:2 * A_D])
    for core in range(8):
        b = core // 4
        s0 = (core % 4) * A_T
        m = {
            "xT": xT_cores[core],
            "condT": np.ascontiguousarray(c[b].reshape(A_KC, 128).T),
            "ada_w": adaw,
            "ada_b": np.ascontiguousarray(ada_b[0:2 * A_D].reshape(2 * A_KC, 128).T),
            "norm_g": np.ascontiguousarray(g.reshape(A_KC, 128).T),
            "w_ext": w_ext,
        }
        if kind == 'even':
            m["pos"] = np.ascontiguousarray(np.asarray(inputs["positions"])[b:b + 1, s0:s0 + A_T]).astype(np.int32)
            m["inv"] = inv
            m["sgn"] = sgn
        else:
            m["wg2"] = np.ascontiguousarray(inputs["gla_w_gate2"][i])
            m["bg2"] = np.ascontiguousarray(np.asarray(inputs["gla_b_gate2"][i]).reshape(4, 128).T)
        maps.append(m)
    return maps


GLA_S_LEN = 8192
GLA_CH = 128
GLA_NCH = GLA_S_LEN // GLA_CH
GLA_GRP = 8
GLA_NGRP = GLA_NCH // GLA_GRP
GLA_DK = 128
GLA_DV = 256
GLA_EPS = 1e-6


def build_gla(k=None, nchunks=GLA_NCH):
    own = k is None
    if own:
        k = K()
    qT_d = k.dram_in("qT", [GLA_DK, GLA_S_LEN])
    kT_d = k.dram_in("kT", [GLA_DK, GLA_S_LEN])
    kt_d = k.dram_in("k_tok", [GLA_S_LEN, GLA_DK])
    la_d = k.dram_in("la_tok", [GLA_S_LEN, GLA_DK])
    v_d = k.dram_in("v_tok", [GLA_S_LEN, GLA_DV])
    r_d = k.dram_in("r_tok", [GLA_S_LEN, GLA_DV])
    g_d = k.dram_in("gnorm", [1, GLA_DV])
    U_d = k.dram_in("U", [128, 128])
    o_d = k.dram_out("o_tok", [GLA_S_LEN, GLA_DV])

    U = k.sb("U", [128, 128])
    Ub = k.sb("Ub", [128, 128], BF16)
    gb = k.sb("gb", [128, GLA_DV])
    k.dma('sp', U[:], U_d[:], reads=[U_d], writes=[U])
    k.dma('sp', gb[:], g_d[0:1, :].partition_broadcast(128), reads=[g_d], writes=[gb])
    k.op('dve', lambda E: E.tensor_copy(Ub[:], U[:]), reads=[U], writes=[Ub])

    S = k.sb("S", [128, GLA_DV])
    Sb = k.sb("Sb", [128, GLA_DV], BF16)
    k.op('dve', lambda E: E.memset(S[:], 0.0), writes=[S])
    k.op('dve', lambda E: E.memset(Sb[:], 0.0), writes=[Sb])

    qTg = [k.sb("qTg%d" % i, [128, GLA_GRP * GLA_CH]) for i in range(2)]
    kTg = [k.sb("kTg%d" % i, [128, GLA_GRP * GLA_CH]) for i in range(2)]
    ktg = [k.sb("ktg%d" % i, [128, GLA_GRP, GLA_DK]) for i in range(2)]
    lag = [k.sb("lag%d" % i, [128, GLA_GRP, GLA_DK]) for i in range(2)]
    vg = [k.sb("vg%d" % i, [128, GLA_GRP, GLA_DV]) for i in range(2)]
    vgb = [k.sb("vgb%d" % i, [128, GLA_GRP, GLA_DV], BF16) for i in range(2)]
    rg = [k.sb("rg%d" % i, [128, GLA_GRP, GLA_DV]) for i in range(2)]
    og = [k.sb("og%d" % i, [128, GLA_GRP, GLA_DV]) for i in range(2)]

    ps_bT = k.ps("bT", [128, 128])
    ps_bt = k.ps("bt", [128, 128])
    ps_A = k.ps("A", [128, 128])
    ps_o = k.ps("o", [128, GLA_DV])
    ps_kv = k.ps("kv", [128, GLA_DV])
    E_ = k.sb("E", [128, 128])
    En = k.sb("En", [128, 128])
    Et = k.sb("Et", [128, 128])
    qe = k.sb("qe", [128, 128], BF16)
    ke = k.sb("ke", [128, 128], BF16)
    ket = k.sb("ket", [128, 128], BF16)
    Am = k.sb("Am", [128, 128], BF16)
    junk = k.sb("junk", [128, GLA_DV])
    ss = k.sb("ss", [128, 1])
    rstd = k.sb("rstd", [128, 1])

    ngrp = (nchunks + GLA_GRP - 1) // GLA_GRP
    for gi in range(ngrp):
        b = gi % 2
        t0 = gi * GLA_GRP * GLA_CH
        tsl = slice(t0, t0 + GLA_GRP * GLA_CH)
        k.dma('sp', qTg[b][:], qT_d[:, tsl], reads=[qT_d], writes=[qTg[b]])
        k.dma('act', kTg[b][:], kT_d[:, tsl], reads=[kT_d], writes=[kTg[b]])
        k.dma('sp', ktg[b][:], kt_d[tsl, :].rearrange("(c p) d -> p c d", p=128), reads=[kt_d], writes=[ktg[b]])
        k.dma('act', lag[b][:], la_d[tsl, :].rearrange("(c p) d -> p c d", p=128), reads=[la_d], writes=[lag[b]])
        k.dma('sp', vg[b][:], v_d[tsl, :].rearrange("(c p) d -> p c d", p=128), reads=[v_d], writes=[vg[b]])
        k.dma('act', rg[b][:], r_d[tsl, :].rearrange("(c p) d -> p c d", p=128), reads=[r_d], writes=[rg[b]])
        k.op('pool', lambda E: E.tensor_copy(vgb[b][:], vg[b][:]), reads=[vg[b]], writes=[vgb[b]])
        k.op('act', lambda E: E.activation(rg[b][:], rg[b][:], AF.Silu), reads=[rg[b]], writes=[rg[b]])
        for c in range(GLA_GRP):
            k.op('pool', lambda E: E.tensor_tensor(rg[b][:, c, :], rg[b][:, c, :], gb[:], ALU.mult), reads=[rg[b], gb], writes=[rg[b]])
        for c in range(GLA_GRP):
            if gi * GLA_GRP + c >= nchunks:
                break
            csl = slice(c * GLA_CH, (c + 1) * GLA_CH)
            k.mm(ps_bT[:], lag[b][:, c, :], U[:], True, True, reads=[lag[b], U], writes=[ps_bT])
            k.mm(ps_bt[:], U[:], lag[b][:, c, :], True, True, reads=[lag[b], U], writes=[ps_bt])
            k.op('act', lambda E: E.activation(E_[:], ps_bT[:], AF.Exp), reads=[ps_bT], writes=[E_])
            k.op('act', lambda E: E.activation(En[:], ps_bT[:], AF.Exp, scale=-1.0), reads=[ps_bT], writes=[En])
            k.op('act', lambda E: E.activation(Et[:], ps_bt[:], AF.Exp, scale=-1.0), reads=[ps_bt], writes=[Et])
            k.op('dve', lambda E: E.scalar_tensor_tensor(qe[:], qTg[b][:, csl], float(GLA_DK) ** -0.5, E_[:], ALU.mult, ALU.mult),
                 reads=[qTg[b], E_], writes=[qe])
            k.op('dve', lambda E: E.tensor_tensor(ke[:], kTg[b][:, csl], En[:], ALU.mult), reads=[kTg[b], En], writes=[ke])
            k.op('pool', lambda E: E.tensor_tensor(ket[:], ktg[b][:, c, :], Et[:], ALU.mult), reads=[ktg[b], Et], writes=[ket])
            k.mm(ps_A[:], ke[:], qe[:], True, True, reads=[ke, qe], writes=[ps_A])
            k.op('dve', lambda E: E.tensor_tensor(Am[:], ps_A[:], U[:], ALU.mult), reads=[ps_A, U], writes=[Am])
            k.mm(ps_o[:], qe[:], Sb[:], True, False, reads=[qe, Sb], writes=[ps_o])
            k.mm(ps_o[:], Am[:], vgb[b][:, c, :], False, True, reads=[Am, vgb[b]], writes=[ps_o])
            k.mm(ps_kv[:], ket[:], vgb[b][:, c, :], True, True, reads=[ket, vgb[b]], writes=[ps_kv])
            k.op('dve', lambda E: E.tensor_tensor(S[:], ps_kv[:], S[:], ALU.add), reads=[ps_kv, S], writes=[S])
            k.op('dve', lambda E: E.tensor_scalar_mul(S[:], S[:], E_[:, GLA_CH - 1:GLA_CH]), reads=[S, E_], writes=[S])
            k.op('act', lambda E: E.copy(Sb[:], S[:]), reads=[S], writes=[Sb])
            k.op('act', lambda E: E.activation(junk[:], ps_o[:], AF.Square, accum_out=ss[:]), reads=[ps_o], writes=[junk, ss])
            k.op('act', lambda E: E.activation(rstd[:], ss[:], AF.Ln, bias=GLA_EPS, scale=1.0 / GLA_DV), reads=[ss], writes=[rstd])
            k.op('act', lambda E: E.activation(rstd[:], rstd[:], AF.Exp, scale=-0.5), reads=[rstd], writes=[rstd])
            k.op('dve', lambda E: E.scalar_tensor_tensor(og[b][:, c, :], ps_o[:], rstd[:], rg[b][:, c, :], ALU.mult, ALU.mult),
                 reads=[ps_o, rstd, rg[b]], writes=[og[b]], waw=False)
        k.dma('pool', o_d[tsl, :].rearrange("(c p) d -> p c d", p=128), og[b][:], reads=[og[b]], writes=[o_d])
    if own:
        return k.finish([o_d])
    return o_d


def host_inputs_gla(layer, inputs, projT_cores):
    i = layer // 2
    P = np.asarray(projT_cores)
    full = [np.concatenate([P[b * 4 + j] for j in range(4)], axis=1) for b in range(2)]
    U = np.triu(np.ones((128, 128), np.float32))
    g = np.ascontiguousarray(np.asarray(inputs["gla_out_norm"][i]).reshape(1, GLA_DV))
    maps = []
    for core in range(8):
        b, h = core // 4, core % 4
        F = full[b]
        qT = F[h * 128:(h + 1) * 128]
        kT = F[512 + h * 128:512 + (h + 1) * 128]
        vT = F[1024 + h * 256:1024 + (h + 1) * 256]
        rT = F[2048 + h * 256:2048 + (h + 1) * 256]
        laT = F[3088 + h * 128:3088 + (h + 1) * 128]
        maps.append(dict(qT=np.ascontiguousarray(qT), kT=np.ascontiguousarray(kT), k_tok=np.ascontiguousarray(kT.T),
                         la_tok=np.ascontiguousarray(laT.T), v_tok=np.ascontiguousarray(vT.T), r_tok=np.ascontiguousarray(rT.T),
                         gnorm=g, U=U))
    return maps


GDN_S_LEN = 8192
GDN_CH = 128
GDN_NCH = GDN_S_LEN // GDN_CH
GDN_GRP = 8
GDN_DK = 128
GDN_DV = 128
GDN_EPS = 1e-6


class Stop(Exception):
    pass


def build_gdn(k, nchunks=GDN_NCH, pfx="", stage=99):
    raw_d = [k.dram_in(pfx + n, [128, 3 + GDN_S_LEN]) for n in ("qraw", "kraw", "vraw")]
    cw_d = k.dram_in(pfx + "cw", [128, 12])
    z_d = k.dram_in(pfx + "z_tok", [GDN_S_LEN, GDN_DV])
    ba_d = k.dram_in(pfx + "ba_tok", [128, GDN_NCH])
    aa_d = k.dram_in(pfx + "aa_tok", [128, GDN_NCH])
    al_d = k.dram_in(pfx + "alog", [128, 1])
    dtb_d = k.dram_in(pfx + "dtb", [128, 1])
    g_d = k.dram_in(pfx + "gnorm", [1, GDN_DV])
    U_d = k.dram_in(pfx + "U", [128, 128])
    Us_d = k.dram_in(pfx + "Us", [128, 128])
    id_d = k.dram_in(pfx + "ident", [128, 128])
    o_d = k.dram_out(pfx + "oa_tok", [GDN_S_LEN, GDN_DV])

    def ld(name, src, shape):
        t = k.sb(name, shape)
        k.dma('sp', t[:], src, reads=[], writes=[t])
        return t
    U = ld("U", U_d[:], [128, 128])
    Us = ld("Us", Us_d[:], [128, 128])
    ident = ld("ident", id_d[:], [128, 128])
    cw = ld("cw", cw_d[:], [128, 12])
    gb = ld("gb", g_d[0:1, :].partition_broadcast(128), [128, GDN_DV])
    ba = ld("ba", ba_d[:], [128, GDN_NCH])
    aa = ld("aa", aa_d[:], [128, GDN_NCH])
    al = ld("al", al_d[:], [128, 1])
    dtb = ld("dtb", dtb_d[:], [128, 1])
    ones = k.sb("ones", [128, 128])
    k.op('pool', lambda E: E.memset(ones[:], 1.0), writes=[ones])

    beta = k.sb("beta", [128, GDN_NCH])
    gt = k.sb("gt", [128, GDN_NCH])
    t1 = k.sb("t1", [128, GDN_NCH])
    k.op('act', lambda E: E.activation(beta[:], ba[:], AF.Sigmoid), reads=[ba], writes=[beta])
    k.op('act', lambda E: E.activation(gt[:], aa[:], AF.Identity, bias=dtb[:], scale=1.0), reads=[aa, dtb], writes=[gt])
    k.op('act', lambda E: E.activation(t1[:], gt[:], AF.Abs), reads=[gt], writes=[t1])
    k.op('act', lambda E: E.activation(t1[:], t1[:], AF.Exp, scale=-1.0), reads=[t1], writes=[t1])
    k.op('act', lambda E: E.activation(t1[:], t1[:], AF.Ln, bias=1.0, scale=1.0), reads=[t1], writes=[t1])
    k.op('dve', lambda E: E.scalar_tensor_tensor(gt[:], gt[:], 0.0, t1[:], ALU.max, ALU.add), reads=[gt, t1], writes=[gt])
    k.op('act', lambda E: E.activation(al[:], al[:], AF.Exp), reads=[al], writes=[al])
    k.op('dve', lambda E: E.tensor_scalar_mul(al[:], al[:], -1.0), reads=[al], writes=[al])
    k.op('dve', lambda E: E.tensor_scalar_mul(gt[:], gt[:], al[:]), reads=[gt, al], writes=[gt])
    ps_misc = k.ps("misc", [128, 512])
    gccol = k.sb("gccol", [128, GDN_NCH])
    k.mm(ps_misc[:, 0:GDN_NCH], U[:], gt[:], True, True, reads=[U, gt], writes=[ps_misc])
    k.op('dve', lambda E: E.tensor_copy(gccol[:], ps_misc[:, 0:GDN_NCH]), reads=[ps_misc], writes=[gccol])

    if stage == 1:
        raise Stop
    S = k.sb("S", [128, GDN_DV])
    Sb = k.sb("Sb", [128, GDN_DV], BF16)
    k.op('dve', lambda E: E.memset(S[:], 0.0), writes=[S])
    k.op('dve', lambda E: E.memset(Sb[:], 0.0), writes=[Sb])

    GW = GDN_GRP * GDN_CH
    rawg = [[k.sb("raw%d_%d" % (j, i), [128, 3 + GW]) for i in range(2)] for j in range(3)]
    cv = [k.sb("cv%d" % j, [128, GW]) for j in range(3)]
    sqb = k.sb("sqb", [128, 512])
    rin = k.sb("rin", [128, 512])
    zg = [k.sb("zg%d" % i, [128, GDN_GRP, GDN_DV]) for i in range(2)]
    og = [k.sb("og%d" % i, [128, GDN_GRP, GDN_DV]) for i in range(2)]
    ps_ss = k.ps("ss", [128, 512])
    ps_a = k.ps("pa", [128, 128])
    ps_b = k.ps("pb", [128, 128])
    ps_c = k.ps("pc", [128, 128])
    ps_o = k.ps("po", [128, GDN_DV])
    ps_s = k.ps("pss", [128, GDN_DV])

    ktok = k.sb("ktok", [128, GDN_DK])
    vtok = k.sb("vtok", [128, GDN_DV])
    gbc = k.sb("gbc", [128, 128])
    grow = k.sb("grow", [128, 128])
    Erow = k.sb("Erow", [128, 128])
    arg = k.sb("arg", [128, 128])
    DT = k.sb("DT", [128, 128])
    DTs = k.sb("DTs", [128, 128])
    NT = k.sb("NT", [128, 128])
    Nn = k.sb("Nn", [128, 128])
    P = [k.sb("P%d" % i, [128, 128]) for i in range(2)]
    PT = [k.sb("PT%d" % i, [128, 128]) for i in range(2)]
    Y = k.sb("Y", [128, 128])
    kgT = k.sb("kgT", [128, 128], BF16)
    qgT = k.sb("qgT", [128, 128], BF16)
    kd = k.sb("kd", [128, 128], BF16)
    Aq = k.sb("Aq", [128, 128], BF16)
    rhsm = k.sb("rhsm", [128, GDN_DV])
    vnew = k.sb("vnew", [128, GDN_DV], BF16)
    ed = k.sb("ed", [128, 1])
    egl = k.sb("egl", [128, 1])
    ngc = k.sb("ngc", [128, 1])
    junk = k.sb("junk", [128, GDN_DV])
    ss = k.sb("ss1", [128, 1])
    rstd = k.sb("rstd1", [128, 1])

    def two(t, name, shape, dt=F32):
        return [t, k.sb(name + "_2", shape, dt)]
    ktok2, vtok2 = two(ktok, "ktok", [128, GDN_DK]), two(vtok, "vtok", [128, GDN_DV])
    gbc2, grow2, Erow2, arg2 = two(gbc, "gbc", [128, 128]), two(grow, "grow", [128, 128]), two(Erow, "Erow", [128, 128]), two(arg, "arg", [128, 128])
    DT2, DTs2, NT2, Nn2, Y2 = two(DT, "DT", [128, 128]), two(DTs, "DTs", [128, 128]), two(NT, "NT", [128, 128]), two(Nn, "Nn", [128, 128]), two(Y, "Y", [128, 128])
    P2 = [P, [k.sb("P%d_2" % i, [128, 128]) for i in range(2)]]
    PT2 = [PT, [k.sb("PT%d_2" % i, [128, 128]) for i in range(2)]]
    kgT2, qgT2 = two(kgT, "kgT", [128, 128], BF16), two(qgT, "qgT", [128, 128], BF16)
    kd2, Aq2 = two(kd, "kd", [128, 128], BF16), two(Aq, "Aq", [128, 128], BF16)
    ed2, egl2, ngc2 = two(ed, "ed", [128, 1]), two(egl, "egl", [128, 1]), two(ngc, "ngc", [128, 1])
    qn, kn, vn = cv

    def group_pre(gi):
        b = gi % 2
        t0 = gi * GW
        for j in range(3):
            k.dma('sp' if j != 1 else 'act', rawg[j][b][:], raw_d[j][:, t0:t0 + 3 + GW], reads=[raw_d[j]], writes=[rawg[j][b]])
        k.dma('act', zg[b][:], z_d[t0:t0 + GW, :].rearrange("(c p) d -> p c d", p=128), reads=[z_d], writes=[zg[b]])
        k.op('act', lambda E: E.activation(zg[b][:], zg[b][:], AF.Silu), reads=[zg[b]], writes=[zg[b]])
        for c in range(GDN_GRP):
            k.op('pool', lambda E: E.tensor_tensor(zg[b][:, c, :], zg[b][:, c, :], gb[:], ALU.mult), reads=[zg[b], gb], writes=[zg[b]])
        for j in range(3):
            r = rawg[j][b]
            k.op('dve', lambda E: E.tensor_scalar_mul(cv[j][:], r[:, 0:GW], cw[:, 4 * j:4 * j + 1]), reads=[r, cw], writes=[cv[j]])
            for w in range(1, 4):
                k.op('dve', lambda E: E.scalar_tensor_tensor(cv[j][:], r[:, w:w + GW], cw[:, 4 * j + w:4 * j + w + 1], cv[j][:], ALU.mult, ALU.add),
                     reads=[r, cw, cv[j]], writes=[cv[j]])
            k.op('act', lambda E: E.activation(cv[j][:], cv[j][:], AF.Silu), reads=[cv[j]], writes=[cv[j]])
        for j in range(2):
            for h2 in range(GW // 512):
                sl = slice(h2 * 512, (h2 + 1) * 512)
                k.op('act', lambda E: E.activation(sqb[:], cv[j][:, sl], AF.Square), reads=[cv[j]], writes=[sqb])
                k.mm(ps_ss[:], ones[:], sqb[:], True, True, reads=[ones, sqb], writes=[ps_ss])
                k.op('act', lambda E: E.activation(rin[:], ps_ss[:], AF.Ln, bias=GDN_EPS, scale=1.0), reads=[ps_ss], writes=[rin])
                k.op('act', lambda E: E.activation(rin[:], rin[:], AF.Exp, scale=-0.5), reads=[rin], writes=[rin])
                k.op('dve', lambda E: E.tensor_tensor(cv[j][:, sl], cv[j][:, sl], rin[:], ALU.mult), reads=[cv[j], rin], writes=[cv[j]])

    def prep(ci):
        x = ci % 2
        c = ci % GDN_GRP
        csl = slice(c * GDN_CH, (c + 1) * GDN_CH)
        ktok, vtok, gbc, grow, Erow, arg = ktok2[x], vtok2[x], gbc2[x], grow2[x], Erow2[x], arg2[x]
        DT, DTs, NT, Nn, Y, P, PT = DT2[x], DTs2[x], NT2[x], Nn2[x], Y2[x], P2[x], PT2[x]
        kgT, qgT, kd, Aq, ed, egl, ngc = kgT2[x], qgT2[x], kd2[x], Aq2[x], ed2[x], egl2[x], ngc2[x]
        k.mm(ps_a[:], kn[:, csl], ident[:], True, True, reads=[kn, ident], writes=[ps_a])
        k.op('dve', lambda E: E.tensor_copy(ktok[:], ps_a[:]), reads=[ps_a], writes=[ktok])
        k.mm(ps_b[:], vn[:, csl], ident[:], True, True, reads=[vn, ident], writes=[ps_b])
        k.op('dve', lambda E: E.tensor_copy(vtok[:], ps_b[:]), reads=[ps_b], writes=[vtok])
        yield
        k.op('dve', lambda E: E.tensor_scalar_mul(gbc[:], ones[:], gt[:, ci:ci + 1]), reads=[gt, ones], writes=[gbc])
        k.mm(ps_c[:], gbc[:], U[:], True, True, reads=[gbc, U], writes=[ps_c])
        k.op('dve', lambda E: E.tensor_copy(grow[:], ps_c[:]), reads=[ps_c], writes=[grow])
        k.op('act', lambda E: E.activation(Erow[:], grow[:], AF.Exp), reads=[grow], writes=[Erow])
        yield
        k.op('dve', lambda E: E.tensor_scalar_sub(arg[:], grow[:], gccol[:, ci:ci + 1]), reads=[grow, gccol], writes=[arg])
        k.op('dve', lambda E: E.tensor_scalar_min(arg[:], arg[:], 0.0), reads=[arg], writes=[arg])
        k.op('act', lambda E: E.activation(arg[:], arg[:], AF.Exp), reads=[arg], writes=[arg])
        k.op('dve', lambda E: E.tensor_tensor(DT[:], arg[:], U[:], ALU.mult), reads=[arg, U], writes=[DT])
        k.op('pool', lambda E: E.tensor_tensor(DTs[:], arg[:], Us[:], ALU.mult), reads=[arg, Us], writes=[DTs])
        yield
        k.op('act', lambda E: E.activation(egl[:], grow[:, GDN_CH - 1:GDN_CH], AF.Exp), reads=[grow], writes=[egl])
        k.op('dve', lambda E: E.tensor_tensor(ngc[:], grow[:, GDN_CH - 1:GDN_CH], gccol[:, ci:ci + 1], ALU.subtract), reads=[grow, gccol], writes=[ngc])
        k.op('act', lambda E: E.activation(ed[:], ngc[:], AF.Exp), reads=[ngc], writes=[ed])
        k.mm(ps_a[:], kn[:, csl], kn[:, csl], True, True, reads=[kn], writes=[ps_a])
        k.op('dve', lambda E: E.scalar_tensor_tensor(NT[:], ps_a[:], beta[:, ci:ci + 1], DTs[:], ALU.mult, ALU.mult),
             reads=[ps_a, beta, DTs], writes=[NT])
        yield
        k.mm(ps_b[:], NT[:], ident[:], True, True, reads=[NT, ident], writes=[ps_b])
        k.op('dve', lambda E: E.tensor_copy(Nn[:], ps_b[:]), reads=[ps_b], writes=[Nn])
        k.op('dve', lambda E: E.tensor_tensor(Y[:], ident[:], NT[:], ALU.subtract), reads=[ident, NT], writes=[Y])
        yield
        k.mm(ps_a[:], NT[:], Nn[:], True, True, reads=[NT, Nn], writes=[ps_a])
        k.op('dve', lambda E: E.tensor_copy(P[0][:], ps_a[:]), reads=[ps_a], writes=[P[0]])
        k.mm(ps_b[:], Nn[:], NT[:], True, True, reads=[NT, Nn], writes=[ps_b])
        k.op('dve', lambda E: E.tensor_copy(PT[0][:], ps_b[:]), reads=[ps_b], writes=[PT[0]])
        yield
        for lv in range(6):
            a_, n_ = lv % 2, (lv + 1) % 2
            k.mm(ps_c[:], P[a_][:], Y[:], True, True, reads=[P[a_], Y], writes=[ps_c])
            k.op('dve', lambda E: E.tensor_tensor(Y[:], Y[:], ps_c[:], ALU.add), reads=[Y, ps_c], writes=[Y])
            if lv < 5:
                k.mm(ps_a[:], PT[a_][:], P[a_][:], True, True, reads=[PT[a_], P[a_]], writes=[ps_a])
                k.mm(ps_b[:], P[a_][:], PT[a_][:], True, True, reads=[PT[a_], P[a_]], writes=[ps_b])
                k.op('dve', lambda E: E.tensor_copy(P[n_][:], ps_a[:]), reads=[ps_a], writes=[P[n_]])
                k.op('dve', lambda E: E.tensor_copy(PT[n_][:], ps_b[:]), reads=[ps_b], writes=[PT[n_]])
            yield
        k.op('dve', lambda E: E.tensor_tensor(kgT[:], kn[:, csl], Erow[:], ALU.mult), reads=[kn, Erow], writes=[kgT])
        k.op('dve', lambda E: E.scalar_tensor_tensor(qgT[:], qn[:, csl], float(GDN_DK) ** -0.5, Erow[:], ALU.mult, ALU.mult),
             reads=[qn, Erow], writes=[qgT])
        k.op('dve', lambda E: E.tensor_scalar_mul(kd[:], ktok[:], ed[:]), reads=[ktok, ed], writes=[kd])
        yield
        k.mm(ps_a[:], kn[:, csl], qn[:, csl], True, True, reads=[kn, qn], writes=[ps_a])
        k.op('dve', lambda E: E.scalar_tensor_tensor(Aq[:], ps_a[:], float(GDN_DK) ** -0.5, DT[:], ALU.mult, ALU.mult),
             reads=[ps_a, DT], writes=[Aq])
        yield

    def seq(ci):
        x = ci % 2
        c = ci % GDN_GRP
        b = (ci // GDN_GRP) % 2
        vtok, Y, kgT, qgT, kd, Aq, egl = vtok2[x], Y2[x], kgT2[x], qgT2[x], kd2[x], Aq2[x], egl2[x]
        k.mm(ps_s[:], kgT[:], Sb[:], True, True, reads=[kgT, Sb], writes=[ps_s])
        k.op('dve', lambda E: E.tensor_tensor(rhsm[:], vtok[:], ps_s[:], ALU.subtract), reads=[vtok, ps_s], writes=[rhsm])
        yield
        k.mm(ps_s[:], Y[:], rhsm[:], True, True, reads=[Y, rhsm], writes=[ps_s])
        k.op('dve', lambda E: E.tensor_scalar_mul(vnew[:], ps_s[:], beta[:, ci:ci + 1]), reads=[ps_s, beta], writes=[vnew])
        yield
        k.mm(ps_o[:], qgT[:], Sb[:], True, False, reads=[qgT, Sb], writes=[ps_o])
        k.mm(ps_o[:], Aq[:], vnew[:], False, True, reads=[Aq, vnew], writes=[ps_o])
        k.mm(ps_s[:], kd[:], vnew[:], True, True, reads=[kd, vnew], writes=[ps_s])
        k.op('dve', lambda E: E.scalar_tensor_tensor(S[:], S[:], egl[:], ps_s[:], ALU.mult, ALU.add), reads=[S, egl, ps_s], writes=[S])
        k.op('act', lambda E: E.copy(Sb[:], S[:]), reads=[S], writes=[Sb])
        yield
        k.op('act', lambda E: E.activation(junk[:], ps_o[:], AF.Square, accum_out=ss[:]), reads=[ps_o], writes=[junk, ss])
        k.op('act', lambda E: E.activation(rstd[:], ss[:], AF.Ln, bias=GDN_EPS, scale=1.0 / GDN_DV), reads=[ss], writes=[rstd])
        k.op('act', lambda E: E.activation(rstd[:], rstd[:], AF.Exp, scale=-0.5), reads=[rstd], writes=[rstd])
        yield
        k.op('dve', lambda E: E.scalar_tensor_tensor(og[b][:, c, :], ps_o[:], rstd[:], zg[b][:, c, :], ALU.mult, ALU.mult),
             reads=[ps_o, rstd, zg[b]], writes=[og[b]], waw=False)
        yield

    def drain(g):
        for _ in g:
            pass

    def corun(g_seq, g_prep):
        alive_s = alive_p = True
        while alive_s or alive_p:
            if alive_s:
                try:
                    next(g_seq)
                except StopIteration:
                    alive_s = False
            for _ in range(3):
                if alive_p:
                    try:
                        next(g_prep)
                    except StopIteration:
                        alive_p = False

    group_pre(0)
    drain(prep(0))
    for ci in range(nchunks):
        nxt = ci + 1
        if nxt < nchunks:
            if nxt % GDN_GRP == 0:
                group_pre(nxt // GDN_GRP)
            corun(seq(ci), prep(nxt))
        else:
            drain(seq(ci))
        if ci % GDN_GRP == GDN_GRP - 1 or ci == nchunks - 1:
            gi = ci // GDN_GRP
            t0 = gi * GW
            k.dma('pool', o_d[t0:t0 + GW, :].rearrange("(c p) d -> p c d", p=128), og[gi % 2][:], reads=[og[gi % 2]], writes=[o_d])
    return o_d


def host_inputs_gdn(layer, inputs, full, core):
    i = layer // 2
    b, h = core // 4, core % 4
    F = full[b]

    def raw(row0):
        a = np.zeros((128, 3 + GDN_S_LEN), np.float32)
        a[:, 3:] = F[row0 + h * 128:row0 + (h + 1) * 128]
        return a
    cwf = np.asarray(inputs["gdn_conv_w"][i])
    cw = np.concatenate([cwf[:, j * 512 + h * 128:j * 512 + (h + 1) * 128].T for j in range(3)], axis=1)
    m = dict(qraw=raw(0), kraw=raw(512), vraw=raw(1024),
             cw=np.ascontiguousarray(cw, dtype=np.float32),
             z_tok=np.ascontiguousarray(F[1536 + h * 128:1536 + (h + 1) * 128].T),
             ba_tok=np.ascontiguousarray(F[3904 + h].reshape(GDN_NCH, 128).T),
             aa_tok=np.ascontiguousarray(F[3908 + h].reshape(GDN_NCH, 128).T),
             alog=np.full((128, 1), np.asarray(inputs["gdn_a_log"])[i, h], np.float32),
             dtb=np.full((128, 1), np.asarray(inputs["gdn_dt_bias"])[i, h], np.float32),
             gnorm=np.ascontiguousarray(np.asarray(inputs["gdn_out_norm"][i]).reshape(1, GDN_DV)),
             U=np.triu(np.ones((128, 128), np.float32)), Us=np.triu(np.ones((128, 128), np.float32), 1),
             ident=np.eye(128, dtype=np.float32))
    return m


DSA_S_LEN = 8192
DSA_QT = 128
DSA_KB = 512
DSA_TOPK = 256
DSA_DH = 128
DSA_NH = 4
DSA_NEG = -30000.0
DSA_NBIS = 30
DSA_NSLOT = 16


def build_dsa_v1(k, nslots=DSA_NSLOT, slot0=0, pfx=""):
    qb_d = k.dram_in(pfx + "qbT", [DSA_NSLOT, 128, DSA_NH, DSA_QT], BF16)
    qi_d = k.dram_in(pfx + "qiT", [DSA_NSLOT, 64, DSA_NH, DSA_QT])
    wi_d = k.dram_in(pfx + "wi", [DSA_NSLOT, DSA_QT, DSA_NH])
    kb_d = k.dram_in(pfx + "kbT", [DSA_NH, 128, DSA_S_LEN], BF16)
    vb_d = k.dram_in(pfx + "vb", [DSA_NH, 128, DSA_S_LEN // 128, DSA_DH], BF16)
    ki_d = k.dram_in(pfx + "kiT", [64, DSA_S_LEN])
    cb_d = k.dram_in(pfx + "cb", [128, DSA_KB])
    idb_d = k.dram_in(pfx + "identb", [128, 128], BF16)
    p2_d = k.dram_in(pfx + "pow2", [128, DSA_NBIS])
    o_d = k.dram_out(pfx + "ob", [DSA_NSLOT, DSA_QT, DSA_NH * DSA_DH])

    kiT = k.sb("kiT", [64, DSA_S_LEN])
    cb = k.sb("cb", [128, DSA_KB])
    idb = k.sb("idb", [128, 128], BF16)
    k.dma('sp', kiT[:, 0:4096], ki_d[:, 0:4096], writes=[kiT])
    k.dma('act', kiT[:, 4096:], ki_d[:, 4096:], writes=[kiT])
    k.dma('sp', cb[:], cb_d[:], writes=[cb])
    k.dma('sp', idb[:], idb_d[:], writes=[idb])
    pow2 = k.sb("pow2", [128, DSA_NBIS])
    dtab = k.sb("dtab", [128, DSA_NBIS])
    k.dma('sp', pow2[:], p2_d[:], writes=[pow2])

    score = k.sb("score", [128, DSA_S_LEN])
    selb = k.sb("selb", [128, DSA_S_LEN], BF16)
    L = k.sb("L", [128, DSA_S_LEN])
    Pm = k.sb("Pm", [128, DSA_S_LEN], BF16)
    KhT = k.sb("KhT", [128, DSA_S_LEN], BF16)
    Vh = k.sb("Vh", [128, DSA_S_LEN // 128, DSA_DH], BF16)
    qb = k.sb("qb", [128, DSA_NH, DSA_QT], BF16)
    qi = k.sb("qi", [64, DSA_NH, DSA_QT])
    wi = k.sb("wi", [128, DSA_NH])
    relu_t = [k.sb("relu%d" % i, [128, DSA_KB]) for i in range(2)]
    PT = [k.sb("PT%d" % i, [128, 128], BF16) for i in range(2)]
    osb = k.sb("osb", [128, DSA_NH * DSA_DH])
    ps_big = [k.ps("big%d" % i, [128, DSA_KB]) for i in range(3)]
    ps_T = [k.ps("psT%d" % i, [128, 128], BF16) for i in range(2)]
    ps_o = k.ps("pso", [128, DSA_DH])
    sc = {n: k.sb("sc_" + n, [128, 1]) for n in ("lo", "hi", "d", "nmid", "acc", "ge", "gh", "t", "mx", "nmx", "rs", "rinv")}
    nbig = [0]

    def big():
        p = ps_big[nbig[0] % 3]
        nbig[0] += 1
        return p

    for n in range(slot0, slot0 + nslots):
        N = DSA_KB * (n + 1)
        NB = n + 1
        k.dma('sp', qb[:], qb_d[n], reads=[], writes=[qb])
        k.dma('act', qi[:], qi_d[n], reads=[], writes=[qi])
        k.dma('sp', wi[:], wi_d[n], reads=[], writes=[wi])
        k.op('dve', lambda E: E.tensor_scalar_mul(wi[:], wi[:], float(256.0 ** -0.5)), reads=[wi], writes=[wi])
        for kb_ in range(NB):
            ksl = slice(kb_ * DSA_KB, (kb_ + 1) * DSA_KB)
            for h in range(DSA_NH):
                ps = big()
                k.mm(ps[:], qi[:, h, :], kiT[:, ksl], True, True, reads=[qi, kiT], writes=[ps])
                rt = relu_t[h % 2]
                k.op('act', lambda E: E.activation(rt[:], ps[:], AF.Relu), reads=[ps], writes=[rt])
                if h == 0:
                    k.op('dve', lambda E: E.tensor_scalar_mul(score[:, ksl], rt[:], wi[:, 0:1]), reads=[rt, wi], writes=[score], waw=False)
                else:
                    k.op('dve', lambda E: E.scalar_tensor_tensor(score[:, ksl], rt[:], wi[:, h:h + 1], score[:, ksl], ALU.mult, ALU.add),
                         reads=[rt, wi, score], writes=[score])
        lo, hi, d, nmid, acc, ge, gh, t_, mx, nmx, rs, rinv = [sc[x] for x in ("lo", "hi", "d", "nmid", "acc", "ge", "gh", "t", "mx", "nmx", "rs", "rinv")]
        k.op('dve', lambda E: E.tensor_reduce(hi[:], score[:, 0:N], AX.X, ALU.max), reads=[score], writes=[hi])
        k.op('dve', lambda E: E.tensor_reduce(lo[:], score[:, 0:N], AX.X, ALU.min), reads=[score], writes=[lo])
        k.op('dve', lambda E: E.tensor_scalar_add(hi[:], hi[:], 1.0), reads=[hi], writes=[hi])
        k.op('dve', lambda E: E.tensor_scalar_add(lo[:], lo[:], -1.0), reads=[lo], writes=[lo])
        k.op('dve', lambda E: E.tensor_tensor(score[:, N - DSA_KB:N], score[:, N - DSA_KB:N], cb[:], ALU.add), reads=[score, cb], writes=[score])
        k.op('dve', lambda E: E.tensor_tensor(d[:], hi[:], lo[:], ALU.subtract), reads=[hi, lo], writes=[d])
        k.op('dve', lambda E: E.tensor_scalar_mul(dtab[:], pow2[:], d[:]), reads=[pow2, d], writes=[dtab])
        for it in range(DSA_NBIS):
            k.op('dve', lambda E: E.scalar_tensor_tensor(nmid[:], lo[:], -1.0, dtab[:, it:it + 1], ALU.mult, ALU.subtract), reads=[lo, dtab], writes=[nmid])
            k.op('act', lambda E: E.activation(L[:, 0:N], score[:, 0:N], AF.Sign, bias=nmid[:], scale=1.0, accum_out=acc[:]),
                 reads=[score, nmid], writes=[L, acc])
            k.op('dve', lambda E: E.tensor_scalar(ge[:], acc[:], float(2 * DSA_TOPK - N), 1.0, ALU.is_ge, ALU.mult), reads=[acc], writes=[ge])
            k.op('dve', lambda E: E.scalar_tensor_tensor(lo[:], ge[:], dtab[:, it:it + 1], lo[:], ALU.mult, ALU.add), reads=[ge, dtab, lo], writes=[lo])
        k.op('dve', lambda E: E.tensor_scalar_sub(L[:, 0:N], score[:, 0:N], lo[:]), reads=[score, lo], writes=[L])
        k.op('dve', lambda E: E.tensor_scalar(L[:, 0:N], L[:, 0:N], 0.0, 1.0, ALU.is_ge, ALU.mult), reads=[L], writes=[L])
        k.op('dve', lambda E: E.tensor_scalar(selb[:, 0:N], L[:, 0:N], -1.0, -DSA_NEG, ALU.add, ALU.mult), reads=[L], writes=[selb])
        for h in range(DSA_NH):
            k.dma('sp', KhT[:, 0:N], kb_d[h, :, 0:N], reads=[], writes=[KhT])
            k.dma('act', Vh[:, 0:4 * NB, :], vb_d[h, :, 0:4 * NB, :], reads=[], writes=[Vh])
            for kb_ in range(NB):
                ksl = slice(kb_ * DSA_KB, (kb_ + 1) * DSA_KB)
                ps = big()
                k.mm(ps[:], qb[:, h, :], KhT[:, ksl], True, True, reads=[qb, KhT], writes=[ps])
                k.op('dve', lambda E: E.scalar_tensor_tensor(L[:, ksl], ps[:], float(DSA_DH) ** -0.5, selb[:, ksl], ALU.mult, ALU.add),
                     reads=[ps, selb], writes=[L], waw=False)
            k.op('dve', lambda E: E.tensor_reduce(mx[:], L[:, 0:N], AX.X, ALU.max), reads=[L], writes=[mx])
            k.op('dve', lambda E: E.tensor_scalar_mul(nmx[:], mx[:], -1.0), reads=[mx], writes=[nmx])
            k.op('act', lambda E: E.activation(Pm[:, 0:N], L[:, 0:N], AF.Exp, bias=nmx[:], scale=1.0, accum_out=rs[:]),
                 reads=[L, nmx], writes=[Pm, rs])
            nkt = 4 * NB
            for kt in range(nkt):
                pt = ps_T[kt % 2]
                sbt = PT[kt % 2]
                k.tr(pt[:], Pm[:, kt * 128:(kt + 1) * 128], idb, reads=[Pm], writes=[pt])
                if kt % 2 == 0:
                    k.op('act', lambda E: E.copy(sbt[:], pt[:]), reads=[pt], writes=[sbt])
                else:
                    k.op('dve', lambda E: E.tensor_copy(sbt[:], pt[:]), reads=[pt], writes=[sbt])
                k.mm(ps_o[:], sbt[:], Vh[:, kt, :], kt == 0, kt == nkt - 1, reads=[sbt, Vh], writes=[ps_o])
            k.op('dve', lambda E: E.reciprocal(rinv[:], rs[:]), reads=[rs], writes=[rinv])
            k.op('dve', lambda E: E.tensor_scalar_mul(osb[:, h * DSA_DH:(h + 1) * DSA_DH], ps_o[:], rinv[:]), reads=[ps_o, rinv], writes=[osb])
        k.dma('pool', o_d[n], osb[:], reads=[osb], writes=[o_d])
    return o_d


def build_dsa(k, nslots=DSA_NSLOT, slot0=0, pfx=""):
    qb_d = k.dram_in(pfx + "qbT", [DSA_NSLOT, 128, DSA_NH, DSA_QT], BF16)
    qi_d = k.dram_in(pfx + "qiT", [DSA_NSLOT, 64, DSA_NH, DSA_QT])
    wi_d = k.dram_in(pfx + "wi", [DSA_NSLOT, DSA_QT, DSA_NH])
    kb_d = k.dram_in(pfx + "kbT", [DSA_NH, 128, DSA_S_LEN], BF16)
    vb_d = k.dram_in(pfx + "vb", [DSA_NH, 128, DSA_S_LEN // 128, DSA_DH], BF16)
    ki_d = k.dram_in(pfx + "kiT", [64, DSA_S_LEN])
    cb_d = k.dram_in(pfx + "cb", [128, DSA_KB])
    idb_d = k.dram_in(pfx + "identb", [128, 128], BF16)
    p2_d = k.dram_in(pfx + "pow2", [128, DSA_NBIS + 1])
    o_d = k.dram_out(pfx + "ob", [DSA_NSLOT, DSA_QT, DSA_NH * DSA_DH])

    kiT = k.sb("kiT", [64, DSA_S_LEN])
    cb = k.sb("cb", [128, DSA_KB])
    idb = k.sb("idb", [128, 128], BF16)
    pow2 = k.sb("pow2", [128, DSA_NBIS + 1])
    dtab = k.sb("dtab", [128, DSA_NBIS + 1])
    ndtab = k.sb("ndtab", [128, DSA_NBIS + 1])
    k.dma('sp', kiT[:, 0:4096], ki_d[:, 0:4096], writes=[kiT])
    k.dma('act', kiT[:, 4096:], ki_d[:, 4096:], writes=[kiT])
    k.dma('sp', cb[:], cb_d[:], writes=[cb])
    k.dma('sp', idb[:], idb_d[:], writes=[idb])
    k.dma('sp', pow2[:], p2_d[:], writes=[pow2])

    score = k.sb("score", [128, DSA_S_LEN])
    junk = k.sb("junk", [128, DSA_S_LEN], BF16)
    selbs = [k.sb("selb%d" % i, [128, DSA_S_LEN], BF16) for i in range(2)]
    L = k.sb("L", [128, DSA_S_LEN])
    Pm = k.sb("Pm", [128, DSA_S_LEN], BF16)
    KhT = k.sb("KhT", [128, DSA_S_LEN], BF16)
    Vh = k.sb("Vh", [128, DSA_S_LEN // 128, DSA_DH], BF16)
    qbs = [k.sb("qb%d" % i, [128, DSA_NH, DSA_QT], BF16) for i in range(2)]
    qi = k.sb("qi", [64, DSA_NH, DSA_QT])
    wi = k.sb("wi", [128, DSA_NH])
    relu_t = [k.sb("relu%d" % i, [128, DSA_KB]) for i in range(2)]
    PT = [k.sb("PT%d" % i, [128, 128], BF16) for i in range(2)]
    osb = k.sb("osb", [128, DSA_NH * DSA_DH])
    ps_ix = [k.ps("ix%d" % i, [128, DSA_KB]) for i in range(2)]
    ps_lg = [k.ps("lg%d" % i, [128, DSA_KB]) for i in range(2)]
    ps_T = [k.ps("psT%d" % i, [128, 128], BF16) for i in range(2)]
    ps_o = k.ps("pso", [128, DSA_DH])
    sc = {n: k.sb("sc_" + n, [128, 1]) for n in ("lo", "hi", "d", "nmid", "acc", "ge", "mx", "nmx", "rs", "rinv", "base")}
    cnt = dict(ix=0, lg=0)

    def phase1(n):
        N = DSA_KB * (n + 1)
        NB = n + 1
        qb = qbs[n % 2]
        selb = selbs[n % 2]
        lo, hi, d, nmid, acc, ge = [sc[x] for x in ("lo", "hi", "d", "nmid", "acc", "ge")]
        k.dma('sp', qb[:], qb_d[n], reads=[], writes=[qb])
        k.dma('sp', qi[:], qi_d[n], reads=[], writes=[qi])
        k.dma('sp', wi[:], wi_d[n], reads=[], writes=[wi])
        k.op('dve', lambda E: E.tensor_scalar_mul(wi[:], wi[:], float(256.0 ** -0.5)), reads=[wi], writes=[wi])
        for kb_ in range(NB):
            ksl = slice(kb_ * DSA_KB, (kb_ + 1) * DSA_KB)
            for h in range(DSA_NH):
                ps = ps_ix[cnt['ix'] % 2]
                cnt['ix'] += 1
                k.mm(ps[:], qi[:, h, :], kiT[:, ksl], True, True, reads=[qi, kiT], writes=[ps])
                rt = relu_t[h % 2]
                k.op('act', lambda E: E.activation(rt[:], ps[:], AF.Relu), reads=[ps], writes=[rt])
                if h == 0:
                    k.op('dve', lambda E: E.tensor_scalar_mul(score[:, ksl], rt[:], wi[:, 0:1]), reads=[rt, wi], writes=[score], waw=False)
                else:
                    k.op('dve', lambda E: E.scalar_tensor_tensor(score[:, ksl], rt[:], wi[:, h:h + 1], score[:, ksl], ALU.mult, ALU.add),
                         reads=[rt, wi, score], writes=[score])
            yield True
        k.op('dve', lambda E: E.tensor_reduce(hi[:], score[:, 0:N], AX.X, ALU.max), reads=[score], writes=[hi])
        k.op('dve', lambda E: E.tensor_reduce(lo[:], score[:, 0:N], AX.X, ALU.min), reads=[score], writes=[lo])
        k.op('dve', lambda E: E.tensor_scalar_add(hi[:], hi[:], 1.0), reads=[hi], writes=[hi])
        k.op('dve', lambda E: E.tensor_scalar_add(lo[:], lo[:], -1.0), reads=[lo], writes=[lo])
        k.op('dve', lambda E: E.tensor_tensor(score[:, N - DSA_KB:N], score[:, N - DSA_KB:N], cb[:], ALU.add), reads=[score, cb], writes=[score])
        k.op('dve', lambda E: E.tensor_tensor(d[:], hi[:], lo[:], ALU.subtract), reads=[hi, lo], writes=[d])
        k.op('dve', lambda E: E.tensor_scalar_mul(dtab[:], pow2[:], d[:]), reads=[pow2, d], writes=[dtab])
        k.op('dve', lambda E: E.tensor_scalar_mul(ndtab[:], dtab[:], -1.0), reads=[dtab], writes=[ndtab])
        base = sc["base"]
        k.op('dve', lambda E: E.scalar_tensor_tensor(nmid[:], lo[:], -1.0, dtab[:, 0:1], ALU.mult, ALU.subtract), reads=[lo, dtab], writes=[nmid])
        yield True
        for it in range(DSA_NBIS):
            k.op('act', lambda E: E.activation(junk[:, 0:N], score[:, 0:N], AF.Sign, bias=nmid[:], scale=1.0, accum_out=acc[:]),
                 reads=[score, nmid], writes=[junk, acc])
            k.op('dve', lambda E: E.tensor_tensor(base[:], nmid[:], dtab[:, it + 1:it + 2], ALU.add), reads=[nmid, dtab], writes=[base])
            yield True
            k.op('dve', lambda E: E.tensor_scalar(ge[:], acc[:], float(2 * DSA_TOPK - N), 1.0, ALU.is_ge, ALU.mult), reads=[acc], writes=[ge])
            k.op('dve', lambda E: E.scalar_tensor_tensor(nmid[:], ge[:], ndtab[:, it:it + 1], base[:], ALU.mult, ALU.add), reads=[ge, ndtab, base], writes=[nmid])
        k.op('dve', lambda E: E.scalar_tensor_tensor(lo[:], nmid[:], -1.0, dtab[:, DSA_NBIS:DSA_NBIS + 1], ALU.mult, ALU.subtract), reads=[nmid, dtab], writes=[lo])
        k.op('dve', lambda E: E.tensor_scalar_sub(score[:, 0:N], score[:, 0:N], lo[:]), reads=[score, lo], writes=[score])
        yield True
        k.op('dve', lambda E: E.tensor_scalar(score[:, 0:N], score[:, 0:N], 0.0, 1.0, ALU.is_ge, ALU.mult), reads=[score], writes=[score])
        yield True
        k.op('dve', lambda E: E.tensor_scalar(selb[:, 0:N], score[:, 0:N], -1.0, -DSA_NEG, ALU.add, ALU.mult), reads=[score], writes=[selb])
        yield True

    def phase2(n):
        N = DSA_KB * (n + 1)
        NB = n + 1
        qb = qbs[n % 2]
        selb = selbs[n % 2]
        mx, nmx, rs, rinv = [sc[x] for x in ("mx", "nmx", "rs", "rinv")]
        for h in range(DSA_NH):
            k.dma('sp', KhT[:, 0:N], kb_d[h, :, 0:N], reads=[], writes=[KhT])
            k.dma('sp', Vh[:, 0:4 * NB, :], vb_d[h, :, 0:4 * NB, :], reads=[], writes=[Vh])
            for kb_ in range(NB):
                ksl = slice(kb_ * DSA_KB, (kb_ + 1) * DSA_KB)
                ps = ps_lg[cnt['lg'] % 2]
                cnt['lg'] += 1
                k.mm(ps[:], qb[:, h, :], KhT[:, ksl], True, True, reads=[qb, KhT], writes=[ps])
                k.op('dve', lambda E: E.scalar_tensor_tensor(L[:, ksl], ps[:], float(DSA_DH) ** -0.5, selb[:, ksl], ALU.mult, ALU.add),
                     reads=[ps, selb], writes=[L], waw=False)
                if kb_ % 2 == 1:
                    yield
            k.op('dve', lambda E: E.tensor_reduce(mx[:], L[:, 0:N], AX.X, ALU.max), reads=[L], writes=[mx])
            k.op('dve', lambda E: E.tensor_scalar_mul(nmx[:], mx[:], -1.0), reads=[mx], writes=[nmx])
            k.op('act', lambda E: E.activation(Pm[:, 0:N], L[:, 0:N], AF.Exp, bias=nmx[:], scale=1.0, accum_out=rs[:]),
                 reads=[L, nmx], writes=[Pm, rs])
            yield
            nkt = 4 * NB
            for kt in range(nkt):
                pt = ps_T[kt % 2]
                sbt = PT[kt % 2]
                k.tr(pt[:], Pm[:, kt * 128:(kt + 1) * 128], idb, reads=[Pm], writes=[pt])
                k.op('dve', lambda E: E.tensor_copy(sbt[:], pt[:]), reads=[pt], writes=[sbt])
                k.mm(ps_o[:], sbt[:], Vh[:, kt, :], kt == 0, kt == nkt - 1, reads=[sbt, Vh], writes=[ps_o])
                if kt % 4 == 3:
                    yield
            k.op('dve', lambda E: E.reciprocal(rinv[:], rs[:]), reads=[rs], writes=[rinv])
            k.op('dve', lambda E: E.tensor_scalar_mul(osb[:, h * DSA_DH:(h + 1) * DSA_DH], ps_o[:], rinv[:]), reads=[ps_o, rinv], writes=[osb])
            yield
        k.dma('pool', o_d[n], osb[:], reads=[osb], writes=[o_d])
        yield

    def drain(g):
        for _ in g:
            pass

    def corun(g2, g1, n2, w1):
        done2 = 0
        wi_ = 0
        alive2 = True
        for _ in g1:
            wi_ += 1
            target = -(-n2 * wi_ // max(w1, 1))
            while alive2 and done2 < target:
                try:
                    next(g2)
                    done2 += 1
                except StopIteration:
                    alive2 = False
        if alive2:
            drain(g2)

    last = slot0 + nslots - 1
    drain(phase1(slot0))
    for n in range(slot0, slot0 + nslots):
        if n < last:
            NB2 = n + 1
            n2 = 4 * (NB2 // 2 + NB2 + 2) + 1
            w1 = (n + 2) + 1 + DSA_NBIS + 3
            corun(phase2(n), phase1(n + 1), n2, w1)
        else:
            drain(phase2(n))
    return o_d


def host_inputs_dsa(full, fullbf, core):
    b, r = core // 4, core % 4
    F, Fb = full[b], fullbf[b]
    tiles = [4 * n + r for n in range(DSA_NSLOT)]
    qbf = Fb[0:512].reshape(DSA_NH, 128, DSA_S_LEN)
    qif = F[3584:3840].reshape(DSA_NH, 64, DSA_S_LEN)
    qbT = np.stack([qbf[:, :, i * 128:(i + 1) * 128].transpose(1, 0, 2) for i in tiles])
    qiT = np.stack([qif[:, :, i * 128:(i + 1) * 128].transpose(1, 0, 2) for i in tiles])
    wi = np.stack([F[3912:3916, i * 128:(i + 1) * 128].T for i in tiles])
    sp = np.arange(DSA_KB)[None, :]
    tq = np.arange(128)[:, None]
    cb = np.where(sp <= 128 * r + tq, 0.0, -1e9).astype(np.float32)
    return dict(qbT=np.ascontiguousarray(qbT), qiT=np.ascontiguousarray(qiT, dtype=np.float32), wi=np.ascontiguousarray(wi, dtype=np.float32),
                kbT=np.ascontiguousarray(Fb[512:1024].reshape(DSA_NH, 128, DSA_S_LEN)),
                vb=np.ascontiguousarray(Fb[1024:1536].reshape(DSA_NH, 128, DSA_S_LEN // 128, 128).transpose(0, 3, 2, 1)),
                kiT=np.ascontiguousarray(F[3840:3904], dtype=np.float32), cb=cb,
                identb=np.eye(128, dtype=np.float32).astype(ml_dtypes.bfloat16),
                pow2=np.tile((0.5 ** np.arange(1, DSA_NBIS + 2)).astype(np.float32)[None, :], (128, 1)))


C_T = 2048
C_NT_ = C_T // 128
C_D = 1024
C_KC = 8
C_DE = 512
C_NE = 32
C_EPS = 1e-6


def ge_ap(k, out, in_ap, sc_ap, reads, writes):
    k.op('dve', lambda E: E.tensor_scalar_sub(out, in_ap, sc_ap), reads=reads, writes=writes)
    k.op('dve', lambda E: E.tensor_scalar(out, out, 0.0, 1.0, ALU.is_ge, ALU.mult), reads=writes, writes=writes)


def build_C(final=False):
    k = K()
    x_d = k.dram_in("x_tok", [C_T, C_D])
    oT_d = k.dram_in("oT", [C_D, C_T])
    wout_d = k.dram_in("w_out", [C_D, C_D])
    condT_d = k.dram_in("condT", [128, C_KC])
    adaw_d = k.dram_in("ada_w", [C_D, 4 * C_D])
    adab_d = k.dram_in("ada_b", [1, 4 * C_D])
    g_d = k.dram_in("norm_g", [1, C_D])
    wr_d = k.dram_in("w_router", [C_D, 36])
    br_d = k.dram_in("b_router", [1, 36])
    w1_d = k.dram_in("w1", [C_NE, C_D, C_DE])
    w3_d = k.dram_in("w3", [C_NE, C_D, C_DE])
    w2_d = k.dram_in("w2", [C_NE, C_DE, C_D])
    id_d = k.dram_in("ident", [128, 128])
    fin_d = k.dram_in("final_g", [1, C_D])
    out_d = k.dram_out("x_out", [C_T, C_D])

    ident = k.sb("ident", [128, 128])
    identb = k.sb("identb", [128, 128], BF16)
    ones1 = k.sb("ones1", [1, 128])
    k.dma('sp', ident[:], id_d[:], writes=[ident])
    k.op('dve', lambda E: E.tensor_copy(identb[:], ident[:]), reads=[ident], writes=[identb])
    k.op('pool', lambda E: E.memset(ones1[:], 1.0), writes=[ones1])

    cond = k.sb("cond", [128, C_KC])
    k.dma('sp', cond[:], condT_d[:], writes=[cond])
    k.op('act', lambda E: E.activation(cond[:], cond[:], AF.Silu), reads=[cond], writes=[cond])
    wst = [k.sb("wst%d" % i, [128, C_KC, 512]) for i in range(2)]
    psA = [k.ps("psA%d" % i, [128, 512]) for i in range(4)]
    npa = [0]

    def pa():
        p = psA[npa[0] % 4]
        npa[0] += 1
        return p
    modbc = k.sb("modbc", [128, 4 * C_D])
    k.dma('sp', modbc[0:1, :], adab_d[:], writes=[modbc])
    for j in range(8):
        b = j % 2
        k.dma('sp' if b == 0 else 'act', wst[b][:], adaw_d[:, j * 512:(j + 1) * 512].rearrange("(c p) n -> p c n", p=128), writes=[wst[b]])
        ps = pa()
        for c in range(C_KC):
            k.mm(ps[0:1, :], cond[:, c:c + 1], wst[b][:, c, :], c == 0, c == C_KC - 1, reads=[cond, wst[b]], writes=[ps])
        k.op('dve', lambda E: E.tensor_tensor(modbc[0:1, j * 512:(j + 1) * 512], ps[0:1, :], modbc[0:1, j * 512:(j + 1) * 512], ALU.add),
             reads=[ps, modbc], writes=[modbc])
    for j in range(8):
        ps = pa()
        k.mm(ps[:], ones1[:], modbc[0:1, j * 512:(j + 1) * 512], True, True, reads=[ones1, modbc], writes=[ps])
        k.op('act', lambda E: E.copy(modbc[:, j * 512:(j + 1) * 512], ps[:]), reads=[ps], writes=[modbc])
    gbc = k.sb("gbc", [128, C_D])
    k.dma('sp', gbc[:], g_d[0:1, :].partition_broadcast(128), writes=[gbc])
    k.op('dve', lambda E: E.scalar_tensor_tensor(modbc[:, 2 * C_D:3 * C_D], modbc[:, 2 * C_D:3 * C_D], 1.0, gbc[:], ALU.add, ALU.mult),
         reads=[modbc, gbc], writes=[modbc])
    if final:
        k.dma('sp', gbc[:], fin_d[0:1, :].partition_broadcast(128), reads=[modbc], writes=[gbc])

    x1 = k.sb("x1", [128, C_NT_, C_D])
    k.dma('sp', x1[:, 0:8, :], x_d[0:1024, :].rearrange("(c p) d -> p c d", p=128), writes=[x1])
    k.dma('act', x1[:, 8:16, :], x_d[1024:2048, :].rearrange("(c p) d -> p c d", p=128), writes=[x1])
    wo_b = k.sb("wo_b", [128, C_KC, C_D], BF16)
    for hh in range(2):
        k.dma('sp', wst[hh][:], wout_d[:, hh * 512:(hh + 1) * 512].rearrange("(c p) n -> p c n", p=128), writes=[wst[hh]])
        k.op('pool', lambda E: E.tensor_copy(wo_b[:, :, hh * 512:(hh + 1) * 512], wst[hh][:]), reads=[wst[hh]], writes=[wo_b])
    w1bs = [k.sb("w1b%d" % i, [128, C_KC, C_DE], BF16) for i in range(2)]
    oTb = w1bs[0]
    mtmp = k.sb("mtmp", [128, 512])
    for g in range(4):
        b = g % 2
        k.dma('sp', wst[b][:], oT_d[:, g * 512:(g + 1) * 512].rearrange("(c p) n -> p c n", p=128), reads=[wo_b], writes=[wst[b]])
        k.op('pool', lambda E: E.tensor_copy(oTb[:], wst[b][:]), reads=[wst[b]], writes=[oTb])
        for tt in range(4):
            ti = g * 4 + tt
            for dh in range(2):
                ps = pa()
                for c in range(C_KC):
                    k.mm(ps[:], oTb[:, c, tt * 128:(tt + 1) * 128], wo_b[:, c, dh * 512:(dh + 1) * 512], c == 0, c == C_KC - 1,
                         reads=[oTb, wo_b], writes=[ps])
                k.op('dve', lambda E: E.tensor_tensor(mtmp[:], ps[:], modbc[:, dh * 512:(dh + 1) * 512], ALU.mult), reads=[ps, modbc], writes=[mtmp])
                k.op('pool', lambda E: E.tensor_tensor(x1[:, ti, dh * 512:(dh + 1) * 512], x1[:, ti, dh * 512:(dh + 1) * 512], mtmp[:], ALU.add),
                     reads=[mtmp, x1], writes=[x1])

    h2T = k.sb("h2T", [128, C_KC, C_T], BF16)
    hscr = k.sb("hscr", [128, 2 * C_D])
    h2f = Buf(hscr[:, 0:C_D], "h2f")
    h2Tf = Buf(hscr[:, C_D:2 * C_D].rearrange("p (c n) -> p c n", c=C_KC), "h2Tf")
    junk = h2f
    wr = k.sb("wr", [128, C_KC, 36])
    brb = k.sb("brb", [128, 36])
    k.dma('sp', wr[:], wr_d[:].rearrange("(c p) n -> p c n", p=128), writes=[wr])
    k.dma('sp', brb[:], br_d[0:1, :].partition_broadcast(128), writes=[brb])
    W32 = k.sb("W32", [128, C_NT_, C_NE])
    lg = k.sb("lg", [128, 36])
    s1 = {n: k.sb("r_" + n, [128, 1]) for n in ("ss", "rstd", "gmax", "gs", "gate", "m1", "m2", "p1", "p2", "t")}
    ohg = k.sb("ohg", [128, 4])
    eg = k.sb("eg", [128, 4])
    el = k.sb("el", [128, 8])
    el2 = k.sb("el2", [128, 8])
    oh1 = k.sb("oh1", [128, 8])
    oh2 = k.sb("oh2", [128, 8])
    we8 = k.sb("we8", [128, 8])
    ps_r = k.ps("ps_r", [128, 36])
    ps_ts = [k.ps("ps_t%d" % i, [128, 128]) for i in range(2)]
    ps_t = ps_ts[0]
    for ti in range(C_NT_):
        ss, rstd = s1["ss"], s1["rstd"]
        k.op('act', lambda E: E.activation(junk[:], x1[:, ti, :], AF.Square, accum_out=ss[:]), reads=[x1], writes=[junk, ss])
        k.op('act', lambda E: E.activation(rstd[:], ss[:], AF.Ln, bias=C_EPS, scale=1.0 / C_D), reads=[ss], writes=[rstd])
        k.op('act', lambda E: E.activation(rstd[:], rstd[:], AF.Exp, scale=-0.5), reads=[rstd], writes=[rstd])
        k.op('dve', lambda E: E.scalar_tensor_tensor(h2f[:], x1[:, ti, :], rstd[:], modbc[:, 2 * C_D:3 * C_D], ALU.mult, ALU.mult),
             reads=[x1, rstd, modbc], writes=[h2f])
        k.op('dve', lambda E: E.tensor_tensor(h2f[:], h2f[:], modbc[:, C_D:2 * C_D], ALU.add), reads=[h2f, modbc], writes=[h2f])
        for c in range(C_KC):
            pst = ps_ts[c % 2]
            k.mm(pst[:], h2f[:, c * 128:(c + 1) * 128], ident[:], True, True, reads=[h2f, ident], writes=[pst])
            k.op('act', lambda E: E.copy(h2Tf[:, c, :], pst[:]), reads=[pst], writes=[h2Tf], waw=False)
            k.op('pool', lambda E: E.tensor_copy(h2T[:, c, ti * 128:(ti + 1) * 128], h2Tf[:, c, :]), reads=[h2Tf], writes=[h2T], waw=False)
        for c in range(C_KC):
            k.mm(ps_r[:], h2Tf[:, c, :], wr[:, c, :], c == 0, c == C_KC - 1, reads=[h2Tf, wr], writes=[ps_r])
        k.op('dve', lambda E: E.tensor_tensor(lg[:], ps_r[:], brb[:], ALU.add), reads=[ps_r, brb], writes=[lg])
        gmax, gs, gate, m1, m2, p1, p2, t_ = [s1[n] for n in ("gmax", "gs", "gate", "m1", "m2", "p1", "p2", "t")]
        k.op('dve', lambda E: E.tensor_reduce(gmax[:], lg[:, 0:4], AX.X, ALU.max), reads=[lg], writes=[gmax])
        ge_ap(k, ohg[:], lg[:, 0:4], gmax[:], [lg, gmax], [ohg])
        k.op('dve', lambda E: E.tensor_scalar_mul(t_[:], gmax[:], -1.0), reads=[gmax], writes=[t_])
        k.op('act', lambda E: E.activation(eg[:], lg[:, 0:4], AF.Exp, bias=t_[:], scale=1.0, accum_out=gs[:]), reads=[lg, t_], writes=[eg, gs])
        k.op('dve', lambda E: E.reciprocal(gate[:], gs[:]), reads=[gs], writes=[gate])
        k.op('dve', lambda E: E.tensor_scalar_mul(el[:], lg[:, 4:12], ohg[:, 0:1]), reads=[lg, ohg], writes=[el])
        for gq in range(1, 4):
            k.op('dve', lambda E: E.scalar_tensor_tensor(el[:], lg[:, 4 + 8 * gq:12 + 8 * gq], ohg[:, gq:gq + 1], el[:], ALU.mult, ALU.add),
                 reads=[lg, ohg, el], writes=[el])
        k.op('dve', lambda E: E.tensor_reduce(m1[:], el[:], AX.X, ALU.max), reads=[el], writes=[m1])
        ge_ap(k, oh1[:], el[:], m1[:], [el, m1], [oh1])
        k.op('dve', lambda E: E.scalar_tensor_tensor(el2[:], oh1[:], -1e9, el[:], ALU.mult, ALU.add), reads=[oh1, el], writes=[el2])
        k.op('dve', lambda E: E.tensor_reduce(m2[:], el2[:], AX.X, ALU.max), reads=[el2], writes=[m2])
        ge_ap(k, oh2[:], el2[:], m2[:], [el2, m2], [oh2])
        k.op('dve', lambda E: E.tensor_tensor(t_[:], m2[:], m1[:], ALU.subtract), reads=[m1, m2], writes=[t_])
        k.op('act', lambda E: E.activation(t_[:], t_[:], AF.Exp), reads=[t_], writes=[t_])
        k.op('dve', lambda E: E.tensor_scalar_add(t_[:], t_[:], 1.0), reads=[t_], writes=[t_])
        k.op('dve', lambda E: E.reciprocal(p1[:], t_[:]), reads=[t_], writes=[p1])
        k.op('dve', lambda E: E.tensor_scalar(p2[:], p1[:], -1.0, 1.0, ALU.mult, ALU.add), reads=[p1], writes=[p2])
        k.op('dve', lambda E: E.tensor_tensor(p1[:], p1[:], gate[:], ALU.mult), reads=[p1, gate], writes=[p1])
        k.op('dve', lambda E: E.tensor_tensor(p2[:], p2[:], gate[:], ALU.mult), reads=[p2, gate], writes=[p2])
        k.op('dve', lambda E: E.tensor_scalar_mul(we8[:], oh1[:], p1[:]), reads=[oh1, p1], writes=[we8])
        k.op('dve', lambda E: E.scalar_tensor_tensor(we8[:], oh2[:], p2[:], we8[:], ALU.mult, ALU.add), reads=[oh2, p2, we8], writes=[we8])
        for gq in range(4):
            k.op('dve', lambda E: E.tensor_scalar_mul(W32[:, ti, gq * 8:(gq + 1) * 8], we8[:], ohg[:, gq:gq + 1]), reads=[we8, ohg], writes=[W32], waw=False)

    w3b1 = Buf(modbc[:, 0:2 * C_D].bitcast(BF16).rearrange("p (c n) -> p c n", c=C_KC), "w3b1")
    w3b1.w, w3b1.r = dict(modbc.w), dict(modbc.r)
    w3bs = [k.sb("w3b0", [128, C_KC, C_DE], BF16), w3b1]
    w2b = Buf(hscr[:].bitcast(BF16).rearrange("p (c n) -> p c n", c=4), "w2b")
    def _merge(*ds):
        o = {}
        for d_ in ds:
            for kk, vv in d_.items():
                if o.get(kk, 0) < vv:
                    o[kk] = vv
        return o
    w2b.w, w2b.r = _merge(h2f.w, h2Tf.w), _merge(h2f.r, h2Tf.r)
    hid = Buf(wo_b[:].rearrange("p c n -> p (c n)").rearrange("p (f t) -> p f t", f=4), "hid")
    hid.w, hid.r = wo_b.w, wo_b.r
    sil = Buf(modbc[:, 2 * C_D:2 * C_D + 512], "sil")
    sil.w, sil.r = dict(modbc.w), dict(modbc.r)
    gtf = slice(3 * C_D, 4 * C_D)
    for e in range(C_NE):
        w1b, w3b = w1bs[e % 2], w3bs[e % 2]
        k.dma('sp', wst[0][:], w1_d[e].rearrange("(c p) n -> p c n", p=128), reads=[], writes=[wst[0]])
        k.op('pool', lambda E: E.tensor_copy(w1b[:], wst[0][:]), reads=[wst[0]], writes=[w1b])
        k.dma('sp', wst[1][:], w3_d[e].rearrange("(c p) n -> p c n", p=128), reads=[], writes=[wst[1]])
        k.op('pool', lambda E: E.tensor_copy(w3b[:], wst[1][:]), reads=[wst[1]], writes=[w3b])
        for fc in range(4):
            for g in range(4):
                p1_, p3_ = pa(), pa()
                for c in range(C_KC):
                    k.mm(p1_[:], w1b[:, c, fc * 128:(fc + 1) * 128], h2T[:, c, g * 512:(g + 1) * 512], c == 0, c == C_KC - 1, reads=[w1b, h2T], writes=[p1_])
                for c in range(C_KC):
                    k.mm(p3_[:], w3b[:, c, fc * 128:(fc + 1) * 128], h2T[:, c, g * 512:(g + 1) * 512], c == 0, c == C_KC - 1, reads=[w3b, h2T], writes=[p3_])
                k.op('act', lambda E: E.activation(sil[:], p1_[:], AF.Silu), reads=[p1_], writes=[sil])
                k.op('dve', lambda E: E.tensor_tensor(hid[:, fc, g * 512:(g + 1) * 512], sil[:], p3_[:], ALU.mult), reads=[sil, p3_], writes=[hid], waw=False)
        k.dma('sp', wst[0][:].rearrange("p c n -> p (c n)").rearrange("p (c n) -> p c n", c=4), w2_d[e].rearrange("(c p) n -> p c n", p=128),
              reads=[], writes=[wst[0]])
        w2v = wst[0][:].rearrange("p c n -> p (c n)").rearrange("p (c n) -> p c n", c=4)
        for fc in range(4):
            k.op('pool', lambda E: E.tensor_tensor(w2b[:, fc, :], w2v[:, fc, :], modbc[:, gtf], ALU.mult), reads=[wst[0], modbc], writes=[w2b], waw=(fc == 0))
        for ti in range(C_NT_):
            for dh in range(2):
                ps = pa()
                for fc in range(4):
                    k.mm(ps[:], hid[:, fc, ti * 128:(ti + 1) * 128], w2b[:, fc, dh * 512:(dh + 1) * 512], fc == 0, fc == 3, reads=[hid, w2b], writes=[ps])
                k.op('dve', lambda E: E.scalar_tensor_tensor(x1[:, ti, dh * 512:(dh + 1) * 512], ps[:], W32[:, ti, e:e + 1], x1[:, ti, dh * 512:(dh + 1) * 512],
                                                              ALU.mult, ALU.add), reads=[ps, W32, x1], writes=[x1], waw=False)
    k.barrier()
    for ti in range(C_NT_):
        if final:
            ss, rstd = s1["ss"], s1["rstd"]
            k.op('act', lambda E: E.activation(junk[:], x1[:, ti, :], AF.Square, accum_out=ss[:]), reads=[x1], writes=[junk, ss])
            k.op('act', lambda E: E.activation(rstd[:], ss[:], AF.Ln, bias=C_EPS, scale=1.0 / C_D), reads=[ss], writes=[rstd])
            k.op('act', lambda E: E.activation(rstd[:], rstd[:], AF.Exp, scale=-0.5), reads=[rstd], writes=[rstd])
            k.op('dve', lambda E: E.scalar_tensor_tensor(x1[:, ti, :], x1[:, ti, :], rstd[:], gbc[:], ALU.mult, ALU.mult), reads=[x1, rstd, gbc], writes=[x1])
    k.dma('sp', out_d[0:1024, :].rearrange("(c p) d -> p c d", p=128), x1[:, 0:8, :], reads=[x1], writes=[out_d])
    k.dma('act', out_d[1024:2048, :].rearrange("(c p) d -> p c d", p=128), x1[:, 8:16, :], reads=[x1], writes=[out_d])
    return k.finish([out_d])


def host_inputs_C(layer, inputs, x_tok_cores, oT_cores, w_out):
    ada_w = np.ascontiguousarray(np.asarray(inputs["ada_w"][layer])[:, 2 * C_D:6 * C_D])
    ada_b = np.ascontiguousarray(np.asarray(inputs["ada_b"][layer])[2 * C_D:6 * C_D].reshape(1, 4 * C_D))
    c = np.asarray(inputs["c"])
    wg = np.asarray(inputs["router_group_w"][layer])
    we = np.asarray(inputs["router_exp_w"][layer])
    wr = np.ascontiguousarray(np.concatenate([wg] + [we[g] for g in range(4)], axis=1))
    br = np.ascontiguousarray(np.concatenate([np.asarray(inputs["router_group_b"][layer]).reshape(-1),
                                               np.asarray(inputs["router_exp_b"][layer]).reshape(-1)]).reshape(1, 36))
    base = dict(w_out=np.ascontiguousarray(w_out), ada_w=ada_w, ada_b=ada_b,
                norm_g=np.ascontiguousarray(np.asarray(inputs["norm_ffn"][layer]).reshape(1, C_D)),
                w_router=wr, b_router=br, w1=np.asarray(inputs["exp_w1"][layer]), w3=np.asarray(inputs["exp_w3"][layer]),
                w2=np.asarray(inputs["exp_w2"][layer]), ident=np.eye(128, dtype=np.float32),
                final_g=np.ascontiguousarray(np.asarray(inputs["final_norm"]).reshape(1, C_D)))
    maps = []
    for core in range(8):
        b = core // 4
        m = dict(base)
        m["x_tok"] = x_tok_cores[core]
        m["oT"] = oT_cores[core]
        m["condT"] = np.ascontiguousarray(c[b].reshape(C_KC, 128).T)
        maps.append(m)
    return maps


_CACHE = {}


def _prog(name, builder):
    if name not in _CACHE:
        _CACHE[name] = builder()
    return _CACHE[name]


def _build_gdn_prog():
    k = K()
    o = build_gdn(k)
    return k.finish([o])


def _build_dsa_prog():
    k = K()
    o = build_dsa(k)
    return k.finish([o])


def _run(nc, maps):
    return run_bass_kernel_spmd(nc, maps, core_ids=list(range(8))).results


def kernel(**inputs):
    inputs = {k_: np.asarray(v) for k_, v in inputs.items()}
    x = inputs["x"]
    T = 2048
    x_tok = [np.ascontiguousarray(x[c // 4, (c % 4) * T:(c % 4 + 1) * T, :]) for c in range(8)]
    for layer in range(4):
        i = layer // 2
        kind = "even" if layer % 2 == 0 else "odd"
        xT = [np.ascontiguousarray(a.T) for a in x_tok]
        resA = _run(_prog("A_" + kind, lambda: build_A(kind)), host_inputs_A(kind, layer, inputs, xT))
        P = [r["projT"] for r in resA]
        full = [np.concatenate([P[b * 4 + j] for j in range(4)], axis=1) for b in range(2)]
        if kind == "even":
            Pb = [np.asarray(r["projbf"]) for r in resA]
            fullbf = [np.concatenate([Pb[b * 4 + j] for j in range(4)], axis=1) for b in range(2)]
            resG = _run(_prog("gdn", _build_gdn_prog), [host_inputs_gdn(layer, inputs, full, c) for c in range(8)])
            resD = _run(_prog("dsa", _build_dsa_prog), [host_inputs_dsa(full, fullbf, c) for c in range(8)])
            o_full = []
            for b in range(2):
                oa = np.concatenate([resG[b * 4 + h]["oa_tok"] for h in range(4)], axis=1)
                ob = np.zeros((8192, 512), np.float32)
                for r in range(4):
                    o = resD[b * 4 + r]["ob"]
                    for n in range(16):
                        t0 = (4 * n + r) * 128
                        ob[t0:t0 + 128] = o[n]
                o_full.append(np.concatenate([oa, ob], axis=1))
            w_out = inputs["even_w_out"][i]
        else:
            resG = _run(_prog("gla", build_gla), host_inputs_gla(layer, inputs, P))
            o_full = [np.concatenate([resG[b * 4 + h]["o_tok"] for h in range(4)], axis=1) for b in range(2)]
            w_out = inputs["gla_w_out"][i]
        oT = [np.ascontiguousarray(o_full[c // 4][(c % 4) * T:(c % 4 + 1) * T, :].T) for c in range(8)]
        final = layer == 3
        resC = _run(_prog("C_%d" % final, lambda: build_C(final)), host_inputs_C(layer, inputs, x_tok, oT, w_out))
        x_tok = [r["x_out"] for r in resC]
    out = np.zeros((2, 8192, 1024), np.float32)
    for c in range(8):
        out[c // 4, (c % 4) * T:(c % 4 + 1) * T, :] = x_tok[c]
    return out
```
